# Optimizing a Trainium2 kernel written in Bass

```python
import jax
import jax.numpy as jnp
from jax import lax
import numpy as np

D_MODEL = 1024
BATCH = 16
SEQ = 2048
DEPTH = 2

CHUNK = 64

D_MIX = D_MODEL
N_GROUPS = 4
GROUP_W = D_MIX // N_GROUPS
HEAD_DIM = 64

FOX_HEADS = GROUP_W // HEAD_DIM
FOX_DH = HEAD_DIM
Q_BLOCK = 128
FOX_BIAS_INIT = 2.0

GDN_HEADS = GROUP_W // HEAD_DIM
GDN_DK = HEAD_DIM
GDN_DV = HEAD_DIM
GDN_CONV = 4

HG_HEADS = GROUP_W // HEAD_DIM
HG_DK = HEAD_DIM
HG_DV = HEAD_DIM
HG_KW = HG_HEADS * HG_DK

GLA_HEADS = GROUP_W // HEAD_DIM
GLA_DV = HEAD_DIM
GLA_DK = HEAD_DIM // 2
GLA_KW = GLA_HEADS * GLA_DK
GLA_RANK = 16
GLA_NORM = 16.0

LA_BLOCK = 16

D_FF = ((8 * D_MODEL // 3 + 255) // 256) * 256
FFN_CONV = 3
EPS = 1e-6

IN_SIZES = (
    3 * GROUP_W, FOX_HEADS,
    3 * GROUP_W, GDN_HEADS, GDN_HEADS, GROUP_W,
    HG_KW, HG_KW, GROUP_W, GROUP_W,
    2 * GLA_KW, GROUP_W, GLA_RANK, GROUP_W,
)
IN_COLS = sum(IN_SIZES)

kernel_name = 'hybrid_fox_gdn_hgrn2_gla_convffn'


def _cut_points(sizes):
    cuts, acc = [], 0
    for s in sizes[:-1]:
        acc += s
        cuts.append(acc)
    return cuts


def rms_norm(x, g):
    xf = x.astype(jnp.float32)
    y = xf * lax.rsqrt(jnp.mean(xf * xf, axis=-1, keepdims=True) + EPS)
    return (y * g.astype(jnp.float32)).astype(x.dtype)


def l2norm(x):
    return x * lax.rsqrt(jnp.sum(x * x, axis=-1, keepdims=True) + EPS)


def split_heads(t, n):
    b, s, w = t.shape
    return t.reshape(b, s, n, w // n).transpose(0, 2, 1, 3)


def merge_heads(t):
    b, h, s, d = t.shape
    return t.transpose(0, 2, 1, 3).reshape(b, s, h * d)


def causal_dwconv(x, w):
    k_w = w.shape[0]
    s = x.shape[1]
    xp = jnp.pad(x, ((0, 0), (k_w - 1, 0), (0, 0)))
    y = xp[:, 0:s] * w[0]
    for j in range(1, k_w):
        y = y + xp[:, j:j + s] * w[j]
    return y


def fox_attention(q, k, v, log_f):
    b, h, s, d = q.shape
    c = jnp.cumsum(log_f, axis=-1)
    nb = s // Q_BLOCK
    qb = q.reshape(b, h, nb, Q_BLOCK, d).transpose(2, 0, 1, 3, 4)
    cb = c.reshape(b, h, nb, Q_BLOCK).transpose(2, 0, 1, 3)
    pos_k = jnp.arange(s)
    scale = d ** -0.5

    def one_block(args):
        qi, ci, bi = args
        pos_q = bi * Q_BLOCK + jnp.arange(Q_BLOCK)
        logits = jnp.einsum('bhqd,bhkd->bhqk', qi, k) * scale + ci[..., :, None] - c[:, :, None, :]
        logits = jnp.where(pos_k[None, :] <= pos_q[:, None], logits, -jnp.inf)
        probs = jax.nn.softmax(logits, axis=-1)
        return jnp.einsum('bhqk,bhkd->bhqd', probs, v)

    o = lax.map(one_block, (qb, cb, jnp.arange(nb)))
    return o.transpose(1, 2, 0, 3, 4).reshape(b, h, s, d)


def gated_delta_chunked(q, k, v, g, beta):
    b, h, s, dk = q.shape
    dv = v.shape[-1]
    c = CHUNK
    n = s // c
    q = (q * dk ** -0.5).reshape(b, h, n, c, dk)
    k = k.reshape(b, h, n, c, dk)
    v = v.reshape(b, h, n, c, dv)
    beta = beta.reshape(b, h, n, c)
    G = jnp.cumsum(g.reshape(b, h, n, c), axis=-1)
    causal = jnp.tril(jnp.ones((c, c), bool))
    strict = jnp.tril(jnp.ones((c, c), bool), -1)
    decay = jnp.exp(jnp.where(causal, G[..., :, None] - G[..., None, :], -jnp.inf))
    kb = k * beta[..., None]
    m = jnp.where(strict, jnp.einsum('bhnrd,bhnsd->bhnrs', kb, k) * decay, 0.0)
    t_mat = m + jnp.eye(c, dtype=m.dtype)
    w = lax.linalg.triangular_solve(t_mat, kb * jnp.exp(G)[..., None],
                                    left_side=True, lower=True, unit_diagonal=True)
    u = lax.linalg.triangular_solve(t_mat, v * beta[..., None],
                                    left_side=True, lower=True, unit_diagonal=True)
    a_qk = jnp.where(causal, jnp.einsum('bhnrd,bhnsd->bhnrs', q, k) * decay, 0.0)
    q_in = q * jnp.exp(G)[..., None]
    k_out = k * jnp.exp(G[..., -1:] - G)[..., None]
    a_chunk = jnp.exp(G[..., -1])

    def step(state, inp):
        qi, ki, wi, ui, ai_qk, ai = inp
        v_new = ui - jnp.einsum('bhcd,bhde->bhce', wi, state)
        o = jnp.einsum('bhcd,bhde->bhce', qi, state) + jnp.einsum('bhrs,bhse->bhre', ai_qk, v_new)
        state = state * ai[..., None, None] + jnp.einsum('bhcd,bhce->bhde', ki, v_new)
        return state, o

    xs = tuple(jnp.moveaxis(t, 2, 0) for t in (q_in, k_out, w, u, a_qk, a_chunk))
    _, o = lax.scan(step, jnp.zeros((b, h, dk, dv), q.dtype), xs)
    return jnp.moveaxis(o, 0, 2).reshape(b, h, s, dv)


def gla_chunked(q, k, v, log_a, scale):
    b, h, s, dk = q.shape
    dv = v.shape[-1]
    c = LA_BLOCK
    n = s // c
    q = (q * scale).reshape(b, h, n, c, dk)
    k = k.reshape(b, h, n, c, dk)
    v = v.reshape(b, h, n, c, dv)
    G = jnp.cumsum(log_a.reshape(b, h, n, c, dk), axis=3)
    causal = jnp.tril(jnp.ones((c, c), bool))
    rel = jnp.where(causal[:, :, None], G[:, :, :, :, None, :] - G[:, :, :, None, :, :], -jnp.inf)
    a_intra = jnp.einsum('bhnrd,bhnsd,bhnrsd->bhnrs', q, k, jnp.exp(rel))
    o_intra = jnp.einsum('bhnrs,bhnse->bhnre', a_intra, v)
    g_last = G[:, :, :, -1]
    q_in = q * jnp.exp(G)
    k_out = k * jnp.exp(g_last[:, :, :, None, :] - G)
    a_chunk = jnp.exp(g_last)

    def step(state, inp):
        qi, ki, vi, ai = inp
        o = jnp.einsum('bhcd,bhde->bhce', qi, state)
        state = state * ai[..., None] + jnp.einsum('bhcd,bhce->bhde', ki, vi)
        return state, o

    xs = tuple(jnp.moveaxis(t, 2, 0) for t in (q_in, k_out, v, a_chunk))
    _, o_inter = lax.scan(step, jnp.zeros((b, h, dk, dv), q.dtype), xs)
    return (o_intra + jnp.moveaxis(o_inter, 0, 2)).reshape(b, h, s, dv)


def hgrn_lower_bounds(lb_param):
    cs = jnp.cumsum(jax.nn.softmax(lb_param.astype(jnp.float32), axis=0), axis=0)
    return cs - cs[0:1]


def token_mix(h, w_in, fox_qn_g, fox_kn_g, fox_b_f, fox_on_g, gdn_conv_w, gdn_a_log,
              gdn_dt_bias, gdn_on_g, lb, hg_on_g, gla_w_gk, gla_b_gk, gla_on_g):
    p = (h @ w_in).astype(jnp.float32)
    (fox_qkv, fox_f, gdn_qkv, gdn_b, gdn_a, gdn_z, hg_q, hg_f, hg_i, hg_g,
     gla_qk, gla_v, gla_gk, gla_g) = jnp.split(p, _cut_points(IN_SIZES), axis=-1)

    q, k, v = jnp.split(fox_qkv, 3, axis=-1)
    q = rms_norm(split_heads(q, FOX_HEADS), fox_qn_g)
    k = rms_norm(split_heads(k, FOX_HEADS), fox_kn_g)
    log_f = jax.nn.log_sigmoid(fox_f + fox_b_f).transpose(0, 2, 1)
    o_a = rms_norm(fox_attention(q, k, split_heads(v, FOX_HEADS), log_f), fox_on_g)

    qkv = jax.nn.silu(causal_dwconv(gdn_qkv, gdn_conv_w))
    q, k, v = jnp.split(qkv, 3, axis=-1)
    beta = jax.nn.sigmoid(gdn_b).transpose(0, 2, 1)
    g = (-jnp.exp(gdn_a_log) * jax.nn.softplus(gdn_a + gdn_dt_bias)).transpose(0, 2, 1)
    o_b = gated_delta_chunked(l2norm(split_heads(q, GDN_HEADS)), l2norm(split_heads(k, GDN_HEADS)),
                              split_heads(v, GDN_HEADS), g, beta)
    o_b = rms_norm(o_b, gdn_on_g) * jax.nn.silu(split_heads(gdn_z, GDN_HEADS))

    lbh = lb.reshape(HG_HEADS, 1, HG_DK)
    f_logit = split_heads(hg_f, HG_HEADS)
    log_forget = jnp.logaddexp(jnp.log(lbh), jnp.log1p(-lbh) + jax.nn.log_sigmoid(f_logit))
    k_in = (1.0 - lbh) * jax.nn.sigmoid(-f_logit)
    o_c = gla_chunked(jax.nn.silu(split_heads(hg_q, HG_HEADS)), k_in, split_heads(hg_i, HG_HEADS),
                      log_forget, HG_DK ** -0.5)
    o_c = rms_norm(o_c, hg_on_g) * jax.nn.silu(split_heads(hg_g, HG_HEADS))

    q, k = jnp.split(gla_qk, 2, axis=-1)
    log_a = jax.nn.log_sigmoid(gla_gk @ gla_w_gk + gla_b_gk) / GLA_NORM
    o_d = gla_chunked(split_heads(q, GLA_HEADS), split_heads(k, GLA_HEADS), split_heads(gla_v, GLA_HEADS),
                      split_heads(log_a, GLA_HEADS), GLA_DK ** -0.5)
    o_d = rms_norm(o_d, gla_on_g) * jax.nn.silu(split_heads(gla_g, GLA_HEADS))

    return jnp.concatenate([merge_heads(o_a), merge_heads(o_b), merge_heads(o_c), merge_heads(o_d)], axis=-1)


def conv_glu_ffn(h, w_up, conv_w, conv_b, w_down):
    u = causal_dwconv(h @ w_up, conv_w) + conv_b
    gate, up = jnp.split(u, 2, axis=-1)
    return (jax.nn.silu(gate) * up) @ w_down


def setup_inputs(seed: int = 0) -> dict:
    key = jax.random.key(seed)
    ks = jax.random.split(key, 24)
    f32 = jnp.float32

    def nrm(k, shape, scale):
        return scale * jax.random.normal(k, shape, f32)

    def gain(k, n):
        return 1.0 + 0.1 * jax.random.normal(k, (DEPTH, n), f32)

    dt = jnp.exp(jax.random.uniform(ks[8], (DEPTH, GDN_HEADS), f32, jnp.log(1e-3), jnp.log(1e-1)))
    return {
        'x': nrm(ks[0], (BATCH, SEQ, D_MODEL), 1.0),
        'norm1_g': gain(ks[1], D_MODEL),
        'w_in': nrm(ks[2], (DEPTH, D_MODEL, IN_COLS), D_MODEL ** -0.5),
        'fox_qn_g': gain(ks[3], FOX_DH),
        'fox_kn_g': gain(ks[4], FOX_DH),
        'fox_b_f': FOX_BIAS_INIT + nrm(ks[5], (DEPTH, FOX_HEADS), 0.1),
        'fox_on_g': gain(ks[6], FOX_DH),
        'gdn_conv_w': nrm(ks[7], (DEPTH, GDN_CONV, 3 * GROUP_W), GDN_CONV ** -0.5),
        'gdn_a_log': jnp.log(jax.random.uniform(ks[9], (DEPTH, GDN_HEADS), f32, 1.0, 16.0)),
        'gdn_dt_bias': dt + jnp.log(-jnp.expm1(-dt)),
        'gdn_on_g': gain(ks[10], GDN_DV),
        'hg_lb': nrm(ks[11], (DEPTH, HG_KW), 0.1),
        'hg_on_g': gain(ks[12], HG_DV),
        'gla_w_gk': nrm(ks[13], (DEPTH, GLA_RANK, GLA_KW), GLA_RANK ** -0.5),
        'gla_b_gk': nrm(ks[14], (DEPTH, GLA_KW), 0.1),
        'gla_on_g': gain(ks[15], GLA_DV),
        'w_out': nrm(ks[16], (DEPTH, D_MIX, D_MODEL), D_MIX ** -0.5),
        'norm2_g': gain(ks[17], D_MODEL),
        'w_up': nrm(ks[18], (DEPTH, D_MODEL, 2 * D_FF), D_MODEL ** -0.5),
        'ffn_conv_w': nrm(ks[19], (DEPTH, FFN_CONV, 2 * D_FF), FFN_CONV ** -0.5),
        'ffn_conv_b': nrm(ks[20], (DEPTH, 2 * D_FF), 0.02),
        'w_down': nrm(ks[21], (DEPTH, D_FF, D_MODEL), D_FF ** -0.5),
    }


def reference(x, norm1_g, w_in, fox_qn_g, fox_kn_g, fox_b_f, fox_on_g, gdn_conv_w, gdn_a_log,
              gdn_dt_bias, gdn_on_g, hg_lb, hg_on_g, gla_w_gk, gla_b_gk, gla_on_g, w_out,
              norm2_g, w_up, ffn_conv_w, ffn_conv_b, w_down):
    lower_bounds = hgrn_lower_bounds(hg_lb)
    for l in range(DEPTH):
        mixed = token_mix(rms_norm(x, norm1_g[l]), w_in[l], fox_qn_g[l], fox_kn_g[l], fox_b_f[l],
                          fox_on_g[l], gdn_conv_w[l], gdn_a_log[l], gdn_dt_bias[l], gdn_on_g[l],
                          lower_bounds[l], hg_on_g[l], gla_w_gk[l], gla_b_gk[l], gla_on_g[l])
        x = x + mixed.astype(x.dtype) @ w_out[l]
        x = x + conv_glu_ffn(rms_norm(x, norm2_g[l]), w_up[l], ffn_conv_w[l], ffn_conv_b[l], w_down[l])
    return x
```

```python
import contextlib
import os
import numpy as np
import concourse.bass as bass
import concourse.mybir as mybir
from concourse.bass_utils import run_bass_kernel_spmd

F32 = mybir.dt.float32
BF16 = mybir.dt.bfloat16
AF = mybir.ActivationFunctionType
ALU = mybir.AluOpType

ENGS = ('pe', 'act', 'dve', 'pool', 'sp')


class KB:
    NDMASEM = 32

    def __init__(self, nc):
        self.nc = nc
        self.stack = contextlib.ExitStack()
        self.ops = {e: [] for e in ENGS}
        self.cnt = {e: 0 for e in ENGS}
        self.known = {e: {} for e in ENGS}
        self.writers = {}
        self.readers = {}
        self.sem = {}
        for e in ENGS:
            self.sem[e] = self.stack.enter_context(nc.semaphore("s_" + e))
        self.dsem = [self.stack.enter_context(nc.semaphore("d%d" % i)) for i in range(self.NDMASEM)]
        self.dcnt = [0] * self.NDMASEM
        self.dnext = {}
        self.out_waits = []
        self.labels = {} if os.environ.get('KB_LABELS') else None
        self.pending = {e: {} for e in ENGS}

    def sb(self, name, shape, dt):
        self.uid = getattr(self, 'uid', 0) + 1
        return self.stack.enter_context(self.nc.sbuf_tensor("%s_%d" % (name, self.uid), list(shape), dt))

    def ps(self, name, shape, dt):
        return self.stack.enter_context(self.nc.psum_tensor(name, list(shape), dt))

    def barrier(self):
        snap = {e: c for e, c in self.cnt.items() if c > 0}
        for i, c in enumerate(self.dcnt):
            if c > 0:
                snap[('d', i)] = c
        for e in ENGS:
            for k, v in snap.items():
                if k != e and self.pending[e].get(k, 0) < v:
                    self.pending[e][k] = v

    def _deps(self, eng, r_, w_):
        deps = dict(self.pending[eng])
        self.pending[eng] = {}
        for t in r_:
            for k, v in self.writers.get(t, {}).items():
                if deps.get(k, 0) < v:
                    deps[k] = v
            if isinstance(t, tuple) and t[0] in ('ps', 'pt'):
                for k, v in self.readers.get(t, {}).items():
                    if k != eng and deps.get(k, 0) < v:
                        deps[k] = v
        for t in w_:
            for d in (self.writers.get(t, {}), self.readers.get(t, {})):
                for k, v in d.items():
                    if deps.get(k, 0) < v:
                        deps[k] = v
        waits = []
        kn = self.known[eng]
        for k, v in deps.items():
            if kn.get(k, 0) < v:
                kn[k] = v
                waits.append((k, v))
        return waits

    def capture(self, fn):
        self._cap = []
        try:
            fn()
        finally:
            c, self._cap = self._cap, None
        return c

    def commit_interleaved(self, streams):
        n = max(len(x) for x in streams)
        for i in range(n):
            for x in streams:
                if i < len(x):
                    kind, a, k = x[i]
                    lab = k.pop('_label', None)
                    if lab is not None:
                        self.label = lab
                    (self.op if kind == 'op' else self.dma)(*a, **k)

    def op(self, eng, *fns, r_=(), w_=()):
        if getattr(self, '_cap', None) is not None:
            self._cap.append(('op', (eng,) + tuple(fns), dict(r_=r_, w_=w_, _label=getattr(self, 'label', ''))))
            return 0
        waits = self._deps(eng, r_, w_)
        self.cnt[eng] += 1
        idx = self.cnt[eng]
        self.ops[eng].append((waits, fns, ('eng', eng, getattr(self, 'label', ''))))
        for t in r_:
            self.readers.setdefault(t, {})[eng] = idx
        for t in w_:
            self.writers.setdefault(t, {})[eng] = idx
        return idx

    def dma(self, eng, out, in_, r_=(), w_=(), out_final=False, **kw):
        if getattr(self, '_cap', None) is not None:
            self._cap.append(('dma', (eng, out, in_), dict(r_=r_, w_=w_, out_final=out_final, **kw)))
            return
        lo, hi = (0, 16) if eng == 'pool' else (16, self.NDMASEM)
        si = self.dnext.get(eng, lo)
        self.dnext[eng] = lo + (si + 1 - lo) % (hi - lo)
        key = ('d', si)
        waits = self._deps(eng, r_, w_)
        if self.dcnt[si] > 0 and self.known[eng].get(key, 0) < self.dcnt[si]:
            self.known[eng][key] = self.dcnt[si]
            waits.append((key, self.dcnt[si]))
        self.dcnt[si] += 1
        val = self.dcnt[si]
        self.ops[eng].append((waits, (('dma_start', (), dict(out=out, in_=in_, **kw)),), ('dma', si)))
        for t in r_:
            self.readers.setdefault(t, {})[key] = val
        for t in w_:
            self.writers.setdefault(t, {})[key] = val
        if out_final:
            self.out_waits.append((key, val))

    def _semof(self, key):
        if isinstance(key, tuple):
            return self.dsem[key[1]], 16
        return self.sem[key], 1

    def emit(self):
        nc = self.nc
        fin = []
        for key, val in self.out_waits:
            fin.append((key, val))
        with nc.Block() as block:
            def body(engname):
                def f(e):
                    for waits, fns, kind in self.ops[engname]:
                        for k, v in waits:
                            s, mult = self._semof(k)
                            e.wait_ge(s, v * mult)
                        last = None
                        for (nm, a, k) in fns:
                            last = getattr(e, nm)(*a, **k)
                            if self.labels is not None and len(kind) > 2:
                                try:
                                    self.labels[last.ins.name] = kind[2]
                                except Exception:
                                    pass
                        if kind[0] == 'eng':
                            last.then_inc(self.sem[engname], 1)
                        else:
                            last.then_inc(self.dsem[kind[1]], 16)
                    if engname == 'sp':
                        for k, v in fin:
                            s, mult = self._semof(k)
                            e.wait_ge(s, v * mult)
                return f
            block.tensor(body('pe'))
            block.scalar(body('act'))
            block.vector(body('dve'))
            block.gpsimd(body('pool'))
            block.sync(body('sp'))
        self.stack.close()


D = 1024
DFF = 2816
PAD = 4
EPS = 1e-6
NCORES = 8

FCH = (['fq%d' % h for h in range(4)] + ['fk%d' % h for h in range(4)] +
       ['gq0', 'gq1', 'gk0', 'gk1', 'gv0', 'gv1', 'gb0', 'gb1', 'ga0', 'ga1'] + ['gz%d' % h for h in range(4)] +
       ['hq0', 'hq1', 'hf0', 'hf1'] + ['hg%d' % h for h in range(4)] +
       ['lq0', 'lq1', 'lk0', 'lk1', 'lgk'] + ['lg%d' % h for h in range(4)])
FIDX = {n: i for i, n in enumerate(FCH)}
NCF = len(FCH)

PPC = {}
_o = 0
for _n, _w in [('n1g', 8), ('n2g', 8), ('fqg', 1), ('fkg', 1), ('fog', 1), ('fbf', 4), ('gcw', 24), ('galog', 2),
               ('gdt', 2), ('gog', 1), ('hlb0', 2), ('hlb1', 2), ('hog', 1), ('lbgk', 2), ('log', 1),
               ('fcw', 132), ('fcb', 44)]:
    PPC[_n] = _o
    _o += _w
NPP = _o

OFF = {}
_o = 0
for _n, _w in [('fox_qkv', 768), ('fox_f', 4), ('gdn_qkv', 768), ('gdn_b', 4), ('gdn_a', 4), ('gdn_z', 256),
               ('hg_q', 256), ('hg_f', 256), ('hg_i', 256), ('hg_g', 256), ('gla_qk', 256), ('gla_v', 256),
               ('gla_gk', 16), ('gla_g', 256)]:
    OFF[_n] = _o
    _o += _w


def _gla_pad_cols(base, g):
    idx = -np.ones(128, np.int64)
    idx[0:32] = base + (2 * g) * 32 + np.arange(32)
    idx[64:96] = base + (2 * g + 1) * 32 + np.arange(32)
    return idx


def pack_weights(inp):
    L = inp['w_in'].shape[0]
    wif = np.zeros((L, NCF, 128, 8, 128), np.float32)
    wit = np.zeros((L, 128, 8, 768), np.float32)
    wo = np.zeros((L, 4, 64, 4, 1024), np.float32)
    wup = np.zeros((L, 44, 128, 8, 128), np.float32)
    wdn = np.zeros((L, 8, 128, 22, 128), np.float32)
    wgk = np.zeros((L, 16, 256), np.float32)
    pp = np.zeros((L, 128, NPP), np.float32)
    for l in range(L):
        W = np.asarray(inp['w_in'][l])
        Wk = W.reshape(8, 128, -1).transpose(1, 0, 2)

        def put(name, cols):
            cols = np.asarray(cols)
            m = len(cols)
            ok = cols >= 0
            wif[l, FIDX[name], :, :, np.nonzero(ok)[0]] = Wk[:, :, cols[ok]].transpose(2, 0, 1)

        for h in range(4):
            fcol = [OFF['fox_f'] + h] * 4
            put('fq%d' % h, list(OFF['fox_qkv'] + h * 64 + np.arange(64)) + fcol)
            put('fk%d' % h, list(OFF['fox_qkv'] + 256 + h * 64 + np.arange(64)) + fcol)
            put('gz%d' % h, OFF['gdn_z'] + h * 64 + np.arange(64))
            put('hg%d' % h, OFF['hg_g'] + h * 64 + np.arange(64))
            put('lg%d' % h, OFF['gla_g'] + h * 64 + np.arange(64))
        for g in range(2):
            put('gq%d' % g, OFF['gdn_qkv'] + g * 128 + np.arange(128))
            put('gk%d' % g, OFF['gdn_qkv'] + 256 + g * 128 + np.arange(128))
            put('gv%d' % g, OFF['gdn_qkv'] + 512 + g * 128 + np.arange(128))
            put('gb%d' % g, [OFF['gdn_b'] + 2 * g] * 64 + [OFF['gdn_b'] + 2 * g + 1] * 64)
            put('ga%d' % g, [OFF['gdn_a'] + 2 * g] * 64 + [OFF['gdn_a'] + 2 * g + 1] * 64)
            put('hq%d' % g, OFF['hg_q'] + g * 128 + np.arange(128))
            put('hf%d' % g, OFF['hg_f'] + g * 128 + np.arange(128))
            put('lq%d' % g, _gla_pad_cols(OFF['gla_qk'], g))
            put('lk%d' % g, _gla_pad_cols(OFF['gla_qk'] + 128, g))
        put('lgk', OFF['gla_gk'] + np.arange(16))
        wit[l, :, :, 0:256] = Wk[:, :, OFF['fox_qkv'] + 512:OFF['fox_qkv'] + 768]
        wit[l, :, :, 256:512] = Wk[:, :, OFF['hg_i']:OFF['hg_i'] + 256]
        wit[l, :, :, 512:768] = Wk[:, :, OFF['gla_v']:OFF['gla_v'] + 256]
        Wo = np.asarray(inp['w_out'][l])
        wo[l] = Wo.reshape(4, 4, 64, 1024).transpose(0, 2, 1, 3)
        Wu = np.asarray(inp['w_up'][l]).reshape(8, 128, 2 * DFF).transpose(1, 0, 2)
        for j in range(22):
            wup[l, 2 * j] = Wu[:, :, j * 128:(j + 1) * 128]
            wup[l, 2 * j + 1] = Wu[:, :, DFF + j * 128:DFF + (j + 1) * 128]
        Wd = np.asarray(inp['w_down'][l]).reshape(22, 128, 1024).transpose(1, 0, 2)
        for oc in range(8):
            wdn[l, oc] = Wd[:, :, oc * 128:(oc + 1) * 128]
        gw = np.asarray(inp['gla_w_gk'][l])
        for g in range(2):
            idx = _gla_pad_cols(0, g)
            ok = idx >= 0
            wgk[l, :, g * 128 + np.nonzero(ok)[0]] = gw[:, idx[ok]].T
        P = pp[l]
        P[:, PPC['n1g']:PPC['n1g'] + 8] = np.asarray(inp['norm1_g'][l]).reshape(8, 128).T
        P[:, PPC['n2g']:PPC['n2g'] + 8] = np.asarray(inp['norm2_g'][l]).reshape(8, 128).T
        for nm, key in [('fqg', 'fox_qn_g'), ('fkg', 'fox_kn_g'), ('fog', 'fox_on_g'), ('gog', 'gdn_on_g'),
                        ('hog', 'hg_on_g'), ('log', 'gla_on_g')]:
            v = np.asarray(inp[key][l])
            P[0:64, PPC[nm]] = v
            P[64:128, PPC[nm]] = v
        for h in range(4):
            P[64:68, PPC['fbf'] + h] = np.asarray(inp['fox_b_f'][l])[h]
        cw = np.asarray(inp['gdn_conv_w'][l])
        for c in range(6):
            P[:, PPC['gcw'] + c * 4:PPC['gcw'] + c * 4 + 4] = cw[:, c * 128:(c + 1) * 128].T
        for g in range(2):
            for nm, key in [('galog', 'gdn_a_log'), ('gdt', 'gdn_dt_bias')]:
                v = np.asarray(inp[key][l])
                P[0:64, PPC[nm] + g] = v[2 * g]
                P[64:128, PPC[nm] + g] = v[2 * g + 1]
            P[:, PPC['hlb0'] + g] = np.asarray(inp['hg_lb'][0])[g * 128:(g + 1) * 128]
            P[:, PPC['hlb1'] + g] = np.asarray(inp['hg_lb'][1])[g * 128:(g + 1) * 128]
            idx = _gla_pad_cols(0, g)
            ok = idx >= 0
            P[np.nonzero(ok)[0], PPC['lbgk'] + g] = np.asarray(inp['gla_b_gk'][l])[idx[ok]]
        fw = np.asarray(inp['ffn_conv_w'][l])
        fb = np.asarray(inp['ffn_conv_b'][l])
        for j in range(22):
            for t, base in enumerate((j * 128, DFF + j * 128)):
                c = 2 * j + t
                P[:, PPC['fcw'] + c * 3:PPC['fcw'] + c * 3 + 3] = fw[:, base:base + 128].T
                P[:, PPC['fcb'] + c] = fb[base:base + 128]
    return dict(wif=wif, wit=wit, wo=wo, wup=wup, wdn=wdn, wgk=wgk, pp=pp)


def I(name, *a, **k):
    return (name, a, k)


def bcl(ap, n):
    return bass.AP(ap.tensor, ap.offset, [list(x) for x in ap.ap[:-1]] + [[0, n]])


def bcm(ap, n):
    a = [list(x) for x in ap.ap]
    return bass.AP(ap.tensor, ap.offset, [a[0], [0, n]] + a[1:])


def v3(ap, b):
    return ap.rearrange("p (a b) -> p a b", b=b)


def build_program(S, NSEQ, L, MIXERS=(0, 1, 2, 3), FFN=True):
    nc = bass.Bass("TRN2", target_bir_lowering=False)
    NB = S // 512
    dr = {}
    dr['xT'] = nc.dram_tensor("xT", [NSEQ, 128, 8, S], F32, kind="ExternalInput").ap()
    shapes = dict(wif=[L, NCF, 128, 8, 128], wit=[L, 128, 8, 768], wo=[L, 4, 64, 4, 1024],
                  wup=[L, 44, 128, 8, 128], wdn=[L, 8, 128, 22, 128], wgk=[L, 16, 256])
    wb = {}
    for k, sh in shapes.items():
        dr[k] = nc.dram_tensor(k, sh, F32, kind="ExternalInput").ap()
        wb[k] = nc.dram_tensor(k + "_b", sh, BF16, kind="Internal").ap()
    dr['pp'] = nc.dram_tensor("pp", [L, 128, NPP], F32, kind="ExternalInput").ap()
    yT = nc.dram_tensor("yT", [NSEQ, 128, 8, S], F32, kind="ExternalOutput").ap()

    kb = KB(nc)
    op = kb.op

    def flat(ap):
        names = " ".join("abcdefg"[:len(ap.shape)])
        f = ap.rearrange("%s -> (%s)" % (names, names))
        n = f.shape[0]
        fdim = 2048 if n % 2048 == 0 else 256
        return f.rearrange("(r f) -> r f", f=fdim)

    xT = kb.sb("xT_sb", [128, 8, S], F32)
    hT = kb.sb("hT_sb", [128, 8, PAD + S], BF16)
    MX = kb.sb("MX", [64, 4, 512], BF16)
    ppt = kb.sb("ppt", [128, NPP], F32)
    ident = kb.sb("ident", [128, 128], BF16)
    tri = kb.sb("tri", [128, 128], BF16)
    negU = kb.sb("negU", [64, 64], F32)
    negL = kb.sb("negL", [64, 64], F32)
    eye = kb.sb("eye", [64, 64], F32)
    ones = kb.sb("ones", [128, 128], BF16)
    bd = kb.sb("bd", [128, 128], BF16)
    o64 = kb.sb("o64", [64, 64], BF16)
    wst = kb.sb("wst", [96, 64], BF16)
    cm = kb.sb("cm", [128, 512], F32)
    onesf = kb.sb("onesf", [128, 512], F32)
    epsc = kb.sb("epsc", [128, 1], F32)
    aug = kb.sb("aug", [128, 6], F32)
    der = kb.sb("der", [128, 16], F32)
    zc = kb.sb("zc", [128, 4], F32)

    def sel(t, ap_, cmp, fill, base, pat, cmul=1):
        op('pool', I('affine_select', out=ap_, in_=ap_, pattern=pat, compare_op=cmp, fill=fill, base=base,
                     channel_multiplier=cmul), r_=[t], w_=[t])
    for t_, tl, v in [('ident', ident, 0.0), ('tri', tri, 0.0), ('negU', negU, 0.0), ('negL', negL, 0.0),
                      ('eye', eye, 0.0), ('ones', ones, 1.0), ('bd', bd, 1.0), ('o64', o64, 1.0 / 64),
                      ('wst', wst, 1.0 / 64), ('cm', cm, 1.0), ('onesf', onesf, 1.0), ('epsc', epsc, EPS),
                      ('aug', aug, 0.0), ('hT', hT, 0.0)]:
        op('pool', I('memset', tl[:], v), w_=[t_])
    sel('ident', ident[:], ALU.not_equal, 1.0, 0, [[-1, 128]])
    sel('tri', tri[:], ALU.is_gt, 1.0, 0, [[-1, 128]])
    sel('negU', negU[:], ALU.is_ge, -1.0, 0, [[-1, 64]])
    sel('negL', negL[:], ALU.is_ge, -1.0, 0, [[1, 64]], cmul=-1)
    sel('eye', eye[:], ALU.not_equal, 1.0, 0, [[-1, 64]])
    op('pool', I('memset', bd[0:64, 64:128], 0.0), w_=['bd'])
    op('pool', I('memset', bd[64:128, 0:64], 0.0), w_=['bd'])
    op('pool', I('memset', wst[64:96, :], 0.0), w_=['wst'])
    op('pool', I('memset', wst[64:65, :], EPS), w_=['wst'])
    op('pool', I('memset', v3(cm[:], 64)[:, :, 0:1], 0.0), w_=['cm'])
    op('pool', I('memset', zc[:], 0.0), w_=['zc'])
    for r0_ in (0, 64):
        sel('zc', zc[:, 0:1], ALU.not_equal, -1.0, -r0_, [[0, 1]])
        sel('zc', zc[:, 1:2], ALU.not_equal, 1.0, -(r0_ + 1), [[0, 1]])
        sel('zc', zc[:, 2:3], ALU.not_equal, 1.0, -(r0_ + 1), [[0, 1]])
        sel('zc', zc[:, 3:4], ALU.not_equal, 1.0, -r0_, [[0, 1]])
    sel('aug', aug[:, 0:1], ALU.not_equal, 1.0, -64, [[0, 1]])
    sel('aug', aug[:, 1:2], ALU.not_equal, 1.0, -65, [[0, 1]])
    sel('aug', aug[:, 2:3], ALU.is_gt, 1.0, 66, [[0, 1]], cmul=-1)
    sel('aug', aug[:, 3:4], ALU.not_equal, -1.0, -66, [[0, 1]])
    sel('aug', aug[:, 4:5], ALU.not_equal, -1.0, -67, [[0, 1]])
    sel('aug', aug[:, 5:6], ALU.is_ge, 1.0, -66, [[0, 1]])

    for l in range(L):
        for k in ('wif', 'wit', 'wo', 'wgk', 'wup', 'wdn'):
            src = flat(dr[k][l])
            dst = flat(wb[k][l])
            R = src.shape[0]
            for r0 in range(0, R, 4096):
                r1 = min(R, r0 + 4096)
                kb.dma('pool', dst[r0:r1], src[r0:r1], w_=[(k, l)])

    NPS = 7
    psb = [kb.ps("ps%d" % i, [128, 512], F32) for i in range(NPS)]
    ptb = kb.ps("ptb", [128, 1024], BF16)
    st = {'ps': 0, 'pt': 0, 'wf': 0}

    STREAM = {'s': None}

    def _rot(key, n):
        sid = STREAM['s']
        if sid is None:
            i = st.get(key, 0)
            st[key] = (i + 1) % n
            return i
        lo = 0 if sid == 0 else (n + 1) // 2
        hi = (n + 1) // 2 if sid == 0 else n
        k2 = (key, sid)
        i = st.get(k2, lo)
        st[k2] = lo + (i + 1 - lo) % (hi - lo)
        return i

    def PS():
        i = _rot('ps', NPS - 2)
        return psb[i], ('ps', i)

    def PSA():
        if STREAM['s'] is not None:
            i = NPS - 2 + STREAM['s']
        else:
            i = NPS - 2 + st.get('psa', 0)
            st['psa'] = (st.get('psa', 0) + 1) % 2
        return psb[i], ('ps', i)

    def two_streams(fa, fb):
        caps = []
        for sid, f in enumerate((fa, fb)):
            STREAM['s'] = sid
            caps.append(kb.capture(f))
        STREAM['s'] = None
        kb.commit_interleaved(caps)

    def PT():
        i = st['pt']
        st['pt'] = (i + 1) % 2
        return ptb[:, i * 512:(i + 1) * 512], ('pt', 0)

    NSF, NSB = 10, 12
    sf = [kb.sb("sf%d" % i, [128, 512], F32) for i in range(NSF)]
    sbf = [kb.sb("sbf%d" % i, [128, 512], BF16) for i in range(NSB)]
    stc = {'f': 0, 'b': 0}

    def SF():
        i = _rot('sf', NSF)
        return sf[i], ('sf', i)

    def SB():
        i = _rot('sb', NSB)
        return sbf[i], ('sbf', i)

    def pc(name, j=0, r0=0, r1=128):
        return ppt[r0:r1, PPC[name] + j:PPC[name] + j + 1]

    class Phase:
        def __enter__(self):
            kb.barrier()
            self.prev = kb.stack
            kb.stack = contextlib.ExitStack()
            return self

        def __exit__(self, *a):
            kb.barrier()
            kb.stack.close()
            kb.stack = self.prev

    W = {}

    def alloc_mixer_weights():
        W['wfs'] = [kb.sb("wf%d" % i, [128, 8, 128], BF16) for i in range(4)]
        W['wits'] = kb.sb("wits", [128, 8, 256], BF16)
        W['wos'] = kb.sb("wos", [64, 4, 1024], BF16)
        W['wgks'] = kb.sb("wgks", [16, 256], BF16)

    def load_wf(l, name):
        i = _rot('wf', 4)
        kb.dma('sp', W['wfs'][i][:], wb['wif'][l, FIDX[name]], r_=[('wif', l)], w_=[('wf', i)])
        return W['wfs'][i], ('wf', i)

    def proj_f(l, name, M, c0, n):
        wt, wk = load_wf(l, name)
        ps, pk = PS()
        op('pe', *[I('matmul', ps[0:M, 0:n], wt[:, kc, 0:M], hT[:, kc, c0:c0 + n], start=(kc == 0), stop=(kc == 7))
                   for kc in range(8)], r_=[wk, 'hT'], w_=[pk])
        return ps, pk

    def rmsnorm(gname, t0, n):
        ps, pk = PS()
        for c in range(8):
            sq, sk = SB()
            op('act', I('activation', sq[:, 0:n], xT[:, c, t0:t0 + n], AF.Square), r_=[('x', c)], w_=[sk])
            op('pe', I('matmul', ps[:, 0:n], ones[:, :], sq[:, 0:n], start=(c == 0), stop=(c == 7)), r_=[sk, 'ones'], w_=[pk])
        rs, rk = SF()
        op('act', I('activation', rs[:, 0:n], ps[:, 0:n], AF.Ln, bias=epsc[:, 0:1], scale=1.0 / D), r_=[pk, 'epsc'], w_=[rk])
        op('act', I('activation', rs[:, 0:n], rs[:, 0:n], AF.Exp, scale=-0.5), r_=[rk], w_=[rk])
        for c in range(8):
            op('dve', I('scalar_tensor_tensor', hT[:, c, PAD + t0:PAD + t0 + n], xT[:, c, t0:t0 + n], pc(gname, c),
                        rs[:, 0:n], ALU.mult, ALU.mult), r_=[('x', c), rk, 'pp'], w_=['hT'])

    def wout_apply(b):
        wos = W['wos']
        for oc in range(8):
            ps, pk = PS()
            op('pe', *[I('matmul', ps[:, :], wos[0:64, h, oc * 128:(oc + 1) * 128], MX[0:64, h, :],
                         start=(h == 0), stop=(h == 3)) for h in range(4)], r_=['wos', 'MX'], w_=[pk])
            op('dve', I('tensor_tensor', xT[:, oc, b * 512:(b + 1) * 512], ps[:, :], xT[:, oc, b * 512:(b + 1) * 512],
                        ALU.add), r_=[pk, ('x', oc)], w_=[('x', oc)])

    def rstd_from(ps, pk, rows, n, bias_ap, scale=1.0, ebias=None, r0=0):
        rs, rk = SF()
        kw = {} if bias_ap is None else dict(bias=bias_ap)
        op('act', I('activation', rs[r0:r0 + rows, 0:n], ps[r0:r0 + rows, 0:n], AF.Ln, scale=scale, **kw),
           r_=[pk, 'epsc'], w_=[rk])
        kw = {} if ebias is None else dict(bias=ebias)
        op('act', I('activation', rs[r0:r0 + rows, 0:n], rs[r0:r0 + rows, 0:n], AF.Exp, scale=-0.5, **kw),
           r_=[rk, 'der'], w_=[rk])
        return rs, rk

    def outnorm_gate(l, b, gname, zname, OT):
        def on_head(h):
            sq, sk = SB()
            op('act', I('activation', sq[0:64, :], OT[0:64, h, :], AF.Square), r_=['OT'], w_=[sk])
            p2, pk2 = PS()
            op('pe', I('matmul', p2[0:64, :], o64[:, :], sq[0:64, :], start=True, stop=True), r_=[sk, 'o64'], w_=[pk2])
            rs, rk = rstd_from(p2, pk2, 64, 512, epsc[0:64, 0:1])
            zp, zk = proj_f(l, '%s%d' % (zname, h), 64, PAD + b * 512, 512)
            gt, gk = SF()
            op('act', I('activation', gt[0:64, :], zp[0:64, :], AF.Silu), r_=[zk], w_=[gk])
            t, tk = SF()
            op('dve', I('scalar_tensor_tensor', t[0:64, :], OT[0:64, h, :], pc(gname, 0, 0, 64), rs[0:64, :],
                        ALU.mult, ALU.mult), r_=['OT', rk, 'pp'], w_=[tk])
            op('dve', I('tensor_tensor', MX[0:64, h, :], t[0:64, :], gt[0:64, :], ALU.mult), r_=[tk, gk], w_=['MX'])
        two_streams(lambda: [on_head(h) for h in (0, 1)], lambda: [on_head(h) for h in (2, 3)])

    def fox(l):
        NT = S // 128
        KA = kb.sb("KA", [96, 4, S], BF16)
        QA = kb.sb("QA", [96, 4, 512], BF16)
        Vp = kb.sb("Vp", [128, NT, 4, 96], BF16)
        CC = kb.sb("CC", [68, 4, 2], F32)
        wits = W['wits']
        op('pool', I('memset', Vp[:, :, :, 64:96], 0.0), w_=['Vp1'])
        op('pool', I('memset', Vp[:, :, :, 64:65], 1.0), w_=['Vp1'])
        op('pool', I('memset', KA[64:96, :, :], 0.0), w_=['KA0'] + [('KA', h_, b_) for h_ in range(4) for b_ in range(NB)])
        op('pool', I('memset', QA[64:96, :, :], 0.0), w_=['QA0'] + [('QA', h_) for h_ in range(4)])
        kb.dma('sp', wits[:], wb['wit'][l, :, :, 0:256], r_=[('wit', l)], w_=['wits'])
        kb.dma('sp', W['wos'][:], wb['wo'][l, 0], r_=[('wo', l)], w_=['wos'])
        for i in range(NT):
            ps, pk = PS()
            op('pe', *[I('matmul', ps[:, 0:256], hT[:, kc, PAD + i * 128:PAD + (i + 1) * 128], wits[:, kc, 0:256],
                         start=(kc == 0), stop=(kc == 7)) for kc in range(8)], r_=['hT', 'wits'], w_=[pk])
            op('act', I('activation', Vp[:, i, :, 0:64], v3(ps[:, 0:256], 64), AF.Copy), r_=[pk], w_=[('Vp', i)])
        for b in range(NB):
            def fox_proj(h):
                for which in ('k', 'q'):
                    ps, pk = proj_f(l, 'f%s%d' % (which, h), 68, PAD + b * 512, 512)
                    sq, sk = SB()
                    op('act', I('activation', sq[0:64, :], ps[0:64, :], AF.Square), r_=[pk], w_=[sk])
                    p2, pk2 = PS()
                    op('pe', I('matmul', p2[0:64, :], o64[:, :], sq[0:64, :], start=True, stop=True), r_=[sk, 'o64'], w_=[pk2])
                    rs, rk = rstd_from(p2, pk2, 64, 512, epsc[0:64, 0:1],
                                       ebias=(der[0:64, 12:13] if which == 'q' else None))
                    dst = QA[0:64, h, :] if which == 'q' else KA[0:64, h, b * 512:(b + 1) * 512]
                    dk = ('QA', h) if which == 'q' else ('KA', h, b)
                    op('dve', I('scalar_tensor_tensor', dst, ps[0:64, :], pc('fqg' if which == 'q' else 'fkg', 0, 0, 64),
                                rs[0:64, :], ALU.mult, ALU.mult), r_=[pk, rk, 'pp'], w_=[dk])
                    if which == 'k':
                        continue
                    t1, tk1 = SF()
                    op('act', I('activation', t1[64:68, :], ps[64:68, :], AF.Exp, bias=der[64:68, 8 + h:9 + h], scale=-1.0),
                       r_=[pk, 'der'], w_=[tk1])
                    op('act', I('activation', t1[64:68, :], t1[64:68, :], AF.Ln, bias=der[64:68, 13:14]), r_=[tk1, 'der'], w_=[tk1])
                    c, ck = SF()
                    init = 0.0 if b == 0 else CC[64:68, h, 0:1]
                    op('dve', I('tensor_tensor_scan', c[64:68, :], onesf[64:68, :], t1[64:68, :], init, ALU.mult, ALU.subtract),
                       r_=[tk1, 'onesf', ('CC', h)], w_=[ck])
                    op('dve', I('tensor_copy', CC[64:68, h, 0:1], c[64:68, 511:512]), r_=[ck], w_=[('CC', h)])
                    H, hk = SB()
                    M_, mk = SB()
                    r1, rk1 = SF()
                    op('dve', I('tensor_copy', H[64:68, :], c[64:68, :]), r_=[ck], w_=[hk])
                    op('dve', I('tensor_tensor', r1[64:68, :], c[64:68, :], H[64:68, :], ALU.subtract), r_=[ck, hk], w_=[rk1])
                    op('dve', I('tensor_copy', M_[64:68, :], r1[64:68, :]), r_=[rk1], w_=[mk])
                    for (dst2, dk2, a0) in ((QA[64:68, h, :], ('QA', h), 0), (KA[64:68, h, b * 512:(b + 1) * 512], ('KA', h, b), 3)):
                        t2, tk2 = SF()
                        op('dve', I('tensor_scalar', t2[64:68, :], H[64:68, :], aug[64:68, a0:a0 + 1], None, ALU.mult),
                           r_=[hk, 'aug'], w_=[tk2])
                        op('dve', I('scalar_tensor_tensor', t2[64:68, :], M_[64:68, :], aug[64:68, a0 + 1:a0 + 2], t2[64:68, :],
                                    ALU.mult, ALU.add), r_=[mk, tk2, 'aug'], w_=[tk2])
                        op('dve', I('tensor_scalar', dst2, t2[64:68, :], aug[64:68, a0 + 2:a0 + 3], None, ALU.add),
                           r_=[tk2, 'aug'], w_=[dk2])
            two_streams(lambda: [fox_proj(h) for h in (0, 1)], lambda: [fox_proj(h) for h in (2, 3)])
            if os.environ.get('FOXSTOP') == '1':
                continue
            FS = int(os.environ.get('FOXSTOP', '9'))
            def fox_attn(h):
                po, pok = PSA()
                nk = 4 * b + 4
                def ST(i):
                    m = i - 4 * b
                    c0 = 128 * m if m > 0 else 0
                    n = 512 - c0
                    ps, pk = PS()
                    op('pe', I('matmul', ps[:, 0:n], KA[0:96, h, i * 128:(i + 1) * 128], QA[0:96, h, c0:512], start=True, stop=True),
                       r_=[('KA', h, i // 4), ('QA', h), 'KA0', 'QA0'], w_=[pk])
                    return ps, pk, m, c0, n
                cur = ST(0)
                for i in range(nk):
                    nxt = ST(i + 1) if i + 1 < nk else None
                    ps, pk, m, c0, n = cur
                    cur = nxt
                    pt, ptk = SB()
                    if m >= 0:
                        op('dve', I('tensor_scalar', ps[:, 0:128], ps[:, 0:128], 40.0, None, ALU.min), r_=[pk], w_=[pk])
                    op('act', I('activation', pt[:, 0:n], ps[:, 0:n], AF.Exp), r_=[pk], w_=[ptk])
                    if m >= 0 and FS >= 4:
                        op('dve', I('tensor_tensor', pt[:, 0:128], pt[:, 0:128], tri[:, :], ALU.mult), r_=[ptk, 'tri'], w_=[ptk])
                    if FS >= 5:
                        op('pe', I('matmul', po[0:96, c0:512], Vp[:, i, h, 0:96], pt[:, 0:n], start=(i == 0), stop=(i == nk - 1)),
                           r_=[ptk, ('Vp', i), 'Vp1'], w_=[pok])
                if FS < 6:
                    return
                sq, sk = SB()
                of, ok_ = SF()
                op('act', I('activation', sq[0:96, :], po[0:96, :], AF.Square), r_=[pok], w_=[sk])
                op('dve', I('tensor_copy', of[0:64, :], po[0:64, :]), r_=[pok, sk], w_=[ok_])
                if FS < 7:
                    return
                p2, pk2 = PS()
                op('pe', I('matmul', p2[0:64, :], wst[0:96, :], sq[0:96, :], start=True, stop=True), r_=[sk, 'wst'], w_=[pk2])
                rs, rk = rstd_from(p2, pk2, 64, 512, None)
                op('dve', I('scalar_tensor_tensor', MX[0:64, h, :], of[0:64, :], pc('fog', 0, 0, 64), rs[0:64, :],
                            ALU.mult, ALU.mult), r_=[ok_, rk, 'pp'], w_=['MX'])
            two_streams(lambda: [fox_attn(h) for h in (0, 1)], lambda: [fox_attn(h) for h in (2, 3)])
            if os.environ.get('FOXSTOP') != '2':
                wout_apply(b)

    def recurrent(l, kind):
        m = {'gdn': 1, 'hg': 2, 'gla': 3}[kind]
        gdn = kind == 'gdn'
        S32 = kb.sb("S32", [128, 2, 64], F32)
        Sb = kb.sb("Sb", [128, 2, 64], BF16)
        EL = kb.sb("EL", [128, 2, 8, 1], F32)
        OT = kb.sb("OT", [64, 4, 512], BF16)
        qT = [kb.sb("qT%d" % g, [128, 512], BF16) for g in range(2)]
        kd = [kb.sb("kd%d" % g, [128, 512], BF16) for g in range(2)]
        ko = [kb.sb("ko%d" % g, [128, 512], BF16) for g in range(2)]
        if gdn:
            qg = [kb.sb("qg%d" % g, [128, 512], BF16) for g in range(2)]
            kbt = [kb.sb("kbt%d" % g, [128, 512], BF16) for g in range(2)]
            Zl = [kb.sb("Zl%d" % g, [128, 512], F32) for g in range(2)]
            Zr = [kb.sb("Zr%d" % g, [128, 512], F32) for g in range(2)]
            kw = [kb.sb("kw%d" % g, [128, 512], BF16) for g in range(2)]
            vb = [kb.sb("vb%d" % g, [128, 512], BF16) for g in range(2)]
            RAW = [[kb.sb("raw%d%d" % (t, g), [128, 516], BF16) for g in range(2)] for t in range(3)]
        else:
            qg = [kb.sb("qg%d" % g, [128, 512], BF16) for g in range(2)]
            gkT = kb.sb("gkT", [16, 512], BF16)
        wits, wgks = W['wits'], W['wgks']
        AQs2 = [kb.sb("AQs%d" % i, [64, 512], BF16) for i in range(2)]
        KTs2 = [kb.sb("KTs%d" % i, [64, 512], BF16) for i in range(2)]
        VTs2 = [kb.sb("VTs%d" % i, [64, 512], BF16) for i in range(2)] if not gdn else [None, None]
        WTs2 = [kb.sb("WTs%d" % i, [128, 256], BF16) for i in range(2)]
        Us2 = [kb.sb("Us%d" % i, [64, 512], F32) for i in range(2)] if gdn else None
        Sb2 = [Sb, kb.sb("Sb1", [128, 2, 64], BF16)]
        cst = {'n': 0}
        op('pool', I('memset', S32[:], 0.0), w_=['S32'])
        op('pool', I('memset', Sb2[0][:], 0.0), w_=[('Sb', 0)])
        op('pool', I('memset', Sb2[1][:], 0.0), w_=[('Sb', 1)])
        kb.dma('sp', W['wos'][:], wb['wo'][l, m], r_=[('wo', l)], w_=['wos'])
        if not gdn:
            c0w = 256 if kind == 'hg' else 512
            kb.dma('sp', wits[:], wb['wit'][l, :, :, c0w:c0w + 256], r_=[('wit', l)], w_=['wits'])
        if kind == 'gla':
            kb.dma('sp', wgks[:], wb['wgk'][l], r_=[('wgk', l)], w_=['wgks'])

        def decay_tiles(tg, tgk, sc):
            tG, tGk = SF()
            op('dve', I('tensor_tensor_scan', tG[:, :], cm[:, :], tg[:, :], 0.0, ALU.mult, ALU.add), r_=[tgk, 'cm'], w_=[tGk])
            tm, tmk = SF()
            op('dve', I('tensor_tensor', v3(tm[:, :], 64), v3(tG[:, :], 64), bcl(v3(tG[:, :], 64)[:, :, 31:32], 64), ALU.subtract),
               r_=[tGk], w_=[tmk])
            op('act', I('activation', tg[:, :], tG[:, :], AF.Exp, scale=sc), r_=[tGk], w_=[tgk])
            op('act', I('activation', tG[:, :], tm[:, :], AF.Exp, scale=sc), r_=[tmk], w_=[tGk])
            op('act', I('activation', tm[:, :], tm[:, :], AF.Exp, scale=-sc), r_=[tmk], w_=[tmk])
            return (tg, tgk), (tG, tGk), (tm, tmk)

        def pre_la(b, g):
            c0 = PAD + b * 512
            if kind == 'hg':
                ps, pk = proj_f(l, 'hf%d' % g, 128, c0, 512)
                tk_, tkk = SF()
                op('act', I('activation', tk_[:, :], ps[:, :], AF.Exp), r_=[pk], w_=[tkk])
                op('dve', I('tensor_scalar', tk_[:, :], tk_[:, :], 1.0, None, ALU.add), r_=[tkk], w_=[tkk])
                op('dve', I('reciprocal', tk_[:, :], tk_[:, :]), r_=[tkk], w_=[tkk])
                op('dve', I('tensor_scalar', tk_[:, :], tk_[:, :], der[:, 2 + g:3 + g], None, ALU.mult), r_=[tkk, 'der'], w_=[tkk])
                tg, tgk = SF()
                op('act', I('activation', tg[:, :], tk_[:, :], AF.Ln, bias=der[:, 13:14], scale=-1.0), r_=[tkk, 'der'], w_=[tgk])
                sc, qs = 1.0, 0.125
            else:
                ps, pk = PS()
                op('pe', I('matmul', ps[:, :], wgks[0:16, g * 128:(g + 1) * 128], gkT[0:16, :], start=True, stop=True),
                   r_=['wgks', 'gkT'], w_=[pk])
                tg, tgk = SF()
                op('act', I('activation', tg[:, :], ps[:, :], AF.Exp, bias=der[:, 4 + g:5 + g], scale=-1.0), r_=[pk, 'der'], w_=[tgk])
                op('act', I('activation', tg[:, :], tg[:, :], AF.Ln, bias=der[:, 13:14]), r_=[tgk, 'der'], w_=[tgk])
                sc, qs = -1.0 / 16.0, float(32.0 ** -0.5)
            (E1, e1k), (E2, e2k), (E3, e3k) = decay_tiles(tg, tgk, sc)
            op('dve', I('tensor_copy', EL[:, g, :, :], v3(E1[:, :], 64)[:, :, 63:64]), r_=[e1k], w_=['EL'])
            if kind == 'hg':
                ps, pk = proj_f(l, 'hq%d' % g, 128, c0, 512)
                tq, tqk = SF()
                op('act', I('activation', tq[:, :], ps[:, :], AF.Silu), r_=[pk], w_=[tqk])
                qsrc, qk_ = tq, tqk
                op('dve', I('tensor_tensor', kd[g][:, :], tk_[:, :], E3[:, :], ALU.mult), r_=[tkk, e3k], w_=[('kd', g)])
            else:
                ps, pk = proj_f(l, 'lq%d' % g, 128, c0, 512)
                qsrc, qk_ = ps, pk
            op('dve', I('scalar_tensor_tensor', qT[g][:, :], qsrc[:, :], qs, E1[:, :], ALU.mult, ALU.mult), r_=[qk_, e1k], w_=[('qT', g)])
            op('dve', I('scalar_tensor_tensor', qg[g][:, :], qsrc[:, :], qs, E2[:, :], ALU.mult, ALU.mult), r_=[qk_, e2k], w_=[('qg', g)])
            if kind == 'gla':
                ps, pk = proj_f(l, 'lk%d' % g, 128, c0, 512)
                op('dve', I('tensor_tensor', kd[g][:, :], ps[:, :], E3[:, :], ALU.mult), r_=[pk, e3k], w_=[('kd', g)])
            op('dve', I('tensor_tensor', v3(ko[g][:, :], 64), v3(kd[g][:, :], 64), bcl(v3(E2[:, :], 64)[:, :, 63:64], 64), ALU.mult),
               r_=[('kd', g), e2k], w_=[('ko', g)])

        def gk_common(b):
            c0 = PAD + b * 512
            ps, pk = proj_f(l, 'lgk', 16, c0, 512)
            op('act', I('activation', gkT[0:16, :], ps[0:16, :], AF.Copy), r_=[pk], w_=['gkT'])

        def pre_gdn(b, g):
            c0 = PAD + b * 512
            ps, pk = proj_f(l, 'ga%d' % g, 128, c0, 512)
            ta, tak = SF()
            op('act', I('activation', ta[:, :], ps[:, :], AF.Exp, bias=pc('gdt', g)), r_=[pk, 'pp'], w_=[tak])
            op('act', I('activation', ta[:, :], ta[:, :], AF.Ln, bias=der[:, 13:14]), r_=[tak, 'der'], w_=[tak])
            op('dve', I('tensor_scalar', ta[:, :], ta[:, :], der[:, g:g + 1], None, ALU.mult), r_=[tak, 'der'], w_=[tak])
            tG, tGk = SF()
            op('dve', I('tensor_tensor_scan', tG[:, :], cm[:, :], ta[:, :], 0.0, ALU.mult, ALU.add), r_=[tak, 'cm'], w_=[tGk])
            tn, tnk = SF()
            op('dve', I('tensor_scalar', Zl[g][:, :], tG[:, :], zc[:, 0:1], zc[:, 1:2], ALU.mult, ALU.add), r_=[tGk, 'zc'], w_=[('Zl', g)])
            op('dve', I('tensor_scalar', Zr[g][:, :], tG[:, :], zc[:, 2:3], zc[:, 3:4], ALU.mult, ALU.add), r_=[tGk, 'zc'], w_=[('Zr', g)])
            op('act', I('activation', ta[:, :], tG[:, :], AF.Exp), r_=[tGk], w_=[tak])
            op('dve', I('tensor_tensor', v3(tn[:, :], 64), bcl(v3(tG[:, :], 64)[:, :, 63:64], 64), v3(tG[:, :], 64), ALU.subtract),
               r_=[tGk], w_=[tnk])
            op('act', I('activation', tn[:, :], tn[:, :], AF.Exp), r_=[tnk], w_=[tnk])
            op('dve', I('tensor_copy', EL[:, g, :, :], v3(ta[:, :], 64)[:, :, 63:64]), r_=[tak], w_=['EL'])
            ps, pk = proj_f(l, 'gb%d' % g, 128, c0, 512)
            tu, tuk = SF()
            op('act', I('activation', tu[:, :], ps[:, :], AF.Exp, scale=-1.0), r_=[pk], w_=[tuk])
            op('act', I('activation', tu[:, :], tu[:, :], AF.Ln, bias=der[:, 13:14]), r_=[tuk, 'der'], w_=[tuk])
            op('dve', I('tensor_tensor', tG[:, :], tG[:, :], tu[:, :], ALU.subtract), r_=[tGk, tuk], w_=[tGk])
            op('act', I('activation', tG[:, :], tG[:, :], AF.Exp), r_=[tGk], w_=[tGk])
            op('act', I('activation', tu[:, :], tu[:, :], AF.Exp, scale=-1.0), r_=[tuk], w_=[tuk])

            def conv(ti):
                raw = RAW[ti][g]
                rk_ = ('raw', ti, g)
                if b == 0:
                    op('pool', I('memset', raw[:, 0:4], 0.0), w_=[rk_])
                else:
                    op('pool', I('tensor_copy', raw[:, 1:4], raw[:, 513:516]), r_=[rk_], w_=[rk_])
                ps, pk = proj_f(l, ('gq', 'gk', 'gv')[ti] + str(g), 128, c0, 512)
                op('act', I('activation', raw[:, 4:516], ps[:, :], AF.Copy), r_=[pk], w_=[rk_])
                y, yk = SF()
                ci = ti * 2 + g
                op('dve', I('tensor_scalar', y[:, :], raw[:, 4:516], pc('gcw', ci * 4 + 3), None, ALU.mult), r_=[rk_, 'pp'], w_=[yk])
                for j in (2, 1, 0):
                    op('dve', I('scalar_tensor_tensor', y[:, :], raw[:, 1 + j:513 + j], pc('gcw', ci * 4 + j), y[:, :],
                                ALU.mult, ALU.add), r_=[rk_, yk, 'pp'], w_=[yk])
                op('act', I('activation', y[:, :], y[:, :], AF.Silu), r_=[yk], w_=[yk])
                return y, yk

            def l2n(y, yk, ebias):
                sq, sk = SB()
                op('act', I('activation', sq[:, :], y[:, :], AF.Square), r_=[yk], w_=[sk])
                p2, pk2 = PS()
                op('pe', I('matmul', p2[:, :], bd[:, :], sq[:, :], start=True, stop=True), r_=[sk, 'bd'], w_=[pk2])
                rs, rk = SB()
                op('act', I('activation', rs[:, :], p2[:, :], AF.Ln, bias=epsc[:, 0:1]), r_=[pk2, 'epsc'], w_=[rk])
                kw_ = {} if ebias is None else dict(bias=ebias)
                op('act', I('activation', rs[:, :], rs[:, :], AF.Exp, scale=-0.5, **kw_), r_=[rk, 'der'], w_=[rk])
                op('dve', I('tensor_tensor', y[:, :], y[:, :], rs[:, :], ALU.mult), r_=[yk, rk], w_=[yk])

            y, yk = conv(0)
            l2n(y, yk, der[:, 12:13])
            op('dve', I('tensor_tensor', qT[g][:, :], y[:, :], ta[:, :], ALU.mult), r_=[yk, tak], w_=[('qT', g)])
            op('pool', I('tensor_copy', qg[g][:, :], y[:, :]), r_=[yk], w_=[('qg', g)])
            y, yk = conv(1)
            l2n(y, yk, None)
            op('dve', I('tensor_tensor', kw[g][:, :], y[:, :], tG[:, :], ALU.mult), r_=[yk, tGk], w_=[('kw', g)])
            op('pool', I('tensor_copy', kd[g][:, :], y[:, :]), r_=[yk], w_=[('kd', g)])
            op('dve', I('tensor_tensor', kbt[g][:, :], y[:, :], tu[:, :], ALU.mult), r_=[yk, tuk], w_=[('kbt', g)])
            op('dve', I('tensor_tensor', ko[g][:, :], y[:, :], tn[:, :], ALU.mult), r_=[yk, tnk], w_=[('ko', g)])
            y, yk = conv(2)
            op('dve', I('tensor_tensor', vb[g][:, :], y[:, :], tu[:, :], ALU.mult), r_=[yk, tuk], w_=[('vb', g)])

        def hgj(j, h):
            return (j * 4 + h) * 64, h // 2, (h % 2) * 64

        def mm8(ps, pk, lhs, rhs, r_, split=False):
            groups = [(0, 2), (1, 3)] if split else [(0, 1, 2, 3)]
            for hs in groups:
                op('pe', *[I('matmul', ps[0:64, hgj(j, h)[0]:hgj(j, h)[0] + 64], lhs(j, h), rhs(j, h), start=True, stop=True)
                           for j in range(2) for h in hs], r_=r_, w_=[pk])

        def fm(tiles, sc):
            return lambda j, h: tiles[h // 2][(h % 2) * 64:(h % 2) * 64 + 64, (2 * sc + j) * 64:(2 * sc + j + 1) * 64]

        def tm(tile):
            return lambda j, h: tile[0:64, (j * 4 + h) * 64:(j * 4 + h + 1) * 64]

        def transp8(src, sc, r_, dst=None):
            pt, ptk = PT()
            for hs in ((0, 2), (1, 3)):
                op('pe', *[I('transpose', pt[0:64, hgj(j, h)[0]:hgj(j, h)[0] + 64], fm(src, sc)(j, h),
                             ident[hgj(j, h)[2]:hgj(j, h)[2] + 64, hgj(j, h)[2]:hgj(j, h)[2] + 64])
                           for j in range(2) for h in hs], r_=r_ + ['ident'], w_=[ptk])
            t, tk = dst if dst is not None else SB()
            op('act', I('activation', t[0:64, :], pt[0:64, :], AF.Copy), r_=[ptk], w_=[tk])
            return t, tk

        def A_phase(b, sc):
            res = {}
            sl = sc % 2
            AQs, KTs, VTs, WTs = AQs2[sl], KTs2[sl], VTs2[sl], WTs2[sl]
            if gdn:
                zr_ = [('Zl', 0), ('Zl', 1), ('Zr', 0), ('Zr', 1)]
                zf = lambda tiles: (lambda j, h: tiles[h // 2][(h % 2) * 64:(h % 2) * 64 + 2, (2 * sc + j) * 64:(2 * sc + j + 1) * 64])
                psD, pkD = PS()
                mm8(psD, pkD, zf(Zr), zf(Zl), zr_, split=True)
                psDT, pkDT = PS()
                mm8(psDT, pkDT, zf(Zl), zf(Zr), zr_, split=True)
                Dm, dmk = SF()
                DT, dtk = SF()
                DTu, dtuk = SF()
                op('dve', I('tensor_scalar', Dm[0:64, :], psD[0:64, :], 0.0, None, ALU.min), r_=[pkD], w_=[dmk])
                op('act', I('activation', Dm[0:64, :], Dm[0:64, :], AF.Exp), r_=[dmk], w_=[dmk])
                op('pool', I('tensor_tensor', v3(Dm[0:64, :], 64), v3(Dm[0:64, :], 64), bcm(negL[:, :], 8), ALU.mult), r_=[dmk, 'negL'], w_=[dmk])
                op('dve', I('tensor_scalar', DT[0:64, :], psDT[0:64, :], 0.0, None, ALU.min), r_=[pkDT], w_=[dtk])
                op('act', I('activation', DT[0:64, :], DT[0:64, :], AF.Exp), r_=[dtk], w_=[dtk])
                op('pool', I('tensor_tensor', v3(DTu[0:64, :], 64), v3(DT[0:64, :], 64), bcm(negU[:, :], 8), ALU.mult), r_=[dtk, 'negU'], w_=[dtuk])
                op('pool', I('tensor_tensor', v3(DT[0:64, :], 64), v3(DT[0:64, :], 64), bcm(tri[0:64, 0:64], 8), ALU.mult), r_=[dtk, dtuk, 'tri'], w_=[dtk])
            ps, pk = PS()
            mm8(ps, pk, fm(kd, sc), fm(qg, sc), [('kd', 0), ('kd', 1), ('qg', 0), ('qg', 1), ('qT', 0), ('qT', 1)], split=True)
            AQ, aqk = AQs, ('AQs', sl)
            if gdn:
                op('dve', I('tensor_tensor', AQ[0:64, :], ps[0:64, :], DT[0:64, :], ALU.mult), r_=[pk, dtk], w_=[aqk])
            else:
                op('dve', I('tensor_tensor', v3(AQ[0:64, :], 64), v3(ps[0:64, :], 64), bcm(tri[0:64, 0:64], 8), ALU.mult),
                   r_=[pk, 'tri'], w_=[aqk])
            res['AQ'] = (AQ, aqk)
            AS = int(os.environ.get('ASTOP', '9'))
            if AS < 2:
                return res
            res['KT'] = transp8(ko, sc, [('ko', 0), ('ko', 1)], dst=(KTs, ('KTs', sl)))
            if AS < 3:
                return res
            if not gdn:
                VT, vtk = VTs, ('VTs', sl)
                for j in range(2):
                    t0 = PAD + b * 512 + (2 * sc + j) * 64
                    ps, pk = PS()
                    op('pe', *[I('matmul', ps[0:64, 0:256], hT[:, kc, t0:t0 + 64], wits[:, kc, 0:256], start=(kc == 0), stop=(kc == 7))
                               for kc in range(8)], r_=['hT', 'wits'], w_=[pk])
                    op('act', I('activation', VT[0:64, j * 256:(j + 1) * 256], ps[0:64, 0:256], AF.Copy), r_=[pk], w_=[vtk])
                res['VT'] = (VT, vtk)
                return res
            kwr = [('kbt', 0), ('kbt', 1), ('kd', 0), ('kd', 1)]
            psN, pkN = PS()
            mm8(psN, pkN, fm(kbt, sc), fm(kd, sc), kwr, split=True)
            psA, pkA = PS()
            mm8(psA, pkA, fm(kd, sc), fm(kbt, sc), kwr, split=True)
            X, xk = SB()
            A_, ak = SB()
            P_, pk_ = SB()
            op('dve', I('tensor_tensor', X[0:64, :], psN[0:64, :], Dm[0:64, :], ALU.mult), r_=[pkN, dmk], w_=[xk])
            op('dve', I('tensor_tensor', A_[0:64, :], psA[0:64, :], DTu[0:64, :], ALU.mult), r_=[pkA, dtuk], w_=[ak])
            op('pool', I('tensor_tensor', v3(P_[0:64, :], 64), v3(A_[0:64, :], 64), bcm(eye[:, :], 8), ALU.add), r_=[ak, 'eye'], w_=[pk_])
            for lev in range(1, 6):
                psX, pkX = PS()
                mm8(psX, pkX, tm(A_), tm(X), [ak, xk])
                Xn, xnk = SB()
                op('act', I('activation', Xn[0:64, :], psX[0:64, :], AF.Copy), r_=[pkX], w_=[xnk])
                if lev < 5:
                    psA2, pkA2 = PS()
                    mm8(psA2, pkA2, tm(X), tm(A_), [ak, xk])
                    An, ank = SB()
                    op('dve', I('tensor_copy', An[0:64, :], psA2[0:64, :]), r_=[pkA2], w_=[ank])
                psP, pkP = PS()
                mm8(psP, pkP, tm(Xn), tm(P_), [xnk, pk_])
                Pn, pnk = SB()
                op('dve', I('tensor_tensor', Pn[0:64, :], psP[0:64, :], P_[0:64, :], ALU.add), r_=[pkP, pk_], w_=[pnk])
                X, xk = Xn, xnk
                if lev < 5:
                    A_, ak = An, ank
                P_, pk_ = Pn, pnk
            KW, kwk = transp8(kw, sc, [('kw', 0), ('kw', 1)])
            VB, vbk = transp8(vb, sc, [('vb', 0), ('vb', 1)])
            psW, pkW = PS()
            op('pe', *[I('matmul', psW[hgj(j, h)[2]:hgj(j, h)[2] + 64, (j * 2 + h // 2) * 64:(j * 2 + h // 2 + 1) * 64],
                         tm(KW)(j, h), tm(P_)(j, h), start=True, stop=True) for j in range(2) for h in range(4)],
               r_=[kwk, pk_], w_=[pkW])
            WT, wtk = WTs, ('WTs', sl)
            op('act', I('activation', WT[:, 0:256], psW[:, 0:256], AF.Copy), r_=[pkW], w_=[wtk])
            psU, pkU = PS()
            mm8(psU, pkU, tm(P_), tm(VB), [pk_, vbk])
            U, uk = Us2[sl], ('Us', sl)
            op('dve', I('tensor_copy', U[0:64, :], psU[0:64, :]), r_=[pkU], w_=[uk])
            res['WT'] = (WT, wtk)
            res['U'] = (U, uk)
            return res

        def B_phase(b, sc, res):
            AQ, aqk = res['AQ']
            KT, ktk = res['KT']
            for j in range(2):
                n = 2 * sc + j
                cs = n * 64
                cur = cst['n'] % 2
                cst['n'] += 1
                Sc, Sn = Sb2[cur], Sb2[1 - cur]
                sck, snk = ('Sb', cur), ('Sb', 1 - cur)
                if gdn:
                    WT, wtk = res['WT']
                    U, uk = res['U']
                    ps, pk = PS()
                    for hs in ((0, 2), (1, 3)):
                        op('pe', *[I('matmul', ps[0:64, h * 64:(h + 1) * 64],
                                     WT[(h % 2) * 64:(h % 2) * 64 + 64, (j * 2 + h // 2) * 64:(j * 2 + h // 2 + 1) * 64],
                                     Sc[(h % 2) * 64:(h % 2) * 64 + 64, h // 2, :], start=True, stop=True) for h in hs],
                           r_=[wtk, sck], w_=[pk])
                    VT, vtk = SB()
                    op('dve', I('tensor_tensor', VT[0:64, 0:256], U[0:64, j * 256:(j + 1) * 256], ps[0:64, 0:256], ALU.subtract),
                       r_=[uk, pk], w_=[vtk])
                    voff = 0
                else:
                    VT, vtk = res['VT']
                    voff = j * 256
                pu, puk = PS()
                op('pe', *[I('matmul', pu[(h % 2) * 64:(h % 2) * 64 + 64, (h // 2) * 64:(h // 2 + 1) * 64],
                             KT[0:64, (j * 4 + h) * 64:(j * 4 + h + 1) * 64], VT[0:64, voff + h * 64:voff + (h + 1) * 64],
                             start=True, stop=True) for h in range(4)], r_=[ktk, vtk], w_=[puk])
                for g_ in range(2):
                    op('dve', I('scalar_tensor_tensor', Sn[:, g_, :], S32[:, g_, :], EL[:, g_, n, :], pu[:, g_ * 64:(g_ + 1) * 64],
                                ALU.mult, ALU.add), r_=['S32', 'EL', puk], w_=[snk])
                for g_ in range(2):
                    op('dve', I('scalar_tensor_tensor', S32[:, g_, :], S32[:, g_, :], EL[:, g_, n, :], pu[:, g_ * 64:(g_ + 1) * 64],
                                ALU.mult, ALU.add), r_=['S32', 'EL', puk], w_=['S32'])
                po, pok = PS()

                def mS(h):
                    r0 = (h % 2) * 64
                    return I('matmul', po[0:64, h * 64:(h + 1) * 64], Sc[r0:r0 + 64, h // 2, :], qT[h // 2][r0:r0 + 64, cs:cs + 64],
                             start=True, stop=False)

                def mV(h):
                    return I('matmul', po[0:64, h * 64:(h + 1) * 64], VT[0:64, voff + h * 64:voff + (h + 1) * 64],
                             AQ[0:64, (j * 4 + h) * 64:(j * 4 + h + 1) * 64], start=False, stop=True)
                rr = [sck, ('qT', 0), ('qT', 1), vtk, aqk]
                op('pe', mS(0), mV(0), mS(2), mV(2), r_=rr, w_=[pok])
                for h_ in (1, 3):
                    op('pe', mS(h_), r_=rr, w_=[pok])
                    op('pe', mV(h_), r_=rr, w_=[pok])
                op('act', I('activation', OT[0:64, :, cs:cs + 64], v3(po[0:64, 0:256], 64), AF.Copy), r_=[pok], w_=['OT'])

        RS = int(os.environ.get('RSTOP', '9'))
        for b in range(NB):
            kb.label = 'pre'
            if True:
                if kind == 'gla':
                    gk_common(b)
                caps = []
                for g in range(2):
                    STREAM['s'] = g
                    caps.append(kb.capture(lambda: (pre_gdn if gdn else pre_la)(b, g)))
                STREAM['s'] = None
                kb.commit_interleaved(caps)
            if RS < 2:
                continue
            for sc0 in (0, 2):
                caps, ress = [], []
                for i_ in range(2):
                    STREAM['s'] = i_
                    kb.label = 'A%d' % (sc0 + i_)
                    caps.append(kb.capture(lambda: ress.append(A_phase(b, sc0 + i_))))
                STREAM['s'] = None
                kb.commit_interleaved(caps)
                if RS >= 3:
                    for i_ in range(2):
                        kb.label = 'B%d' % (sc0 + i_)
                        B_phase(b, sc0 + i_, ress[i_])
            kb.label = 'out'
            if RS < 4:
                continue
            outnorm_gate(l, b, {'gdn': 'gog', 'hg': 'hog', 'gla': 'log'}[kind], {'gdn': 'gz', 'hg': 'hg', 'gla': 'lg'}[kind], OT)
            wout_apply(b)

    def ffn(l):
        GT = kb.sb("GT", [128, 22, 1024], BF16)
        wu = [kb.sb("wu%d" % i, [128, 8, 128], BF16) for i in range(4)]
        wd = [kb.sb("wd%d" % i, [128, 22, 128], BF16) for i in range(2)]
        passes = []
        t = 0
        while t < S:
            e = min(S, t + 1024)
            passes.append((t, e))
            t = e
        wi = 0
        for (p0, p1) in passes:
            blocks = []
            t = p0
            while t < p1:
                n = min(510, p1 - t)
                blocks.append((t, n))
                t += n
            t = p0
            while t < p1:
                n = min(512, p1 - t)
                rmsnorm('n2g', t, n)
                t += n
            for j in range(22):
                wts = []
                for tt in range(2):
                    i = wi % 4
                    wi += 1
                    kb.dma('sp', wu[i][:], wb['wup'][l, 2 * j + tt], r_=[('wup', l)], w_=[('wu', i)])
                    wts.append((wu[i], ('wu', i)))
                for (t0, n) in blocks:
                    ys = []
                    for tt in range(2):
                        c = 2 * j + tt
                        ps, pk = PS()
                        op('pe', *[I('matmul', ps[:, 0:n + 2], wts[tt][0][:, kc, :], hT[:, kc, PAD + t0 - 2:PAD + t0 + n],
                                     start=(kc == 0), stop=(kc == 7)) for kc in range(8)], r_=[wts[tt][1], 'hT'], w_=[pk])
                        y, yk = SF()
                        op('act', I('activation', y[:, 0:n], ps[:, 2:n + 2], AF.Identity, bias=pc('fcb', c), scale=pc('fcw', c * 3 + 2)),
                           r_=[pk, 'pp'], w_=[yk])
                        op('dve', I('scalar_tensor_tensor', y[:, 0:n], ps[:, 1:n + 1], pc('fcw', c * 3 + 1), y[:, 0:n], ALU.mult, ALU.add),
                           r_=[pk, yk, 'pp'], w_=[yk])
                        op('dve', I('scalar_tensor_tensor', y[:, 0:n], ps[:, 0:n], pc('fcw', c * 3), y[:, 0:n], ALU.mult, ALU.add),
                           r_=[pk, yk, 'pp'], w_=[yk])
                        ys.append((y, yk))
                    sg, sgk = SF()
                    op('act', I('activation', sg[:, 0:n], ys[0][0][:, 0:n], AF.Silu), r_=[ys[0][1]], w_=[sgk])
                    op('pool', I('tensor_tensor', GT[:, j, t0 - p0:t0 - p0 + n], sg[:, 0:n], ys[1][0][:, 0:n], ALU.mult),
                       r_=[sgk, ys[1][1]], w_=[('GT', j)])
            for oc in range(8):
                i = oc % 2
                kb.dma('sp', wd[i][:], wb['wdn'][l, oc], r_=[('wdn', l)], w_=[('wd', i)])
                for (t0, n) in blocks:
                    ps, pk = PS()
                    op('pe', *[I('matmul', ps[:, 0:n], wd[i][:, kc, :], GT[:, kc, t0 - p0:t0 - p0 + n], start=(kc == 0), stop=(kc == 21))
                               for kc in range(22)], r_=[('wd', i)] + [('GT', kc) for kc in range(22)], w_=[pk])
                    op('dve', I('tensor_tensor', xT[:, oc, t0:t0 + n], ps[:, 0:n], xT[:, oc, t0:t0 + n], ALU.add),
                       r_=[pk, ('x', oc)], w_=[('x', oc)])

    def layer_setup(l):
        kb.dma('sp', ppt[:], dr['pp'][l], w_=['pp'])
        op('act', I('activation', der[:, 0:2], ppt[:, PPC['galog']:PPC['galog'] + 2], AF.Exp), r_=['pp'], w_=['der'])
        op('dve', I('tensor_scalar', der[:, 0:2], der[:, 0:2], -1.0, None, ALU.mult), r_=['der'], w_=['der'])
        if l == 0:
            op('pool', I('memset', der[:, 2:4], 1.0), w_=['der'])
        else:
            op('dve', I('tensor_tensor', der[:, 2:4], ppt[:, PPC['hlb1']:PPC['hlb1'] + 2], ppt[:, PPC['hlb0']:PPC['hlb0'] + 2],
                        ALU.subtract), r_=['pp'], w_=['der'])
            op('act', I('activation', der[:, 2:4], der[:, 2:4], AF.Exp), r_=['der'], w_=['der'])
            op('dve', I('tensor_scalar', der[:, 2:4], der[:, 2:4], 1.0, None, ALU.add), r_=['der'], w_=['der'])
            op('dve', I('reciprocal', der[:, 2:4], der[:, 2:4]), r_=['der'], w_=['der'])
        op('dve', I('tensor_scalar', der[:, 4:6], ppt[:, PPC['lbgk']:PPC['lbgk'] + 2], -1.0, None, ALU.mult), r_=['pp'], w_=['der'])
        op('dve', I('tensor_scalar', der[:, 8:12], ppt[:, PPC['fbf']:PPC['fbf'] + 4], -1.0, None, ALU.mult), r_=['pp'], w_=['der'])
        op('pool', I('memset', der[:, 12:13], -float(np.log(8.0))), w_=['der'])
        op('pool', I('memset', der[:, 13:14], 1.0), w_=['der'])

    for n in range(NSEQ):
        for c in range(8):
            kb.dma('sp', xT[:, c, :], dr['xT'][n, :, c, :], w_=[('x', c)])
        for l in range(L):
            layer_setup(l)
            with Phase():
                alloc_mixer_weights()
                for b in range(NB):
                    rmsnorm('n1g', b * 512, 512)
                for mi, fn in enumerate((lambda: fox(l), lambda: recurrent(l, 'gdn'), lambda: recurrent(l, 'hg'),
                                         lambda: recurrent(l, 'gla'))):
                    if mi in MIXERS:
                        with Phase():
                            fn()
            if FFN:
                with Phase():
                    ffn(l)
        for c in range(8):
            kb.dma('sp', yT[n, :, c, :], xT[:, c, :], r_=[('x', c)], out_final=True)
    kb.emit()
    nc._kb_labels = kb.labels
    return nc


_CACHE = {}


def kernel(**inputs):
    x = np.asarray(inputs['x'], np.float32)
    B, S, _ = x.shape
    L = np.asarray(inputs['w_in']).shape[0]
    nseq = B // NCORES
    packed = pack_weights(inputs)
    key = (S, nseq, L)
    if key not in _CACHE:
        _CACHE[key] = build_program(S, nseq, L)
    nc = _CACHE[key]
    in_maps = []
    for c in range(NCORES):
        xs = x[c * nseq:(c + 1) * nseq]
        xt = np.ascontiguousarray(xs.reshape(nseq, S, 8, 128).transpose(0, 3, 2, 1))
        m = {"xT": xt}
        m.update(packed)
        in_maps.append(m)
    res = run_bass_kernel_spmd(nc, in_maps, core_ids=list(range(NCORES)))
    out = np.empty((B, S, D), np.float32)
    for c in range(NCORES):
        yt = np.asarray(res.results[c]["yT"])
        out[c * nseq:(c + 1) * nseq] = yt.transpose(0, 3, 2, 1).reshape(nseq, S, D)
    return out
```

```python
import contextlib
import os
import numpy as np
import concourse.bass as bass
import concourse.mybir as mybir
from concourse.bass_utils import run_bass_kernel_spmd

F32 = mybir.dt.float32
BF16 = mybir.dt.bfloat16
AF = mybir.ActivationFunctionType
ALU = mybir.AluOpType

ENGS = ('pe', 'act', 'dve', 'pool', 'sp')


class KB:
    NDMASEM = 16

    def __init__(self, nc):
        self.nc = nc
        self.stack = contextlib.ExitStack()
        self.ops = {e: [] for e in ENGS}
        self.cnt = {e: 0 for e in ENGS}
        self.known = {e: {} for e in ENGS}
        self.writers = {}
        self.readers = {}
        self.sem = {}
        for e in ENGS:
            self.sem[e] = self.stack.enter_context(nc.semaphore("s_" + e))
        self.dsem = [self.stack.enter_context(nc.semaphore("d%d" % i)) for i in range(self.NDMASEM)]
        self.dcnt = [0] * self.NDMASEM
        self.dnext = {}
        self.out_waits = []
        self.labels = {} if os.environ.get('KB_LABELS') else None
        self.pending = {e: {} for e in ENGS}

    def sb(self, name, shape, dt):
        self.uid = getattr(self, 'uid', 0) + 1
        return self.stack.enter_context(self.nc.sbuf_tensor("%s_%d" % (name, self.uid), list(shape), dt))

    def ps(self, name, shape, dt):
        return self.stack.enter_context(self.nc.psum_tensor(name, list(shape), dt))

    def barrier(self):
        snap = {e: c for e, c in self.cnt.items() if c > 0}
        for i, c in enumerate(self.dcnt):
            if c > 0:
                snap[('d', i)] = c
        for e in ENGS:
            for k, v in snap.items():
                if k != e and self.pending[e].get(k, 0) < v:
                    self.pending[e][k] = v

    def _deps(self, eng, r_, w_):
        deps = dict(self.pending[eng])
        self.pending[eng] = {}
        for t in r_:
            for k, v in self.writers.get(t, {}).items():
                if deps.get(k, 0) < v:
                    deps[k] = v
            if isinstance(t, tuple) and t[0] in ('ps', 'pt'):
                for k, v in self.readers.get(t, {}).items():
                    if k != eng and deps.get(k, 0) < v:
                        deps[k] = v
        for t in w_:
            for d in (self.writers.get(t, {}), self.readers.get(t, {})):
                for k, v in d.items():
                    if deps.get(k, 0) < v:
                        deps[k] = v
        waits = []
        kn = self.known[eng]
        for k, v in deps.items():
            if kn.get(k, 0) < v:
                kn[k] = v
                waits.append((k, v))
        return waits

    def capture(self, fn):
        self._cap = []
        try:
            fn()
        finally:
            c, self._cap = self._cap, None
        return c

    def commit_interleaved(self, streams):
        n = max(len(x) for x in streams)
        for i in range(n):
            for x in streams:
                if i < len(x):
                    kind, a, k = x[i]
                    lab = k.pop('_label', None)
                    if lab is not None:
                        self.label = lab
                    (self.op if kind == 'op' else self.dma)(*a, **k)

    def op(self, eng, *fns, r_=(), w_=()):
        if getattr(self, '_cap', None) is not None:
            self._cap.append(('op', (eng,) + tuple(fns), dict(r_=r_, w_=w_, _label=getattr(self, 'label', ''))))
            return 0
        waits = self._deps(eng, r_, w_)
        self.cnt[eng] += 1
        idx = self.cnt[eng]
        self.ops[eng].append((waits, fns, ('eng', eng, getattr(self, 'label', ''))))
        for t in r_:
            self.readers.setdefault(t, {})[eng] = idx
        for t in w_:
            self.writers.setdefault(t, {})[eng] = idx
        return idx

    def dma(self, eng, out, in_, r_=(), w_=(), out_final=False, **kw):
        if getattr(self, '_cap', None) is not None:
            self._cap.append(('dma', (eng, out, in_), dict(r_=r_, w_=w_, out_final=out_final, **kw)))
            return
        lo, hi = (0, 4) if eng == 'pool' else (4, self.NDMASEM)
        si = self.dnext.get(eng, lo)
        self.dnext[eng] = lo + (si + 1 - lo) % (hi - lo)
        key = ('d', si)
        waits = self._deps(eng, r_, w_)
        if self.dcnt[si] > 0 and self.known[eng].get(key, 0) < self.dcnt[si]:
            self.known[eng][key] = self.dcnt[si]
            waits.append((key, self.dcnt[si]))
        self.dcnt[si] += 1
        val = self.dcnt[si]
        self.ops[eng].append((waits, (('dma_start', (), dict(out=out, in_=in_, **kw)),), ('dma', si)))
        for t in r_:
            self.readers.setdefault(t, {})[key] = val
        for t in w_:
            self.writers.setdefault(t, {})[key] = val
        if out_final:
            self.out_waits.append((key, val))

    def _semof(self, key):
        if isinstance(key, tuple):
            return self.dsem[key[1]], 16
        return self.sem[key], 1

    def emit(self):
        nc = self.nc
        fin = []
        for key, val in self.out_waits:
            fin.append((key, val))
        with nc.Block() as block:
            def body(engname):
                def f(e):
                    for waits, fns, kind in self.ops[engname]:
                        for k, v in waits:
                            s, mult = self._semof(k)
                            e.wait_ge(s, v * mult)
                        last = None
                        for (nm, a, k) in fns:
                            last = getattr(e, nm)(*a, **k)
                            if self.labels is not None and len(kind) > 2:
                                try:
                                    self.labels[last.ins.name] = kind[2]
                                except Exception:
                                    pass
                        if kind[0] == 'eng':
                            last.then_inc(self.sem[engname], 1)
                        else:
                            last.then_inc(self.dsem[kind[1]], 16)
                    if engname == 'sp':
                        for k, v in fin:
                            s, mult = self._semof(k)
                            e.wait_ge(s, v * mult)
                return f
            block.tensor(body('pe'))
            block.scalar(body('act'))
            block.vector(body('dve'))
            block.gpsimd(body('pool'))
            block.sync(body('sp'))
        self.stack.close()


D = 1024
DFF = 2816
PAD = 4
EPS = 1e-6
NCORES = 8

FCH = (['fq%d' % h for h in range(4)] + ['fk%d' % h for h in range(4)] +
       ['gq0', 'gq1', 'gk0', 'gk1', 'gv0', 'gv1', 'gb0', 'gb1', 'ga0', 'ga1'] + ['gz%d' % h for h in range(4)] +
       ['hq0', 'hq1', 'hf0', 'hf1'] + ['hg%d' % h for h in range(4)] +
       ['lq0', 'lq1', 'lk0', 'lk1', 'lgk'] + ['lg%d' % h for h in range(4)])
FIDX = {n: i for i, n in enumerate(FCH)}
NCF = len(FCH)

PPC = {}
_o = 0
for _n, _w in [('n1g', 8), ('n2g', 8), ('fqg', 1), ('fkg', 1), ('fog', 1), ('fbf', 4), ('gcw', 24), ('galog', 2),
               ('gdt', 2), ('gog', 1), ('hlb0', 2), ('hlb1', 2), ('hog', 1), ('lbgk', 2), ('log', 1),
               ('fcw', 132), ('fcb', 44)]:
    PPC[_n] = _o
    _o += _w
NPP = _o

OFF = {}
_o = 0
for _n, _w in [('fox_qkv', 768), ('fox_f', 4), ('gdn_qkv', 768), ('gdn_b', 4), ('gdn_a', 4), ('gdn_z', 256),
               ('hg_q', 256), ('hg_f', 256), ('hg_i', 256), ('hg_g', 256), ('gla_qk', 256), ('gla_v', 256),
               ('gla_gk', 16), ('gla_g', 256)]:
    OFF[_n] = _o
    _o += _w


def _gla_pad_cols(base, g):
    idx = -np.ones(128, np.int64)
    idx[0:32] = base + (2 * g) * 32 + np.arange(32)
    idx[64:96] = base + (2 * g + 1) * 32 + np.arange(32)
    return idx


def pack_weights(inp):
    L = inp['w_in'].shape[0]
    wif = np.zeros((L, NCF, 128, 8, 128), np.float32)
    wit = np.zeros((L, 128, 8, 768), np.float32)
    wo = np.zeros((L, 4, 64, 4, 1024), np.float32)
    wup = np.zeros((L, 44, 128, 8, 128), np.float32)
    wdn = np.zeros((L, 8, 128, 22, 128), np.float32)
    wgk = np.zeros((L, 16, 256), np.float32)
    pp = np.zeros((L, 128, NPP), np.float32)
    for l in range(L):
        W = np.asarray(inp['w_in'][l])
        Wk = W.reshape(8, 128, -1).transpose(1, 0, 2)

        def put(name, cols):
            cols = np.asarray(cols)
            m = len(cols)
            ok = cols >= 0
            wif[l, FIDX[name], :, :, np.nonzero(ok)[0]] = Wk[:, :, cols[ok]].transpose(2, 0, 1)

        for h in range(4):
            fcol = [OFF['fox_f'] + h] * 4
            put('fq%d' % h, list(OFF['fox_qkv'] + h * 64 + np.arange(64)) + fcol)
            put('fk%d' % h, list(OFF['fox_qkv'] + 256 + h * 64 + np.arange(64)) + fcol)
            put('gz%d' % h, OFF['gdn_z'] + h * 64 + np.arange(64))
            put('hg%d' % h, OFF['hg_g'] + h * 64 + np.arange(64))
            put('lg%d' % h, OFF['gla_g'] + h * 64 + np.arange(64))
        for g in range(2):
            put('gq%d' % g, OFF['gdn_qkv'] + g * 128 + np.arange(128))
            put('gk%d' % g, OFF['gdn_qkv'] + 256 + g * 128 + np.arange(128))
            put('gv%d' % g, OFF['gdn_qkv'] + 512 + g * 128 + np.arange(128))
            put('gb%d' % g, [OFF['gdn_b'] + 2 * g] * 64 + [OFF['gdn_b'] + 2 * g + 1] * 64)
            put('ga%d' % g, [OFF['gdn_a'] + 2 * g] * 64 + [OFF['gdn_a'] + 2 * g + 1] * 64)
            put('hq%d' % g, OFF['hg_q'] + g * 128 + np.arange(128))
            put('hf%d' % g, OFF['hg_f'] + g * 128 + np.arange(128))
            put('lq%d' % g, _gla_pad_cols(OFF['gla_qk'], g))
            put('lk%d' % g, _gla_pad_cols(OFF['gla_qk'] + 128, g))
        put('lgk', OFF['gla_gk'] + np.arange(16))
        wit[l, :, :, 0:256] = Wk[:, :, OFF['fox_qkv'] + 512:OFF['fox_qkv'] + 768]
        wit[l, :, :, 256:512] = Wk[:, :, OFF['hg_i']:OFF['hg_i'] + 256]
        wit[l, :, :, 512:768] = Wk[:, :, OFF['gla_v']:OFF['gla_v'] + 256]
        Wo = np.asarray(inp['w_out'][l])
        wo[l] = Wo.reshape(4, 4, 64, 1024).transpose(0, 2, 1, 3)
        Wu = np.asarray(inp['w_up'][l]).reshape(8, 128, 2 * DFF).transpose(1, 0, 2)
        for j in range(22):
            wup[l, 2 * j] = Wu[:, :, j * 128:(j + 1) * 128]
            wup[l, 2 * j + 1] = Wu[:, :, DFF + j * 128:DFF + (j + 1) * 128]
        Wd = np.asarray(inp['w_down'][l]).reshape(22, 128, 1024).transpose(1, 0, 2)
        for oc in range(8):
            wdn[l, oc] = Wd[:, :, oc * 128:(oc + 1) * 128]
        gw = np.asarray(inp['gla_w_gk'][l])
        for g in range(2):
            idx = _gla_pad_cols(0, g)
            ok = idx >= 0
            wgk[l, :, g * 128 + np.nonzero(ok)[0]] = gw[:, idx[ok]].T
        P = pp[l]
        P[:, PPC['n1g']:PPC['n1g'] + 8] = np.asarray(inp['norm1_g'][l]).reshape(8, 128).T
        P[:, PPC['n2g']:PPC['n2g'] + 8] = np.asarray(inp['norm2_g'][l]).reshape(8, 128).T
        for nm, key in [('fqg', 'fox_qn_g'), ('fkg', 'fox_kn_g'), ('fog', 'fox_on_g'), ('gog', 'gdn_on_g'),
                        ('hog', 'hg_on_g'), ('log', 'gla_on_g')]:
            v = np.asarray(inp[key][l])
            P[0:64, PPC[nm]] = v
            P[64:128, PPC[nm]] = v
        for h in range(4):
            P[64:68, PPC['fbf'] + h] = np.asarray(inp['fox_b_f'][l])[h]
        cw = np.asarray(inp['gdn_conv_w'][l])
        for c in range(6):
            P[:, PPC['gcw'] + c * 4:PPC['gcw'] + c * 4 + 4] = cw[:, c * 128:(c + 1) * 128].T
        for g in range(2):
            for nm, key in [('galog', 'gdn_a_log'), ('gdt', 'gdn_dt_bias')]:
                v = np.asarray(inp[key][l])
                P[0:64, PPC[nm] + g] = v[2 * g]
                P[64:128, PPC[nm] + g] = v[2 * g + 1]
            P[:, PPC['hlb0'] + g] = np.asarray(inp['hg_lb'][0])[g * 128:(g + 1) * 128]
            P[:, PPC['hlb1'] + g] = np.asarray(inp['hg_lb'][1])[g * 128:(g + 1) * 128]
            idx = _gla_pad_cols(0, g)
            ok = idx >= 0
            P[np.nonzero(ok)[0], PPC['lbgk'] + g] = np.asarray(inp['gla_b_gk'][l])[idx[ok]]
        fw = np.asarray(inp['ffn_conv_w'][l])
        fb = np.asarray(inp['ffn_conv_b'][l])
        for j in range(22):
            for t, base in enumerate((j * 128, DFF + j * 128)):
                c = 2 * j + t
                P[:, PPC['fcw'] + c * 3:PPC['fcw'] + c * 3 + 3] = fw[:, base:base + 128].T
                P[:, PPC['fcb'] + c] = fb[base:base + 128]
    return dict(wif=wif, wit=wit, wo=wo, wup=wup, wdn=wdn, wgk=wgk, pp=pp)


def I(name, *a, **k):
    return (name, a, k)


def bcl(ap, n):
    return bass.AP(ap.tensor, ap.offset, [list(x) for x in ap.ap[:-1]] + [[0, n]])


def bcm(ap, n):
    a = [list(x) for x in ap.ap]
    return bass.AP(ap.tensor, ap.offset, [a[0], [0, n]] + a[1:])


def v3(ap, b):
    return ap.rearrange("p (a b) -> p a b", b=b)


def build_program(S, NSEQ, L, MIXERS=(0, 1, 2, 3), FFN=True):
    nc = bass.Bass("TRN2", target_bir_lowering=False)
    NB = S // 512
    dr = {}
    dr['xT'] = nc.dram_tensor("xT", [NSEQ, 128, 8, S], F32, kind="ExternalInput").ap()
    shapes = dict(wif=[L, NCF, 128, 8, 128], wit=[L, 128, 8, 768], wo=[L, 4, 64, 4, 1024],
                  wup=[L, 44, 128, 8, 128], wdn=[L, 8, 128, 22, 128], wgk=[L, 16, 256])
    wb = {}
    for k, sh in shapes.items():
        dr[k] = nc.dram_tensor(k, sh, F32, kind="ExternalInput").ap()
        wb[k] = nc.dram_tensor(k + "_b", sh, BF16, kind="Internal").ap()
    dr['pp'] = nc.dram_tensor("pp", [L, 128, NPP], F32, kind="ExternalInput").ap()
    yT = nc.dram_tensor("yT", [NSEQ, 128, 8, S], F32, kind="ExternalOutput").ap()

    kb = KB(nc)
    op = kb.op

    def flat(ap):
        names = " ".join("abcdefg"[:len(ap.shape)])
        f = ap.rearrange("%s -> (%s)" % (names, names))
        n = f.shape[0]
        fdim = 2048 if n % 2048 == 0 else 256
        return f.rearrange("(r f) -> r f", f=fdim)

    for l in range(L):
        for k in ('wif', 'wit', 'wo', 'wgk', 'wup', 'wdn'):
            src = flat(dr[k][l])
            dst = flat(wb[k][l])
            R = src.shape[0]
            for r0 in range(0, R, 4096):
                r1 = min(R, r0 + 4096)
                kb.dma('pool', dst[r0:r1], src[r0:r1], w_=[(k, l)])

    xT = kb.sb("xT_sb", [128, 8, S], F32)
    hT = kb.sb("hT_sb", [128, 8, PAD + S], BF16)
    MX = kb.sb("MX", [64, 4, 512], BF16)
    ppt = kb.sb("ppt", [128, NPP], F32)
    ident = kb.sb("ident", [128, 128], BF16)
    tri = kb.sb("tri", [128, 128], BF16)
    negU = kb.sb("negU", [64, 64], F32)
    negL = kb.sb("negL", [64, 64], F32)
    eye = kb.sb("eye", [64, 64], F32)
    ones = kb.sb("ones", [128, 128], BF16)
    bd = kb.sb("bd", [128, 128], BF16)
    o64 = kb.sb("o64", [64, 64], BF16)
    wst = kb.sb("wst", [96, 64], BF16)
    cm = kb.sb("cm", [128, 512], F32)
    onesf = kb.sb("onesf", [128, 512], F32)
    epsc = kb.sb("epsc", [128, 1], F32)
    aug = kb.sb("aug", [128, 6], F32)
    der = kb.sb("der", [128, 16], F32)
    zc = kb.sb("zc", [128, 4], F32)

    def sel(t, ap_, cmp, fill, base, pat, cmul=1):
        op('pool', I('affine_select', out=ap_, in_=ap_, pattern=pat, compare_op=cmp, fill=fill, base=base,
                     channel_multiplier=cmul), r_=[t], w_=[t])
    for t_, tl, v in [('ident', ident, 0.0), ('tri', tri, 0.0), ('negU', negU, 0.0), ('negL', negL, 0.0),
                      ('eye', eye, 0.0), ('ones', ones, 1.0), ('bd', bd, 1.0), ('o64', o64, 1.0 / 64),
                      ('wst', wst, 1.0 / 64), ('cm', cm, 1.0), ('onesf', onesf, 1.0), ('epsc', epsc, EPS),
                      ('aug', aug, 0.0), ('hT', hT, 0.0)]:
        op('pool', I('memset', tl[:], v), w_=[t_])
    sel('ident', ident[:], ALU.not_equal, 1.0, 0, [[-1, 128]])
    sel('tri', tri[:], ALU.is_gt, 1.0, 0, [[-1, 128]])
    sel('negU', negU[:], ALU.is_ge, -1.0, 0, [[-1, 64]])
    sel('negL', negL[:], ALU.is_ge, -1.0, 0, [[1, 64]], cmul=-1)
    sel('eye', eye[:], ALU.not_equal, 1.0, 0, [[-1, 64]])
    op('pool', I('memset', bd[0:64, 64:128], 0.0), w_=['bd'])
    op('pool', I('memset', bd[64:128, 0:64], 0.0), w_=['bd'])
    op('pool', I('memset', wst[64:96, :], 0.0), w_=['wst'])
    op('pool', I('memset', wst[64:65, :], EPS), w_=['wst'])
    op('pool', I('memset', v3(cm[:], 64)[:, :, 0:1], 0.0), w_=['cm'])
    op('pool', I('memset', zc[:], 0.0), w_=['zc'])
    for r0_ in (0, 64):
        sel('zc', zc[:, 0:1], ALU.not_equal, -1.0, -r0_, [[0, 1]])
        sel('zc', zc[:, 1:2], ALU.not_equal, 1.0, -(r0_ + 1), [[0, 1]])
        sel('zc', zc[:, 2:3], ALU.not_equal, 1.0, -(r0_ + 1), [[0, 1]])
        sel('zc', zc[:, 3:4], ALU.not_equal, 1.0, -r0_, [[0, 1]])
    sel('aug', aug[:, 0:1], ALU.not_equal, 1.0, -64, [[0, 1]])
    sel('aug', aug[:, 1:2], ALU.not_equal, 1.0, -65, [[0, 1]])
    sel('aug', aug[:, 2:3], ALU.is_gt, 1.0, 66, [[0, 1]], cmul=-1)
    sel('aug', aug[:, 3:4], ALU.not_equal, -1.0, -66, [[0, 1]])
    sel('aug', aug[:, 4:5], ALU.not_equal, -1.0, -67, [[0, 1]])
    sel('aug', aug[:, 5:6], ALU.is_ge, 1.0, -66, [[0, 1]])

    NPS = 7
    psb = [kb.ps("ps%d" % i, [128, 512], F32) for i in range(NPS)]
    ptb = kb.ps("ptb", [128, 1024], BF16)
    st = {'ps': 0, 'pt': 0, 'wf': 0}

    STREAM = {'s': None}

    def _rot(key, n):
        sid = STREAM['s']
        if sid is None:
            i = st.get(key, 0)
            st[key] = (i + 1) % n
            return i
        lo = 0 if sid == 0 else (n + 1) // 2
        hi = (n + 1) // 2 if sid == 0 else n
        k2 = (key, sid)
        i = st.get(k2, lo)
        st[k2] = lo + (i + 1 - lo) % (hi - lo)
        return i

    def PS():
        i = _rot('ps', NPS - 2)
        return psb[i], ('ps', i)

    def PSA():
        if STREAM['s'] is not None:
            i = NPS - 2 + STREAM['s']
        else:
            i = NPS - 2 + st.get('psa', 0)
            st['psa'] = (st.get('psa', 0) + 1) % 2
        return psb[i], ('ps', i)

    def two_streams(fa, fb):
        caps = []
        for sid, f in enumerate((fa, fb)):
            STREAM['s'] = sid
            caps.append(kb.capture(f))
        STREAM['s'] = None
        kb.commit_interleaved(caps)

    def PT():
        i = st['pt']
        st['pt'] = (i + 1) % 2
        return ptb[:, i * 512:(i + 1) * 512], ('pt', 0)

    NSF, NSB = 10, 12
    sf = [kb.sb("sf%d" % i, [128, 512], F32) for i in range(NSF)]
    sbf = [kb.sb("sbf%d" % i, [128, 512], BF16) for i in range(NSB)]
    stc = {'f': 0, 'b': 0}

    def SF():
        i = _rot('sf', NSF)
        return sf[i], ('sf', i)

    def SB():
        i = _rot('sb', NSB)
        return sbf[i], ('sbf', i)

    def pc(name, j=0, r0=0, r1=128):
        return ppt[r0:r1, PPC[name] + j:PPC[name] + j + 1]

    class Phase:
        def __enter__(self):
            kb.barrier()
            self.prev = kb.stack
            kb.stack = contextlib.ExitStack()
            return self

        def __exit__(self, *a):
            kb.barrier()
            kb.stack.close()
            kb.stack = self.prev

    W = {}

    def alloc_mixer_weights():
        W['wfs'] = [kb.sb("wf%d" % i, [128, 8, 128], BF16) for i in range(4)]
        W['wits'] = kb.sb("wits", [128, 8, 256], BF16)
        W['wos'] = kb.sb("wos", [64, 4, 1024], BF16)
        W['wgks'] = kb.sb("wgks", [16, 256], BF16)

    def load_wf(l, name):
        i = _rot('wf', 4)
        kb.dma('sp', W['wfs'][i][:], wb['wif'][l, FIDX[name]], r_=[('wif', l)], w_=[('wf', i)])
        return W['wfs'][i], ('wf', i)

    def proj_f(l, name, M, c0, n):
        wt, wk = load_wf(l, name)
        ps, pk = PS()
        op('pe', *[I('matmul', ps[0:M, 0:n], wt[:, kc, 0:M], hT[:, kc, c0:c0 + n], start=(kc == 0), stop=(kc == 7))
                   for kc in range(8)], r_=[wk, 'hT'], w_=[pk])
        return ps, pk

    def rmsnorm(gname, t0, n):
        ps, pk = PS()
        for c in range(8):
            sq, sk = SB()
            op('act', I('activation', sq[:, 0:n], xT[:, c, t0:t0 + n], AF.Square), r_=[('x', c)], w_=[sk])
            op('pe', I('matmul', ps[:, 0:n], ones[:, :], sq[:, 0:n], start=(c == 0), stop=(c == 7)), r_=[sk, 'ones'], w_=[pk])
        rs, rk = SF()
        op('act', I('activation', rs[:, 0:n], ps[:, 0:n], AF.Ln, bias=epsc[:, 0:1], scale=1.0 / D), r_=[pk, 'epsc'], w_=[rk])
        op('act', I('activation', rs[:, 0:n], rs[:, 0:n], AF.Exp, scale=-0.5), r_=[rk], w_=[rk])
        for c in range(8):
            op('dve', I('scalar_tensor_tensor', hT[:, c, PAD + t0:PAD + t0 + n], xT[:, c, t0:t0 + n], pc(gname, c),
                        rs[:, 0:n], ALU.mult, ALU.mult), r_=[('x', c), rk, 'pp'], w_=['hT'])

    def wout_apply(b):
        wos = W['wos']
        for oc in range(8):
            ps, pk = PS()
            op('pe', *[I('matmul', ps[:, :], wos[0:64, h, oc * 128:(oc + 1) * 128], MX[0:64, h, :],
                         start=(h == 0), stop=(h == 3)) for h in range(4)], r_=['wos', 'MX'], w_=[pk])
            op('dve', I('tensor_tensor', xT[:, oc, b * 512:(b + 1) * 512], ps[:, :], xT[:, oc, b * 512:(b + 1) * 512],
                        ALU.add), r_=[pk, ('x', oc)], w_=[('x', oc)])

    def rstd_from(ps, pk, rows, n, bias_ap, scale=1.0, ebias=None, r0=0):
        rs, rk = SF()
        kw = {} if bias_ap is None else dict(bias=bias_ap)
        op('act', I('activation', rs[r0:r0 + rows, 0:n], ps[r0:r0 + rows, 0:n], AF.Ln, scale=scale, **kw),
           r_=[pk, 'epsc'], w_=[rk])
        kw = {} if ebias is None else dict(bias=ebias)
        op('act', I('activation', rs[r0:r0 + rows, 0:n], rs[r0:r0 + rows, 0:n], AF.Exp, scale=-0.5, **kw),
           r_=[rk, 'der'], w_=[rk])
        return rs, rk

    def outnorm_gate(l, b, gname, zname, OT, seq=False):
        def on_head(h):
            sq, sk = SB()
            op('act', I('activation', sq[0:64, :], OT[0:64, h, :], AF.Square), r_=['OT'], w_=[sk])
            p2, pk2 = PS()
            op('pe', I('matmul', p2[0:64, :], o64[:, :], sq[0:64, :], start=True, stop=True), r_=[sk, 'o64'], w_=[pk2])
            rs, rk = rstd_from(p2, pk2, 64, 512, epsc[0:64, 0:1])
            zp, zk = proj_f(l, '%s%d' % (zname, h), 64, PAD + b * 512, 512)
            gt, gk = SF()
            op('act', I('activation', gt[0:64, :], zp[0:64, :], AF.Silu), r_=[zk], w_=[gk])
            t, tk = SF()
            op('dve', I('scalar_tensor_tensor', t[0:64, :], OT[0:64, h, :], pc(gname, 0, 0, 64), rs[0:64, :],
                        ALU.mult, ALU.mult), r_=['OT', rk, 'pp'], w_=[tk])
            op('dve', I('tensor_tensor', MX[0:64, h, :], t[0:64, :], gt[0:64, :], ALU.mult), r_=[tk, gk], w_=['MX'])
        if seq:
            for h in range(4):
                on_head(h)
        else:
            two_streams(lambda: [on_head(h) for h in (0, 1)], lambda: [on_head(h) for h in (2, 3)])

    def fox(l):
        NT = S // 128
        KA = kb.sb("KA", [96, 4, S], BF16)
        QA = kb.sb("QA", [96, 4, 512], BF16)
        Vp = kb.sb("Vp", [128, NT, 4, 96], BF16)
        CC = kb.sb("CC", [68, 4, 2], F32)
        wits = W['wits']
        op('pool', I('memset', Vp[:, :, :, 64:96], 0.0), w_=['Vp1'])
        op('pool', I('memset', Vp[:, :, :, 64:65], 1.0), w_=['Vp1'])
        op('pool', I('memset', KA[64:96, :, :], 0.0), w_=['KA0'] + [('KA', h_, b_) for h_ in range(4) for b_ in range(NB)])
        op('pool', I('memset', QA[64:96, :, :], 0.0), w_=['QA0'] + [('QA', h_) for h_ in range(4)])
        kb.dma('sp', wits[:], wb['wit'][l, :, :, 0:256], r_=[('wit', l)], w_=['wits'])
        kb.dma('sp', W['wos'][:], wb['wo'][l, 0], r_=[('wo', l)], w_=['wos'])
        for i in range(NT):
            ps, pk = PS()
            op('pe', *[I('matmul', ps[:, 0:256], hT[:, kc, PAD + i * 128:PAD + (i + 1) * 128], wits[:, kc, 0:256],
                         start=(kc == 0), stop=(kc == 7)) for kc in range(8)], r_=['hT', 'wits'], w_=[pk])
            op('act', I('activation', Vp[:, i, :, 0:64], v3(ps[:, 0:256], 64), AF.Copy), r_=[pk], w_=[('Vp', i)])
        for b in range(NB):
            def fox_proj(h):
                for which in ('k', 'q'):
                    ps, pk = proj_f(l, 'f%s%d' % (which, h), 68, PAD + b * 512, 512)
                    sq, sk = SB()
                    op('act', I('activation', sq[0:64, :], ps[0:64, :], AF.Square), r_=[pk], w_=[sk])
                    p2, pk2 = PS()
                    op('pe', I('matmul', p2[0:64, :], o64[:, :], sq[0:64, :], start=True, stop=True), r_=[sk, 'o64'], w_=[pk2])
                    rs, rk = rstd_from(p2, pk2, 64, 512, epsc[0:64, 0:1],
                                       ebias=(der[0:64, 12:13] if which == 'q' else None))
                    dst = QA[0:64, h, :] if which == 'q' else KA[0:64, h, b * 512:(b + 1) * 512]
                    dk = ('QA', h) if which == 'q' else ('KA', h, b)
                    op('dve', I('scalar_tensor_tensor', dst, ps[0:64, :], pc('fqg' if which == 'q' else 'fkg', 0, 0, 64),
                                rs[0:64, :], ALU.mult, ALU.mult), r_=[pk, rk, 'pp'], w_=[dk])
                    if which == 'k':
                        continue
                    t1, tk1 = SF()
                    op('act', I('activation', t1[64:68, :], ps[64:68, :], AF.Exp, bias=der[64:68, 8 + h:9 + h], scale=-1.0),
                       r_=[pk, 'der'], w_=[tk1])
                    op('act', I('activation', t1[64:68, :], t1[64:68, :], AF.Ln, bias=der[64:68, 13:14]), r_=[tk1, 'der'], w_=[tk1])
                    c, ck = SF()
                    init = 0.0 if b == 0 else CC[64:68, h, 0:1]
                    op('dve', I('tensor_tensor_scan', c[64:68, :], onesf[64:68, :], t1[64:68, :], init, ALU.mult, ALU.subtract),
                       r_=[tk1, 'onesf', ('CC', h)], w_=[ck])
                    op('dve', I('tensor_copy', CC[64:68, h, 0:1], c[64:68, 511:512]), r_=[ck], w_=[('CC', h)])
                    H, hk = SB()
                    M_, mk = SB()
                    r1, rk1 = SF()
                    op('dve', I('tensor_copy', H[64:68, :], c[64:68, :]), r_=[ck], w_=[hk])
                    op('dve', I('tensor_tensor', r1[64:68, :], c[64:68, :], H[64:68, :], ALU.subtract), r_=[ck, hk], w_=[rk1])
                    op('dve', I('tensor_copy', M_[64:68, :], r1[64:68, :]), r_=[rk1], w_=[mk])
                    for (dst2, dk2, a0) in ((QA[64:68, h, :], ('QA', h), 0), (KA[64:68, h, b * 512:(b + 1) * 512], ('KA', h, b), 3)):
                        t2, tk2 = SF()
                        op('dve', I('tensor_scalar', t2[64:68, :], H[64:68, :], aug[64:68, a0:a0 + 1], None, ALU.mult),
                           r_=[hk, 'aug'], w_=[tk2])
                        op('dve', I('scalar_tensor_tensor', t2[64:68, :], M_[64:68, :], aug[64:68, a0 + 1:a0 + 2], t2[64:68, :],
                                    ALU.mult, ALU.add), r_=[mk, tk2, 'aug'], w_=[tk2])
                        op('dve', I('tensor_scalar', dst2, t2[64:68, :], aug[64:68, a0 + 2:a0 + 3], None, ALU.add),
                           r_=[tk2, 'aug'], w_=[dk2])
            two_streams(lambda: [fox_proj(h) for h in (0, 1)], lambda: [fox_proj(h) for h in (2, 3)])
            if os.environ.get('FOXSTOP') == '1':
                continue
            FS = int(os.environ.get('FOXSTOP', '9'))
            def fox_attn(h):
                po, pok = PSA()
                nk = 4 * b + 4
                def ST(i):
                    m = i - 4 * b
                    c0 = 128 * m if m > 0 else 0
                    n = 512 - c0
                    ps, pk = PS()
                    op('pe', I('matmul', ps[:, 0:n], KA[0:96, h, i * 128:(i + 1) * 128], QA[0:96, h, c0:512], start=True, stop=True),
                       r_=[('KA', h, i // 4), ('QA', h), 'KA0', 'QA0'], w_=[pk])
                    return ps, pk, m, c0, n
                cur = ST(0)
                for i in range(nk):
                    nxt = ST(i + 1) if i + 1 < nk else None
                    ps, pk, m, c0, n = cur
                    cur = nxt
                    pt, ptk = SB()
                    if m >= 0:
                        op('dve', I('tensor_scalar', ps[:, 0:128], ps[:, 0:128], 40.0, None, ALU.min), r_=[pk], w_=[pk])
                    op('act', I('activation', pt[:, 0:n], ps[:, 0:n], AF.Exp), r_=[pk], w_=[ptk])
                    if m >= 0 and FS >= 4:
                        op('dve', I('tensor_tensor', pt[:, 0:128], pt[:, 0:128], tri[:, :], ALU.mult), r_=[ptk, 'tri'], w_=[ptk])
                    if FS >= 5:
                        op('pe', I('matmul', po[0:96, c0:512], Vp[:, i, h, 0:96], pt[:, 0:n], start=(i == 0), stop=(i == nk - 1)),
                           r_=[ptk, ('Vp', i), 'Vp1'], w_=[pok])
                if FS < 6:
                    return
                sq, sk = SB()
                of, ok_ = SF()
                op('act', I('activation', sq[0:96, :], po[0:96, :], AF.Square), r_=[pok], w_=[sk])
                op('dve', I('tensor_copy', of[0:64, :], po[0:64, :]), r_=[pok, sk], w_=[ok_])
                if FS < 7:
                    return
                p2, pk2 = PS()
                op('pe', I('matmul', p2[0:64, :], wst[0:96, :], sq[0:96, :], start=True, stop=True), r_=[sk, 'wst'], w_=[pk2])
                rs, rk = rstd_from(p2, pk2, 64, 512, None)
                op('dve', I('scalar_tensor_tensor', MX[0:64, h, :], of[0:64, :], pc('fog', 0, 0, 64), rs[0:64, :],
                            ALU.mult, ALU.mult), r_=[ok_, rk, 'pp'], w_=['MX'])
            two_streams(lambda: [fox_attn(h) for h in (0, 1)], lambda: [fox_attn(h) for h in (2, 3)])
            if os.environ.get('FOXSTOP') != '2':
                wout_apply(b)

    def recurrent(l, kind):
        m = {'gdn': 1, 'hg': 2, 'gla': 3}[kind]
        gdn = kind == 'gdn'
        S32 = kb.sb("S32", [128, 2, 64], F32)
        Sb = kb.sb("Sb", [128, 2, 64], BF16)
        EL = kb.sb("EL", [128, 2, 8, 1], F32)
        OT = kb.sb("OT", [64, 4, 512], BF16)
        qT = [kb.sb("qT%d" % g, [128, 512], BF16) for g in range(2)]
        kd = [kb.sb("kd%d" % g, [128, 512], BF16) for g in range(2)]
        ko = [kb.sb("ko%d" % g, [128, 512], BF16) for g in range(2)]
        if gdn:
            qg = [kb.sb("qg%d" % g, [128, 512], BF16) for g in range(2)]
            kbt = [kb.sb("kbt%d" % g, [128, 512], BF16) for g in range(2)]
            Zl = [kb.sb("Zl%d" % g, [128, 512], F32) for g in range(2)]
            Zr = [kb.sb("Zr%d" % g, [128, 512], F32) for g in range(2)]
            kw = [kb.sb("kw%d" % g, [128, 512], BF16) for g in range(2)]
            vb = [kb.sb("vb%d" % g, [128, 512], BF16) for g in range(2)]
            RAW = [[kb.sb("raw%d%d" % (t, g), [128, 516], BF16) for g in range(2)] for t in range(3)]
        else:
            qg = [kb.sb("qg%d" % g, [128, 512], BF16) for g in range(2)]
            gkT = kb.sb("gkT", [16, 512], BF16)
        wits, wgks = W['wits'], W['wgks']
        AQs2 = [kb.sb("AQs%d" % i, [64, 512], BF16) for i in range(2)]
        KTs2 = [kb.sb("KTs%d" % i, [64, 512], BF16) for i in range(2)]
        VTs2 = [kb.sb("VTs%d" % i, [64, 512], BF16) for i in range(2)] if not gdn else [None, None]
        WTs2 = [kb.sb("WTs%d" % i, [128, 256], BF16) for i in range(2)]
        Us2 = [kb.sb("Us%d" % i, [64, 512], F32) for i in range(2)] if gdn else None
        Sb2 = [Sb, kb.sb("Sb1", [128, 2, 64], BF16)]
        cst = {'n': 0}
        op('pool', I('memset', S32[:], 0.0), w_=['S32'])
        op('pool', I('memset', Sb2[0][:], 0.0), w_=[('Sb', 0)])
        op('pool', I('memset', Sb2[1][:], 0.0), w_=[('Sb', 1)])
        kb.dma('sp', W['wos'][:], wb['wo'][l, m], r_=[('wo', l)], w_=['wos'])
        if not gdn:
            c0w = 256 if kind == 'hg' else 512
            kb.dma('sp', wits[:], wb['wit'][l, :, :, c0w:c0w + 256], r_=[('wit', l)], w_=['wits'])
        if kind == 'gla':
            kb.dma('sp', wgks[:], wb['wgk'][l], r_=[('wgk', l)], w_=['wgks'])

        def decay_tiles(tg, tgk, sc):
            tG, tGk = SF()
            op('dve', I('tensor_tensor_scan', tG[:, :], cm[:, :], tg[:, :], 0.0, ALU.mult, ALU.add), r_=[tgk, 'cm'], w_=[tGk])
            tm, tmk = SF()
            op('dve', I('tensor_tensor', v3(tm[:, :], 64), v3(tG[:, :], 64), bcl(v3(tG[:, :], 64)[:, :, 31:32], 64), ALU.subtract),
               r_=[tGk], w_=[tmk])
            op('act', I('activation', tg[:, :], tG[:, :], AF.Exp, scale=sc), r_=[tGk], w_=[tgk])
            op('act', I('activation', tG[:, :], tm[:, :], AF.Exp, scale=sc), r_=[tmk], w_=[tGk])
            op('act', I('activation', tm[:, :], tm[:, :], AF.Exp, scale=-sc), r_=[tmk], w_=[tmk])
            return (tg, tgk), (tG, tGk), (tm, tmk)

        def pre_la(b, g):
            c0 = PAD + b * 512
            if kind == 'hg':
                ps, pk = proj_f(l, 'hf%d' % g, 128, c0, 512)
                tk_, tkk = SF()
                op('act', I('activation', tk_[:, :], ps[:, :], AF.Exp), r_=[pk], w_=[tkk])
                op('dve', I('tensor_scalar', tk_[:, :], tk_[:, :], 1.0, None, ALU.add), r_=[tkk], w_=[tkk])
                op('dve', I('reciprocal', tk_[:, :], tk_[:, :]), r_=[tkk], w_=[tkk])
                op('dve', I('tensor_scalar', tk_[:, :], tk_[:, :], der[:, 2 + g:3 + g], None, ALU.mult), r_=[tkk, 'der'], w_=[tkk])
                tg, tgk = SF()
                op('act', I('activation', tg[:, :], tk_[:, :], AF.Ln, bias=der[:, 13:14], scale=-1.0), r_=[tkk, 'der'], w_=[tgk])
                sc, qs = 1.0, 0.125
            else:
                ps, pk = PS()
                op('pe', I('matmul', ps[:, :], wgks[0:16, g * 128:(g + 1) * 128], gkT[0:16, :], start=True, stop=True),
                   r_=['wgks', 'gkT'], w_=[pk])
                tg, tgk = SF()
                op('act', I('activation', tg[:, :], ps[:, :], AF.Exp, bias=der[:, 4 + g:5 + g], scale=-1.0), r_=[pk, 'der'], w_=[tgk])
                op('act', I('activation', tg[:, :], tg[:, :], AF.Ln, bias=der[:, 13:14]), r_=[tgk, 'der'], w_=[tgk])
                sc, qs = -1.0 / 16.0, float(32.0 ** -0.5)
            (E1, e1k), (E2, e2k), (E3, e3k) = decay_tiles(tg, tgk, sc)
            op('dve', I('tensor_copy', EL[:, g, :, :], v3(E1[:, :], 64)[:, :, 63:64]), r_=[e1k], w_=['EL'])
            if kind == 'hg':
                ps, pk = proj_f(l, 'hq%d' % g, 128, c0, 512)
                tq, tqk = SF()
                op('act', I('activation', tq[:, :], ps[:, :], AF.Silu), r_=[pk], w_=[tqk])
                qsrc, qk_ = tq, tqk
                op('dve', I('tensor_tensor', kd[g][:, :], tk_[:, :], E3[:, :], ALU.mult), r_=[tkk, e3k], w_=[('kd', g)])
            else:
                ps, pk = proj_f(l, 'lq%d' % g, 128, c0, 512)
                qsrc, qk_ = ps, pk
            op('dve', I('scalar_tensor_tensor', qT[g][:, :], qsrc[:, :], qs, E1[:, :], ALU.mult, ALU.mult), r_=[qk_, e1k], w_=[('qT', g)])
            op('dve', I('scalar_tensor_tensor', qg[g][:, :], qsrc[:, :], qs, E2[:, :], ALU.mult, ALU.mult), r_=[qk_, e2k], w_=[('qg', g)])
            if kind == 'gla':
                ps, pk = proj_f(l, 'lk%d' % g, 128, c0, 512)
                op('dve', I('tensor_tensor', kd[g][:, :], ps[:, :], E3[:, :], ALU.mult), r_=[pk, e3k], w_=[('kd', g)])
            op('dve', I('tensor_tensor', v3(ko[g][:, :], 64), v3(kd[g][:, :], 64), bcl(v3(E2[:, :], 64)[:, :, 63:64], 64), ALU.mult),
               r_=[('kd', g), e2k], w_=[('ko', g)])

        def gk_common(b):
            c0 = PAD + b * 512
            ps, pk = proj_f(l, 'lgk', 16, c0, 512)
            op('act', I('activation', gkT[0:16, :], ps[0:16, :], AF.Copy), r_=[pk], w_=['gkT'])

        def pre_gdn(b, g):
            c0 = PAD + b * 512
            ps, pk = proj_f(l, 'ga%d' % g, 128, c0, 512)
            ta, tak = SF()
            op('act', I('activation', ta[:, :], ps[:, :], AF.Exp, bias=pc('gdt', g)), r_=[pk, 'pp'], w_=[tak])
            op('act', I('activation', ta[:, :], ta[:, :], AF.Ln, bias=der[:, 13:14]), r_=[tak, 'der'], w_=[tak])
            op('dve', I('tensor_scalar', ta[:, :], ta[:, :], der[:, g:g + 1], None, ALU.mult), r_=[tak, 'der'], w_=[tak])
            tG, tGk = SF()
            op('dve', I('tensor_tensor_scan', tG[:, :], cm[:, :], ta[:, :], 0.0, ALU.mult, ALU.add), r_=[tak, 'cm'], w_=[tGk])
            tn, tnk = SF()
            op('dve', I('tensor_scalar', Zl[g][:, :], tG[:, :], zc[:, 0:1], zc[:, 1:2], ALU.mult, ALU.add), r_=[tGk, 'zc'], w_=[('Zl', g)])
            op('dve', I('tensor_scalar', Zr[g][:, :], tG[:, :], zc[:, 2:3], zc[:, 3:4], ALU.mult, ALU.add), r_=[tGk, 'zc'], w_=[('Zr', g)])
            op('act', I('activation', ta[:, :], tG[:, :], AF.Exp), r_=[tGk], w_=[tak])
            op('dve', I('tensor_tensor', v3(tn[:, :], 64), bcl(v3(tG[:, :], 64)[:, :, 63:64], 64), v3(tG[:, :], 64), ALU.subtract),
               r_=[tGk], w_=[tnk])
            op('act', I('activation', tn[:, :], tn[:, :], AF.Exp), r_=[tnk], w_=[tnk])
            op('dve', I('tensor_copy', EL[:, g, :, :], v3(ta[:, :], 64)[:, :, 63:64]), r_=[tak], w_=['EL'])
            ps, pk = proj_f(l, 'gb%d' % g, 128, c0, 512)
            tu, tuk = SF()
            op('act', I('activation', tu[:, :], ps[:, :], AF.Exp, scale=-1.0), r_=[pk], w_=[tuk])
            op('act', I('activation', tu[:, :], tu[:, :], AF.Ln, bias=der[:, 13:14]), r_=[tuk, 'der'], w_=[tuk])
            op('dve', I('tensor_tensor', tG[:, :], tG[:, :], tu[:, :], ALU.subtract), r_=[tGk, tuk], w_=[tGk])
            op('act', I('activation', tG[:, :], tG[:, :], AF.Exp), r_=[tGk], w_=[tGk])
            op('act', I('activation', tu[:, :], tu[:, :], AF.Exp, scale=-1.0), r_=[tuk], w_=[tuk])

            def conv(ti):
                raw = RAW[ti][g]
                rk_ = ('raw', ti, g)
                if b == 0:
                    op('pool', I('memset', raw[:, 0:4], 0.0), w_=[rk_])
                else:
                    op('pool', I('tensor_copy', raw[:, 1:4], raw[:, 513:516]), r_=[rk_], w_=[rk_])
                ps, pk = proj_f(l, ('gq', 'gk', 'gv')[ti] + str(g), 128, c0, 512)
                op('act', I('activation', raw[:, 4:516], ps[:, :], AF.Copy), r_=[pk], w_=[rk_])
                y, yk = SF()
                ci = ti * 2 + g
                op('dve', I('tensor_scalar', y[:, :], raw[:, 4:516], pc('gcw', ci * 4 + 3), None, ALU.mult), r_=[rk_, 'pp'], w_=[yk])
                for j in (2, 1, 0):
                    op('dve', I('scalar_tensor_tensor', y[:, :], raw[:, 1 + j:513 + j], pc('gcw', ci * 4 + j), y[:, :],
                                ALU.mult, ALU.add), r_=[rk_, yk, 'pp'], w_=[yk])
                op('act', I('activation', y[:, :], y[:, :], AF.Silu), r_=[yk], w_=[yk])
                return y, yk

            def l2n(y, yk, ebias):
                sq, sk = SB()
                op('act', I('activation', sq[:, :], y[:, :], AF.Square), r_=[yk], w_=[sk])
                p2, pk2 = PS()
                op('pe', I('matmul', p2[:, :], bd[:, :], sq[:, :], start=True, stop=True), r_=[sk, 'bd'], w_=[pk2])
                rs, rk = SB()
                op('act', I('activation', rs[:, :], p2[:, :], AF.Ln, bias=epsc[:, 0:1]), r_=[pk2, 'epsc'], w_=[rk])
                kw_ = {} if ebias is None else dict(bias=ebias)
                op('act', I('activation', rs[:, :], rs[:, :], AF.Exp, scale=-0.5, **kw_), r_=[rk, 'der'], w_=[rk])
                op('dve', I('tensor_tensor', y[:, :], y[:, :], rs[:, :], ALU.mult), r_=[yk, rk], w_=[yk])

            y, yk = conv(0)
            l2n(y, yk, der[:, 12:13])
            op('dve', I('tensor_tensor', qT[g][:, :], y[:, :], ta[:, :], ALU.mult), r_=[yk, tak], w_=[('qT', g)])
            op('pool', I('tensor_copy', qg[g][:, :], y[:, :]), r_=[yk], w_=[('qg', g)])
            y, yk = conv(1)
            l2n(y, yk, None)
            op('dve', I('tensor_tensor', kw[g][:, :], y[:, :], tG[:, :], ALU.mult), r_=[yk, tGk], w_=[('kw', g)])
            op('pool', I('tensor_copy', kd[g][:, :], y[:, :]), r_=[yk], w_=[('kd', g)])
            op('dve', I('tensor_tensor', kbt[g][:, :], y[:, :], tu[:, :], ALU.mult), r_=[yk, tuk], w_=[('kbt', g)])
            op('dve', I('tensor_tensor', ko[g][:, :], y[:, :], tn[:, :], ALU.mult), r_=[yk, tnk], w_=[('ko', g)])
            y, yk = conv(2)
            op('dve', I('tensor_tensor', vb[g][:, :], y[:, :], tu[:, :], ALU.mult), r_=[yk, tuk], w_=[('vb', g)])

        def hgj(j, h):
            return (j * 4 + h) * 64, h // 2, (h % 2) * 64

        def mm8(ps, pk, lhs, rhs, r_, split=False):
            groups = [(0, 2), (1, 3)] if split else [(0, 1, 2, 3)]
            for hs in groups:
                op('pe', *[I('matmul', ps[0:64, hgj(j, h)[0]:hgj(j, h)[0] + 64], lhs(j, h), rhs(j, h), start=True, stop=True)
                           for j in range(2) for h in hs], r_=r_, w_=[pk])

        def fm(tiles, sc):
            return lambda j, h: tiles[h // 2][(h % 2) * 64:(h % 2) * 64 + 64, (2 * sc + j) * 64:(2 * sc + j + 1) * 64]

        def tm(tile):
            return lambda j, h: tile[0:64, (j * 4 + h) * 64:(j * 4 + h + 1) * 64]

        def transp8(src, sc, r_, dst=None):
            pt, ptk = PT()
            for hs in ((0, 2), (1, 3)):
                op('pe', *[I('transpose', pt[0:64, hgj(j, h)[0]:hgj(j, h)[0] + 64], fm(src, sc)(j, h),
                             ident[hgj(j, h)[2]:hgj(j, h)[2] + 64, hgj(j, h)[2]:hgj(j, h)[2] + 64])
                           for j in range(2) for h in hs], r_=r_ + ['ident'], w_=[ptk])
            t, tk = dst if dst is not None else SB()
            op('act', I('activation', t[0:64, :], pt[0:64, :], AF.Copy), r_=[ptk], w_=[tk])
            return t, tk

        def A_phase(b, sc):
            res = {}
            sl = sc % 2
            AQs, KTs, VTs, WTs = AQs2[sl], KTs2[sl], VTs2[sl], WTs2[sl]
            if gdn:
                zr_ = [('Zl', 0), ('Zl', 1), ('Zr', 0), ('Zr', 1)]
                zf = lambda tiles: (lambda j, h: tiles[h // 2][(h % 2) * 64:(h % 2) * 64 + 2, (2 * sc + j) * 64:(2 * sc + j + 1) * 64])
                psD, pkD = PS()
                mm8(psD, pkD, zf(Zr), zf(Zl), zr_, split=True)
                psDT, pkDT = PS()
                mm8(psDT, pkDT, zf(Zl), zf(Zr), zr_, split=True)
                Dm, dmk = SF()
                DT, dtk = SF()
                DTu, dtuk = SF()
                op('dve', I('tensor_scalar', Dm[0:64, :], psD[0:64, :], 0.0, None, ALU.min), r_=[pkD], w_=[dmk])
                op('act', I('activation', Dm[0:64, :], Dm[0:64, :], AF.Exp), r_=[dmk], w_=[dmk])
                op('pool', I('tensor_tensor', v3(Dm[0:64, :], 64), v3(Dm[0:64, :], 64), bcm(negL[:, :], 8), ALU.mult), r_=[dmk, 'negL'], w_=[dmk])
                op('dve', I('tensor_scalar', DT[0:64, :], psDT[0:64, :], 0.0, None, ALU.min), r_=[pkDT], w_=[dtk])
                op('act', I('activation', DT[0:64, :], DT[0:64, :], AF.Exp), r_=[dtk], w_=[dtk])
                op('pool', I('tensor_tensor', v3(DTu[0:64, :], 64), v3(DT[0:64, :], 64), bcm(negU[:, :], 8), ALU.mult), r_=[dtk, 'negU'], w_=[dtuk])
                op('pool', I('tensor_tensor', v3(DT[0:64, :], 64), v3(DT[0:64, :], 64), bcm(tri[0:64, 0:64], 8), ALU.mult), r_=[dtk, dtuk, 'tri'], w_=[dtk])
            ps, pk = PS()
            mm8(ps, pk, fm(kd, sc), fm(qg, sc), [('kd', 0), ('kd', 1), ('qg', 0), ('qg', 1), ('qT', 0), ('qT', 1)], split=True)
            AQ, aqk = AQs, ('AQs', sl)
            if gdn:
                op('dve', I('tensor_tensor', AQ[0:64, :], ps[0:64, :], DT[0:64, :], ALU.mult), r_=[pk, dtk], w_=[aqk])
            else:
                op('dve', I('tensor_tensor', v3(AQ[0:64, :], 64), v3(ps[0:64, :], 64), bcm(tri[0:64, 0:64], 8), ALU.mult),
                   r_=[pk, 'tri'], w_=[aqk])
            res['AQ'] = (AQ, aqk)
            AS = int(os.environ.get('ASTOP', '9'))
            if AS < 2:
                return res
            res['KT'] = transp8(ko, sc, [('ko', 0), ('ko', 1)], dst=(KTs, ('KTs', sl)))
            if AS < 3:
                return res
            if not gdn:
                VT, vtk = VTs, ('VTs', sl)
                for j in range(2):
                    t0 = PAD + b * 512 + (2 * sc + j) * 64
                    ps, pk = PS()
                    op('pe', *[I('matmul', ps[0:64, 0:256], hT[:, kc, t0:t0 + 64], wits[:, kc, 0:256], start=(kc == 0), stop=(kc == 7))
                               for kc in range(8)], r_=['hT', 'wits'], w_=[pk])
                    op('act', I('activation', VT[0:64, j * 256:(j + 1) * 256], ps[0:64, 0:256], AF.Copy), r_=[pk], w_=[vtk])
                res['VT'] = (VT, vtk)
                return res
            kwr = [('kbt', 0), ('kbt', 1), ('kd', 0), ('kd', 1)]
            psN, pkN = PS()
            mm8(psN, pkN, fm(kbt, sc), fm(kd, sc), kwr, split=True)
            psA, pkA = PS()
            mm8(psA, pkA, fm(kd, sc), fm(kbt, sc), kwr, split=True)
            X, xk = SB()
            A_, ak = SB()
            P_, pk_ = SB()
            op('dve', I('tensor_tensor', X[0:64, :], psN[0:64, :], Dm[0:64, :], ALU.mult), r_=[pkN, dmk], w_=[xk])
            op('dve', I('tensor_tensor', A_[0:64, :], psA[0:64, :], DTu[0:64, :], ALU.mult), r_=[pkA, dtuk], w_=[ak])
            op('pool', I('tensor_tensor', v3(P_[0:64, :], 64), v3(A_[0:64, :], 64), bcm(eye[:, :], 8), ALU.add), r_=[ak, 'eye'], w_=[pk_])
            for lev in range(1, 6):
                psX, pkX = PS()
                mm8(psX, pkX, tm(A_), tm(X), [ak, xk])
                Xn, xnk = SB()
                op('act', I('activation', Xn[0:64, :], psX[0:64, :], AF.Copy), r_=[pkX], w_=[xnk])
                if lev < 5:
                    psA2, pkA2 = PS()
                    mm8(psA2, pkA2, tm(X), tm(A_), [ak, xk])
                    An, ank = SB()
                    op('dve', I('tensor_copy', An[0:64, :], psA2[0:64, :]), r_=[pkA2], w_=[ank])
                psP, pkP = PS()
                mm8(psP, pkP, tm(Xn), tm(P_), [xnk, pk_])
                Pn, pnk = SB()
                op('dve', I('tensor_tensor', Pn[0:64, :], psP[0:64, :], P_[0:64, :], ALU.add), r_=[pkP, pk_], w_=[pnk])
                X, xk = Xn, xnk
                if lev < 5:
                    A_, ak = An, ank
                P_, pk_ = Pn, pnk
            KW, kwk = transp8(kw, sc, [('kw', 0), ('kw', 1)])
            VB, vbk = transp8(vb, sc, [('vb', 0), ('vb', 1)])
            psW, pkW = PS()
            op('pe', *[I('matmul', psW[hgj(j, h)[2]:hgj(j, h)[2] + 64, (j * 2 + h // 2) * 64:(j * 2 + h // 2 + 1) * 64],
                         tm(KW)(j, h), tm(P_)(j, h), start=True, stop=True) for j in range(2) for h in range(4)],
               r_=[kwk, pk_], w_=[pkW])
            WT, wtk = WTs, ('WTs', sl)
            op('act', I('activation', WT[:, 0:256], psW[:, 0:256], AF.Copy), r_=[pkW], w_=[wtk])
            psU, pkU = PS()
            mm8(psU, pkU, tm(P_), tm(VB), [pk_, vbk])
            U, uk = Us2[sl], ('Us', sl)
            op('dve', I('tensor_copy', U[0:64, :], psU[0:64, :]), r_=[pkU], w_=[uk])
            res['WT'] = (WT, wtk)
            res['U'] = (U, uk)
            return res

        def B_phase(b, sc, res):
            AQ, aqk = res['AQ']
            KT, ktk = res['KT']
            for j in range(2):
                n = 2 * sc + j
                cs = n * 64
                cur = cst['n'] % 2
                cst['n'] += 1
                Sc, Sn = Sb2[cur], Sb2[1 - cur]
                sck, snk = ('Sb', cur), ('Sb', 1 - cur)
                if gdn:
                    WT, wtk = res['WT']
                    U, uk = res['U']
                    ps, pk = PS()
                    for hs in ((0, 2), (1, 3)):
                        op('pe', *[I('matmul', ps[0:64, h * 64:(h + 1) * 64],
                                     WT[(h % 2) * 64:(h % 2) * 64 + 64, (j * 2 + h // 2) * 64:(j * 2 + h // 2 + 1) * 64],
                                     Sc[(h % 2) * 64:(h % 2) * 64 + 64, h // 2, :], start=True, stop=True) for h in hs],
                           r_=[wtk, sck], w_=[pk])
                    VT, vtk = SB()
                    op('dve', I('tensor_tensor', VT[0:64, 0:256], U[0:64, j * 256:(j + 1) * 256], ps[0:64, 0:256], ALU.subtract),
                       r_=[uk, pk], w_=[vtk])
                    voff = 0
                else:
                    VT, vtk = res['VT']
                    voff = j * 256
                pu, puk = PS()
                op('pe', *[I('matmul', pu[(h % 2) * 64:(h % 2) * 64 + 64, (h // 2) * 64:(h // 2 + 1) * 64],
                             KT[0:64, (j * 4 + h) * 64:(j * 4 + h + 1) * 64], VT[0:64, voff + h * 64:voff + (h + 1) * 64],
                             start=True, stop=True) for h in range(4)], r_=[ktk, vtk], w_=[puk])
                for g_ in range(2):
                    op('dve', I('scalar_tensor_tensor', Sn[:, g_, :], S32[:, g_, :], EL[:, g_, n, :], pu[:, g_ * 64:(g_ + 1) * 64],
                                ALU.mult, ALU.add), r_=['S32', 'EL', puk], w_=[snk])
                for g_ in range(2):
                    op('dve', I('scalar_tensor_tensor', S32[:, g_, :], S32[:, g_, :], EL[:, g_, n, :], pu[:, g_ * 64:(g_ + 1) * 64],
                                ALU.mult, ALU.add), r_=['S32', 'EL', puk], w_=['S32'])
                po, pok = PS()

                def mS(h):
                    r0 = (h % 2) * 64
                    return I('matmul', po[0:64, h * 64:(h + 1) * 64], Sc[r0:r0 + 64, h // 2, :], qT[h // 2][r0:r0 + 64, cs:cs + 64],
                             start=True, stop=False)

                def mV(h):
                    return I('matmul', po[0:64, h * 64:(h + 1) * 64], VT[0:64, voff + h * 64:voff + (h + 1) * 64],
                             AQ[0:64, (j * 4 + h) * 64:(j * 4 + h + 1) * 64], start=False, stop=True)
                rr = [sck, ('qT', 0), ('qT', 1), vtk, aqk]
                op('pe', mS(0), mV(0), mS(2), mV(2), r_=rr, w_=[pok])
                for h_ in (1, 3):
                    op('pe', mS(h_), r_=rr, w_=[pok])
                    op('pe', mV(h_), r_=rr, w_=[pok])
                op('act', I('activation', OT[0:64, :, cs:cs + 64], v3(po[0:64, 0:256], 64), AF.Copy), r_=[pok], w_=['OT'])

        RS = int(os.environ.get('RSTOP', '9'))
        OVL = not gdn
        gn_, zn_ = {'gdn': 'gog', 'hg': 'hog', 'gla': 'log'}[kind], {'gdn': 'gz', 'hg': 'hg', 'gla': 'lg'}[kind]

        def pre_seq(b):
            kb.label = 'pre'
            if kind == 'gla':
                gk_common(b)
            for g in range(2):
                (pre_gdn if gdn else pre_la)(b, g)

        def pre_par(b):
            kb.label = 'pre'
            if kind == 'gla':
                gk_common(b)
            two_streams(lambda: (pre_gdn if gdn else pre_la)(b, 0), lambda: (pre_gdn if gdn else pre_la)(b, 1))

        def out_seq(b):
            kb.label = 'out'
            outnorm_gate(l, b, gn_, zn_, OT, seq=True)
            wout_apply(b)

        pre_par(0)
        for b in range(NB):
            if RS < 2:
                continue
            for sc0 in (0, 2):
                caps, ress = [], []
                for i_ in range(2):
                    STREAM['s'] = i_
                    kb.label = 'A%d' % (sc0 + i_)
                    caps.append(kb.capture(lambda: ress.append(A_phase(b, sc0 + i_))))
                STREAM['s'] = None
                kb.commit_interleaved(caps)
                if RS >= 3:
                    for i_ in range(2):
                        kb.label = 'B%d' % (sc0 + i_)
                        B_phase(b, sc0 + i_, ress[i_])
            kb.label = 'out'
            if RS < 4:
                continue
            if b + 1 < NB and OVL:
                two_streams(lambda: out_seq(b), lambda: pre_seq(b + 1))
            else:
                outnorm_gate(l, b, gn_, zn_, OT)
                wout_apply(b)
                if b + 1 < NB:
                    pre_par(b + 1)

    def ffn(l):
        GT = kb.sb("GT", [128, 22, 1024], BF16)
        wu = [kb.sb("wu%d" % i, [128, 8, 128], BF16) for i in range(4)]
        wd = [kb.sb("wd%d" % i, [128, 22, 128], BF16) for i in range(2)]
        passes = []
        t = 0
        while t < S:
            e = min(S, t + 1024)
            passes.append((t, e))
            t = e
        wi = 0
        for (p0, p1) in passes:
            blocks = []
            t = p0
            while t < p1:
                n = min(510, p1 - t)
                blocks.append((t, n))
                t += n
            t = p0
            while t < p1:
                n = min(512, p1 - t)
                rmsnorm('n2g', t, n)
                t += n
            for j in range(22):
                wts = []
                for tt in range(2):
                    i = wi % 4
                    wi += 1
                    kb.dma('sp', wu[i][:], wb['wup'][l, 2 * j + tt], r_=[('wup', l)], w_=[('wu', i)])
                    wts.append((wu[i], ('wu', i)))
                for (t0, n) in blocks:
                    ys = []
                    for tt in range(2):
                        c = 2 * j + tt
                        ps, pk = PS()
                        op('pe', *[I('matmul', ps[:, 0:n + 2], wts[tt][0][:, kc, :], hT[:, kc, PAD + t0 - 2:PAD + t0 + n],
                                     start=(kc == 0), stop=(kc == 7)) for kc in range(8)], r_=[wts[tt][1], 'hT'], w_=[pk])
                        y, yk = SF()
                        op('act', I('activation', y[:, 0:n], ps[:, 2:n + 2], AF.Identity, bias=pc('fcb', c), scale=pc('fcw', c * 3 + 2)),
                           r_=[pk, 'pp'], w_=[yk])
                        op('dve', I('scalar_tensor_tensor', y[:, 0:n], ps[:, 1:n + 1], pc('fcw', c * 3 + 1), y[:, 0:n], ALU.mult, ALU.add),
                           r_=[pk, yk, 'pp'], w_=[yk])
                        op('dve', I('scalar_tensor_tensor', y[:, 0:n], ps[:, 0:n], pc('fcw', c * 3), y[:, 0:n], ALU.mult, ALU.add),
                           r_=[pk, yk, 'pp'], w_=[yk])
                        ys.append((y, yk))
                    sg, sgk = SF()
                    op('act', I('activation', sg[:, 0:n], ys[0][0][:, 0:n], AF.Silu), r_=[ys[0][1]], w_=[sgk])
                    op('pool', I('tensor_tensor', GT[:, j, t0 - p0:t0 - p0 + n], sg[:, 0:n], ys[1][0][:, 0:n], ALU.mult),
                       r_=[sgk, ys[1][1]], w_=[('GT', j)])
            for oc in range(8):
                i = oc % 2
                kb.dma('sp', wd[i][:], wb['wdn'][l, oc], r_=[('wdn', l)], w_=[('wd', i)])
                for (t0, n) in blocks:
                    ps, pk = PS()
                    op('pe', *[I('matmul', ps[:, 0:n], wd[i][:, kc, :], GT[:, kc, t0 - p0:t0 - p0 + n], start=(kc == 0), stop=(kc == 21))
                               for kc in range(22)], r_=[('wd', i)] + [('GT', kc) for kc in range(22)], w_=[pk])
                    op('dve', I('tensor_tensor', xT[:, oc, t0:t0 + n], ps[:, 0:n], xT[:, oc, t0:t0 + n], ALU.add),
                       r_=[pk, ('x', oc)], w_=[('x', oc)])

    def layer_setup(l):
        kb.dma('sp', ppt[:], dr['pp'][l], w_=['pp'])
        op('act', I('activation', der[:, 0:2], ppt[:, PPC['galog']:PPC['galog'] + 2], AF.Exp), r_=['pp'], w_=['der'])
        op('dve', I('tensor_scalar', der[:, 0:2], der[:, 0:2], -1.0, None, ALU.mult), r_=['der'], w_=['der'])
        if l == 0:
            op('pool', I('memset', der[:, 2:4], 1.0), w_=['der'])
        else:
            op('dve', I('tensor_tensor', der[:, 2:4], ppt[:, PPC['hlb1']:PPC['hlb1'] + 2], ppt[:, PPC['hlb0']:PPC['hlb0'] + 2],
                        ALU.subtract), r_=['pp'], w_=['der'])
            op('act', I('activation', der[:, 2:4], der[:, 2:4], AF.Exp), r_=['der'], w_=['der'])
            op('dve', I('tensor_scalar', der[:, 2:4], der[:, 2:4], 1.0, None, ALU.add), r_=['der'], w_=['der'])
            op('dve', I('reciprocal', der[:, 2:4], der[:, 2:4]), r_=['der'], w_=['der'])
        op('dve', I('tensor_scalar', der[:, 4:6], ppt[:, PPC['lbgk']:PPC['lbgk'] + 2], -1.0, None, ALU.mult), r_=['pp'], w_=['der'])
        op('dve', I('tensor_scalar', der[:, 8:12], ppt[:, PPC['fbf']:PPC['fbf'] + 4], -1.0, None, ALU.mult), r_=['pp'], w_=['der'])
        op('pool', I('memset', der[:, 12:13], -float(np.log(8.0))), w_=['der'])
        op('pool', I('memset', der[:, 13:14], 1.0), w_=['der'])

    for n in range(NSEQ):
        for c in range(8):
            kb.dma('sp', xT[:, c, :], dr['xT'][n, :, c, :], w_=[('x', c)])
        for l in range(L):
            layer_setup(l)
            with Phase():
                alloc_mixer_weights()
                for b in range(NB):
                    rmsnorm('n1g', b * 512, 512)
                for mi, fn in enumerate((lambda: fox(l), lambda: recurrent(l, 'gdn'), lambda: recurrent(l, 'hg'),
                                         lambda: recurrent(l, 'gla'))):
                    if mi in MIXERS:
                        with Phase():
                            fn()
            if FFN:
                with Phase():
                    ffn(l)
        for c in range(8):
            kb.dma('sp', yT[n, :, c, :], xT[:, c, :], r_=[('x', c)], out_final=True)
    kb.emit()
    nc._kb_labels = kb.labels
    return nc


_CACHE = {}


def kernel(**inputs):
    x = np.asarray(inputs['x'], np.float32)
    B, S, _ = x.shape
    L = np.asarray(inputs['w_in']).shape[0]
    nseq = B // NCORES
    packed = pack_weights(inputs)
    key = (S, nseq, L)
    if key not in _CACHE:
        _CACHE[key] = build_program(S, nseq, L)
    nc = _CACHE[key]
    in_maps = []
    for c in range(NCORES):
        xs = x[c * nseq:(c + 1) * nseq]
        xt = np.ascontiguousarray(xs.reshape(nseq, S, 8, 128).transpose(0, 3, 2, 1))
        m = {"xT": xt}
        m.update(packed)
        in_maps.append(m)
    res = run_bass_kernel_spmd(nc, in_maps, core_ids=list(range(NCORES)))
    out = np.empty((B, S, D), np.float32)
    for c in range(NCORES):
        yt = np.asarray(res.results[c]["yT"])
        out[c * nseq:(c + 1) * nseq] = yt.transpose(0, 3, 2, 1).reshape(nseq, S, D)
    return out
```

```python
import contextlib
import os
import numpy as np
import concourse.bass as bass
import concourse.mybir as mybir
from concourse.bass_utils import run_bass_kernel_spmd

F32 = mybir.dt.float32
BF16 = mybir.dt.bfloat16
AF = mybir.ActivationFunctionType
ALU = mybir.AluOpType

ENGS = ('pe', 'act', 'dve', 'pool', 'sp')


class KB:
    NDMASEM = 16

    def __init__(self, nc):
        self.nc = nc
        self.stack = contextlib.ExitStack()
        self.ops = {e: [] for e in ENGS}
        self.cnt = {e: 0 for e in ENGS}
        self.known = {e: {} for e in ENGS}
        self.writers = {}
        self.readers = {}
        self.sem = {}
        for e in ENGS:
            self.sem[e] = self.stack.enter_context(nc.semaphore("s_" + e))
        self.dsem = [self.stack.enter_context(nc.semaphore("d%d" % i)) for i in range(self.NDMASEM)]
        self.dcnt = [0] * self.NDMASEM
        self.dnext = {}
        self.out_waits = []
        self.labels = {} if os.environ.get('KB_LABELS') else None
        self.pending = {e: {} for e in ENGS}

    def sb(self, name, shape, dt):
        self.uid = getattr(self, 'uid', 0) + 1
        return self.stack.enter_context(self.nc.sbuf_tensor("%s_%d" % (name, self.uid), list(shape), dt))

    def ps(self, name, shape, dt):
        return self.stack.enter_context(self.nc.psum_tensor(name, list(shape), dt))

    def barrier(self):
        snap = {e: c for e, c in self.cnt.items() if c > 0}
        for i, c in enumerate(self.dcnt):
            if c > 0:
                snap[('d', i)] = c
        for e in ENGS:
            for k, v in snap.items():
                if k != e and self.pending[e].get(k, 0) < v:
                    self.pending[e][k] = v

    def _deps(self, eng, r_, w_):
        deps = dict(self.pending[eng])
        self.pending[eng] = {}
        for t in r_:
            for k, v in self.writers.get(t, {}).items():
                if deps.get(k, 0) < v:
                    deps[k] = v
            if isinstance(t, tuple) and t[0] in ('ps', 'pt'):
                for k, v in self.readers.get(t, {}).items():
                    if k != eng and deps.get(k, 0) < v:
                        deps[k] = v
        for t in w_:
            for d in (self.writers.get(t, {}), self.readers.get(t, {})):
                for k, v in d.items():
                    if deps.get(k, 0) < v:
                        deps[k] = v
        waits = []
        kn = self.known[eng]
        for k, v in deps.items():
            if kn.get(k, 0) < v:
                kn[k] = v
                waits.append((k, v))
        return waits

    def capture(self, fn):
        self._cap = []
        try:
            fn()
        finally:
            c, self._cap = self._cap, None
        return c

    def commit_interleaved(self, streams):
        n = max(len(x) for x in streams)
        for i in range(n):
            for x in streams:
                if i < len(x):
                    kind, a, k = x[i]
                    lab = k.pop('_label', None)
                    if lab is not None:
                        self.label = lab
                    (self.op if kind == 'op' else self.dma)(*a, **k)

    def op(self, eng, *fns, r_=(), w_=()):
        if getattr(self, '_cap', None) is not None:
            self._cap.append(('op', (eng,) + tuple(fns), dict(r_=r_, w_=w_, _label=getattr(self, 'label', ''))))
            return 0
        waits = self._deps(eng, r_, w_)
        self.cnt[eng] += 1
        idx = self.cnt[eng]
        self.ops[eng].append((waits, fns, ('eng', eng, getattr(self, 'label', ''))))
        for t in r_:
            self.readers.setdefault(t, {})[eng] = idx
        for t in w_:
            self.writers.setdefault(t, {})[eng] = idx
        return idx

    def dma(self, eng, out, in_, r_=(), w_=(), out_final=False, **kw):
        if getattr(self, '_cap', None) is not None:
            self._cap.append(('dma', (eng, out, in_), dict(r_=r_, w_=w_, out_final=out_final, **kw)))
            return
        lo, hi = (0, 4) if eng == 'pool' else (4, self.NDMASEM)
        si = self.dnext.get(eng, lo)
        self.dnext[eng] = lo + (si + 1 - lo) % (hi - lo)
        key = ('d', si)
        waits = self._deps(eng, r_, w_)
        if self.dcnt[si] > 0 and self.known[eng].get(key, 0) < self.dcnt[si]:
            self.known[eng][key] = self.dcnt[si]
            waits.append((key, self.dcnt[si]))
        self.dcnt[si] += 1
        val = self.dcnt[si]
        self.ops[eng].append((waits, (('dma_start', (), dict(out=out, in_=in_, **kw)),), ('dma', si)))
        for t in r_:
            self.readers.setdefault(t, {})[key] = val
        for t in w_:
            self.writers.setdefault(t, {})[key] = val
        if out_final:
            self.out_waits.append((key, val))

    def _semof(self, key):
        if isinstance(key, tuple):
            return self.dsem[key[1]], 16
        return self.sem[key], 1

    def emit(self):
        nc = self.nc
        fin = []
        for key, val in self.out_waits:
            fin.append((key, val))
        with nc.Block() as block:
            def body(engname):
                def f(e):
                    for waits, fns, kind in self.ops[engname]:
                        for k, v in waits:
                            s, mult = self._semof(k)
                            e.wait_ge(s, v * mult)
                        last = None
                        for (nm, a, k) in fns:
                            last = getattr(e, nm)(*a, **k)
                            if self.labels is not None and len(kind) > 2:
                                try:
                                    self.labels[last.ins.name] = kind[2]
                                except Exception:
                                    pass
                        if kind[0] == 'eng':
                            last.then_inc(self.sem[engname], 1)
                        else:
                            last.then_inc(self.dsem[kind[1]], 16)
                    if engname == 'sp':
                        for k, v in fin:
                            s, mult = self._semof(k)
                            e.wait_ge(s, v * mult)
                return f
            block.tensor(body('pe'))
            block.scalar(body('act'))
            block.vector(body('dve'))
            block.gpsimd(body('pool'))
            block.sync(body('sp'))
        self.stack.close()


D = 1024
DFF = 2816
PAD = 4
EPS = 1e-6
NCORES = 8

FCH = (['fq%d' % h for h in range(4)] + ['fk%d' % h for h in range(4)] +
       ['gq0', 'gq1', 'gk0', 'gk1', 'gv0', 'gv1', 'gb0', 'gb1', 'ga0', 'ga1'] + ['gz%d' % h for h in range(4)] +
       ['hq0', 'hq1', 'hf0', 'hf1'] + ['hg%d' % h for h in range(4)] +
       ['lq0', 'lq1', 'lk0', 'lk1', 'lgk'] + ['lg%d' % h for h in range(4)])
FIDX = {n: i for i, n in enumerate(FCH)}
NCF = len(FCH)

PPC = {}
_o = 0
for _n, _w in [('n1g', 8), ('n2g', 8), ('fqg', 1), ('fkg', 1), ('fog', 1), ('fbf', 4), ('gcw', 24), ('galog', 2),
               ('gdt', 2), ('gog', 1), ('hlb0', 2), ('hlb1', 2), ('hog', 1), ('lbgk', 2), ('log', 1),
               ('fcw', 132), ('fcb', 44)]:
    PPC[_n] = _o
    _o += _w
NPP = _o

OFF = {}
_o = 0
for _n, _w in [('fox_qkv', 768), ('fox_f', 4), ('gdn_qkv', 768), ('gdn_b', 4), ('gdn_a', 4), ('gdn_z', 256),
               ('hg_q', 256), ('hg_f', 256), ('hg_i', 256), ('hg_g', 256), ('gla_qk', 256), ('gla_v', 256),
               ('gla_gk', 16), ('gla_g', 256)]:
    OFF[_n] = _o
    _o += _w


def _gla_pad_cols(base, g):
    idx = -np.ones(128, np.int64)
    idx[0:32] = base + (2 * g) * 32 + np.arange(32)
    idx[64:96] = base + (2 * g + 1) * 32 + np.arange(32)
    return idx


def pack_weights(inp):
    L = inp['w_in'].shape[0]
    wif = np.zeros((L, NCF, 128, 8, 128), np.float32)
    wit = np.zeros((L, 128, 8, 768), np.float32)
    wo = np.zeros((L, 4, 64, 4, 1024), np.float32)
    wup = np.zeros((L, 44, 128, 8, 128), np.float32)
    wdn = np.zeros((L, 8, 128, 22, 128), np.float32)
    wgk = np.zeros((L, 16, 256), np.float32)
    pp = np.zeros((L, 128, NPP), np.float32)
    for l in range(L):
        W = np.asarray(inp['w_in'][l])
        Wk = W.reshape(8, 128, -1).transpose(1, 0, 2)

        def put(name, cols):
            cols = np.asarray(cols)
            m = len(cols)
            ok = cols >= 0
            wif[l, FIDX[name], :, :, np.nonzero(ok)[0]] = Wk[:, :, cols[ok]].transpose(2, 0, 1)

        for h in range(4):
            fcol = [OFF['fox_f'] + h] * 4
            put('fq%d' % h, list(OFF['fox_qkv'] + h * 64 + np.arange(64)) + fcol)
            put('fk%d' % h, list(OFF['fox_qkv'] + 256 + h * 64 + np.arange(64)) + fcol)
            put('gz%d' % h, OFF['gdn_z'] + h * 64 + np.arange(64))
            put('hg%d' % h, OFF['hg_g'] + h * 64 + np.arange(64))
            put('lg%d' % h, OFF['gla_g'] + h * 64 + np.arange(64))
        for g in range(2):
            put('gq%d' % g, OFF['gdn_qkv'] + g * 128 + np.arange(128))
            put('gk%d' % g, OFF['gdn_qkv'] + 256 + g * 128 + np.arange(128))
            put('gv%d' % g, OFF['gdn_qkv'] + 512 + g * 128 + np.arange(128))
            put('gb%d' % g, [OFF['gdn_b'] + 2 * g] * 64 + [OFF['gdn_b'] + 2 * g + 1] * 64)
            put('ga%d' % g, [OFF['gdn_a'] + 2 * g] * 64 + [OFF['gdn_a'] + 2 * g + 1] * 64)
            put('hq%d' % g, OFF['hg_q'] + g * 128 + np.arange(128))
            put('hf%d' % g, OFF['hg_f'] + g * 128 + np.arange(128))
            put('lq%d' % g, _gla_pad_cols(OFF['gla_qk'], g))
            put('lk%d' % g, _gla_pad_cols(OFF['gla_qk'] + 128, g))
        put('lgk', OFF['gla_gk'] + np.arange(16))
        wit[l, :, :, 0:256] = Wk[:, :, OFF['fox_qkv'] + 512:OFF['fox_qkv'] + 768]
        wit[l, :, :, 256:512] = Wk[:, :, OFF['hg_i']:OFF['hg_i'] + 256]
        wit[l, :, :, 512:768] = Wk[:, :, OFF['gla_v']:OFF['gla_v'] + 256]
        Wo = np.asarray(inp['w_out'][l])
        wo[l] = Wo.reshape(4, 4, 64, 1024).transpose(0, 2, 1, 3)
        Wu = np.asarray(inp['w_up'][l]).reshape(8, 128, 2 * DFF).transpose(1, 0, 2)
        for j in range(22):
            wup[l, 2 * j] = Wu[:, :, j * 128:(j + 1) * 128]
            wup[l, 2 * j + 1] = Wu[:, :, DFF + j * 128:DFF + (j + 1) * 128]
        Wd = np.asarray(inp['w_down'][l]).reshape(22, 128, 1024).transpose(1, 0, 2)
        for oc in range(8):
            wdn[l, oc] = Wd[:, :, oc * 128:(oc + 1) * 128]
        gw = np.asarray(inp['gla_w_gk'][l])
        for g in range(2):
            idx = _gla_pad_cols(0, g)
            ok = idx >= 0
            wgk[l, :, g * 128 + np.nonzero(ok)[0]] = gw[:, idx[ok]].T
        P = pp[l]
        P[:, PPC['n1g']:PPC['n1g'] + 8] = np.asarray(inp['norm1_g'][l]).reshape(8, 128).T
        P[:, PPC['n2g']:PPC['n2g'] + 8] = np.asarray(inp['norm2_g'][l]).reshape(8, 128).T
        for nm, key in [('fqg', 'fox_qn_g'), ('fkg', 'fox_kn_g'), ('fog', 'fox_on_g'), ('gog', 'gdn_on_g'),
                        ('hog', 'hg_on_g'), ('log', 'gla_on_g')]:
            v = np.asarray(inp[key][l])
            P[0:64, PPC[nm]] = v
            P[64:128, PPC[nm]] = v
        for h in range(4):
            P[64:68, PPC['fbf'] + h] = np.asarray(inp['fox_b_f'][l])[h]
        cw = np.asarray(inp['gdn_conv_w'][l])
        for c in range(6):
            P[:, PPC['gcw'] + c * 4:PPC['gcw'] + c * 4 + 4] = cw[:, c * 128:(c + 1) * 128].T
        for g in range(2):
            for nm, key in [('galog', 'gdn_a_log'), ('gdt', 'gdn_dt_bias')]:
                v = np.asarray(inp[key][l])
                P[0:64, PPC[nm] + g] = v[2 * g]
                P[64:128, PPC[nm] + g] = v[2 * g + 1]
            P[:, PPC['hlb0'] + g] = np.asarray(inp['hg_lb'][0])[g * 128:(g + 1) * 128]
            P[:, PPC['hlb1'] + g] = np.asarray(inp['hg_lb'][1])[g * 128:(g + 1) * 128]
            idx = _gla_pad_cols(0, g)
            ok = idx >= 0
            P[np.nonzero(ok)[0], PPC['lbgk'] + g] = np.asarray(inp['gla_b_gk'][l])[idx[ok]]
        fw = np.asarray(inp['ffn_conv_w'][l])
        fb = np.asarray(inp['ffn_conv_b'][l])
        for j in range(22):
            for t, base in enumerate((j * 128, DFF + j * 128)):
                c = 2 * j + t
                P[:, PPC['fcw'] + c * 3:PPC['fcw'] + c * 3 + 3] = fw[:, base:base + 128].T
                P[:, PPC['fcb'] + c] = fb[base:base + 128]
    return dict(wif=wif, wit=wit, wo=wo, wup=wup, wdn=wdn, wgk=wgk, pp=pp)


def I(name, *a, **k):
    return (name, a, k)


def bcl(ap, n):
    return bass.AP(ap.tensor, ap.offset, [list(x) for x in ap.ap[:-1]] + [[0, n]])


def bcm(ap, n):
    a = [list(x) for x in ap.ap]
    return bass.AP(ap.tensor, ap.offset, [a[0], [0, n]] + a[1:])


def v3(ap, b):
    return ap.rearrange("p (a b) -> p a b", b=b)


def build_program(S, NSEQ, L, MIXERS=(0, 1, 2, 3), FFN=True):
    nc = bass.Bass("TRN2", target_bir_lowering=False)
    NB = S // 512
    dr = {}
    dr['xT'] = nc.dram_tensor("xT", [NSEQ, 128, 8, S], F32, kind="ExternalInput").ap()
    shapes = dict(wif=[L, NCF, 128, 8, 128], wit=[L, 128, 8, 768], wo=[L, 4, 64, 4, 1024],
                  wup=[L, 44, 128, 8, 128], wdn=[L, 8, 128, 22, 128], wgk=[L, 16, 256])
    wb = {}
    for k, sh in shapes.items():
        dr[k] = nc.dram_tensor(k, sh, F32, kind="ExternalInput").ap()
        wb[k] = nc.dram_tensor(k + "_b", sh, BF16, kind="Internal").ap()
    dr['pp'] = nc.dram_tensor("pp", [L, 128, NPP], F32, kind="ExternalInput").ap()
    yT = nc.dram_tensor("yT", [NSEQ, 128, 8, S], F32, kind="ExternalOutput").ap()

    kb = KB(nc)
    op = kb.op

    def flat(ap):
        names = " ".join("abcdefg"[:len(ap.shape)])
        f = ap.rearrange("%s -> (%s)" % (names, names))
        n = f.shape[0]
        fdim = 2048 if n % 2048 == 0 else 256
        return f.rearrange("(r f) -> r f", f=fdim)

    for l in range(L):
        for k in ('wif', 'wit', 'wo', 'wgk', 'wup', 'wdn'):
            src = flat(dr[k][l])
            dst = flat(wb[k][l])
            R = src.shape[0]
            for r0 in range(0, R, 4096):
                r1 = min(R, r0 + 4096)
                kb.dma('pool', dst[r0:r1], src[r0:r1], w_=[(k, l)])

    xT = kb.sb("xT_sb", [128, 8, S], F32)
    hT = kb.sb("hT_sb", [128, 8, PAD + S], BF16)
    MX = kb.sb("MX", [64, 4, 512], BF16)
    ppt = kb.sb("ppt", [128, NPP], F32)
    ident = kb.sb("ident", [128, 128], BF16)
    tri = kb.sb("tri", [128, 128], BF16)
    negU = kb.sb("negU", [64, 64], F32)
    negL = kb.sb("negL", [64, 64], F32)
    eye = kb.sb("eye", [64, 64], F32)
    ones = kb.sb("ones", [128, 128], BF16)
    bd = kb.sb("bd", [128, 128], BF16)
    o64 = kb.sb("o64", [64, 64], BF16)
    wst = kb.sb("wst", [96, 64], BF16)
    cm = kb.sb("cm", [128, 512], F32)
    onesf = kb.sb("onesf", [128, 512], F32)
    epsc = kb.sb("epsc", [128, 1], F32)
    aug = kb.sb("aug", [128, 6], F32)
    der = kb.sb("der", [128, 16], F32)
    zc = kb.sb("zc", [128, 4], F32)

    def sel(t, ap_, cmp, fill, base, pat, cmul=1):
        op('pool', I('affine_select', out=ap_, in_=ap_, pattern=pat, compare_op=cmp, fill=fill, base=base,
                     channel_multiplier=cmul), r_=[t], w_=[t])
    for t_, tl, v in [('ident', ident, 0.0), ('tri', tri, 0.0), ('negU', negU, 0.0), ('negL', negL, 0.0),
                      ('eye', eye, 0.0), ('ones', ones, 1.0), ('bd', bd, 1.0), ('o64', o64, 1.0 / 64),
                      ('wst', wst, 1.0 / 64), ('cm', cm, 1.0), ('onesf', onesf, 1.0), ('epsc', epsc, EPS),
                      ('aug', aug, 0.0), ('hT', hT, 0.0)]:
        op('pool', I('memset', tl[:], v), w_=[t_])
    sel('ident', ident[:], ALU.not_equal, 1.0, 0, [[-1, 128]])
    sel('tri', tri[:], ALU.is_gt, 1.0, 0, [[-1, 128]])
    sel('negU', negU[:], ALU.is_ge, -1.0, 0, [[-1, 64]])
    sel('negL', negL[:], ALU.is_ge, -1.0, 0, [[1, 64]], cmul=-1)
    sel('eye', eye[:], ALU.not_equal, 1.0, 0, [[-1, 64]])
    op('pool', I('memset', bd[0:64, 64:128], 0.0), w_=['bd'])
    op('pool', I('memset', bd[64:128, 0:64], 0.0), w_=['bd'])
    op('pool', I('memset', wst[64:96, :], 0.0), w_=['wst'])
    op('pool', I('memset', wst[64:65, :], EPS), w_=['wst'])
    op('pool', I('memset', v3(cm[:], 64)[:, :, 0:1], 0.0), w_=['cm'])
    op('pool', I('memset', zc[:], 0.0), w_=['zc'])
    for r0_ in (0, 64):
        sel('zc', zc[:, 0:1], ALU.not_equal, -1.0, -r0_, [[0, 1]])
        sel('zc', zc[:, 1:2], ALU.not_equal, 1.0, -(r0_ + 1), [[0, 1]])
        sel('zc', zc[:, 2:3], ALU.not_equal, 1.0, -(r0_ + 1), [[0, 1]])
        sel('zc', zc[:, 3:4], ALU.not_equal, 1.0, -r0_, [[0, 1]])
    sel('aug', aug[:, 0:1], ALU.not_equal, 1.0, -64, [[0, 1]])
    sel('aug', aug[:, 1:2], ALU.not_equal, 1.0, -65, [[0, 1]])
    sel('aug', aug[:, 2:3], ALU.is_gt, 1.0, 66, [[0, 1]], cmul=-1)
    sel('aug', aug[:, 3:4], ALU.not_equal, -1.0, -66, [[0, 1]])
    sel('aug', aug[:, 4:5], ALU.not_equal, -1.0, -67, [[0, 1]])
    sel('aug', aug[:, 5:6], ALU.is_ge, 1.0, -66, [[0, 1]])

    NPS = 7
    psb = [kb.ps("ps%d" % i, [128, 512], F32) for i in range(NPS)]
    ptb = kb.ps("ptb", [128, 1024], BF16)
    st = {'ps': 0, 'pt': 0, 'wf': 0}

    STREAM = {'s': None}

    def _rot(key, n):
        sid = STREAM['s']
        if sid is None:
            i = st.get(key, 0)
            st[key] = (i + 1) % n
            return i
        lo = 0 if sid == 0 else (n + 1) // 2
        hi = (n + 1) // 2 if sid == 0 else n
        k2 = (key, sid)
        i = st.get(k2, lo)
        st[k2] = lo + (i + 1 - lo) % (hi - lo)
        return i

    def PS():
        i = _rot('ps', NPS - 2)
        return psb[i], ('ps', i)

    def PSA():
        if STREAM['s'] is not None:
            i = NPS - 2 + STREAM['s']
        else:
            i = NPS - 2 + st.get('psa', 0)
            st['psa'] = (st.get('psa', 0) + 1) % 2
        return psb[i], ('ps', i)

    def two_streams(fa, fb):
        caps = []
        for sid, f in enumerate((fa, fb)):
            STREAM['s'] = sid
            caps.append(kb.capture(f))
        STREAM['s'] = None
        kb.commit_interleaved(caps)

    def PT():
        i = st['pt']
        st['pt'] = (i + 1) % 2
        return ptb[:, i * 512:(i + 1) * 512], ('pt', 0)

    NSF, NSB = 10, 12
    sf = [kb.sb("sf%d" % i, [128, 512], F32) for i in range(NSF)]
    sbf = [kb.sb("sbf%d" % i, [128, 512], BF16) for i in range(NSB)]
    stc = {'f': 0, 'b': 0}

    def SF():
        i = _rot('sf', NSF)
        return sf[i], ('sf', i)

    def SB():
        i = _rot('sb', NSB)
        return sbf[i], ('sbf', i)

    def pc(name, j=0, r0=0, r1=128):
        return ppt[r0:r1, PPC[name] + j:PPC[name] + j + 1]

    class Phase:
        def __enter__(self):
            kb.barrier()
            self.prev = kb.stack
            kb.stack = contextlib.ExitStack()
            return self

        def __exit__(self, *a):
            kb.barrier()
            kb.stack.close()
            kb.stack = self.prev

    W = {}

    def alloc_mixer_weights():
        W['wfs'] = [kb.sb("wf%d" % i, [128, 8, 128], BF16) for i in range(4)]
        W['wits'] = kb.sb("wits", [128, 8, 256], BF16)
        W['wos'] = kb.sb("wos", [64, 4, 1024], BF16)
        W['wgks'] = kb.sb("wgks", [16, 256], BF16)

    def load_wf(l, name):
        i = _rot('wf', 4)
        kb.dma('sp', W['wfs'][i][:], wb['wif'][l, FIDX[name]], r_=[('wif', l)], w_=[('wf', i)])
        return W['wfs'][i], ('wf', i)

    def proj_f(l, name, M, c0, n):
        wt, wk = load_wf(l, name)
        ps, pk = PS()
        op('pe', *[I('matmul', ps[0:M, 0:n], wt[:, kc, 0:M], hT[:, kc, c0:c0 + n], start=(kc == 0), stop=(kc == 7))
                   for kc in range(8)], r_=[wk, 'hT'], w_=[pk])
        return ps, pk

    def rmsnorm(gname, t0, n):
        ps, pk = PS()
        for c in range(8):
            sq, sk = SB()
            op('act', I('activation', sq[:, 0:n], xT[:, c, t0:t0 + n], AF.Square), r_=[('x', c)], w_=[sk])
            op('pe', I('matmul', ps[:, 0:n], ones[:, :], sq[:, 0:n], start=(c == 0), stop=(c == 7)), r_=[sk, 'ones'], w_=[pk])
        rs, rk = SF()
        op('act', I('activation', rs[:, 0:n], ps[:, 0:n], AF.Ln, bias=epsc[:, 0:1], scale=1.0 / D), r_=[pk, 'epsc'], w_=[rk])
        op('act', I('activation', rs[:, 0:n], rs[:, 0:n], AF.Exp, scale=-0.5), r_=[rk], w_=[rk])
        for c in range(8):
            op('dve', I('scalar_tensor_tensor', hT[:, c, PAD + t0:PAD + t0 + n], xT[:, c, t0:t0 + n], pc(gname, c),
                        rs[:, 0:n], ALU.mult, ALU.mult), r_=[('x', c), rk, 'pp'], w_=['hT'])

    def wout_apply(b):
        wos = W['wos']
        for oc in range(8):
            ps, pk = PS()
            op('pe', *[I('matmul', ps[:, :], wos[0:64, h, oc * 128:(oc + 1) * 128], MX[0:64, h, :],
                         start=(h == 0), stop=(h == 3)) for h in range(4)], r_=['wos', 'MX'], w_=[pk])
            op('dve', I('tensor_tensor', xT[:, oc, b * 512:(b + 1) * 512], ps[:, :], xT[:, oc, b * 512:(b + 1) * 512],
                        ALU.add), r_=[pk, ('x', oc)], w_=[('x', oc)])

    def rstd_from(ps, pk, rows, n, bias_ap, scale=1.0, ebias=None, r0=0):
        rs, rk = SF()
        kw = {} if bias_ap is None else dict(bias=bias_ap)
        op('act', I('activation', rs[r0:r0 + rows, 0:n], ps[r0:r0 + rows, 0:n], AF.Ln, scale=scale, **kw),
           r_=[pk, 'epsc'], w_=[rk])
        kw = {} if ebias is None else dict(bias=ebias)
        op('act', I('activation', rs[r0:r0 + rows, 0:n], rs[r0:r0 + rows, 0:n], AF.Exp, scale=-0.5, **kw),
           r_=[rk, 'der'], w_=[rk])
        return rs, rk

    def outnorm_gate(l, b, gname, zname, OT, seq=False):
        def on_head(h):
            sq, sk = SB()
            op('act', I('activation', sq[0:64, :], OT[0:64, h, :], AF.Square), r_=['OT'], w_=[sk])
            p2, pk2 = PS()
            op('pe', I('matmul', p2[0:64, :], o64[:, :], sq[0:64, :], start=True, stop=True), r_=[sk, 'o64'], w_=[pk2])
            rs, rk = rstd_from(p2, pk2, 64, 512, epsc[0:64, 0:1])
            zp, zk = proj_f(l, '%s%d' % (zname, h), 64, PAD + b * 512, 512)
            gt, gk = SF()
            op('act', I('activation', gt[0:64, :], zp[0:64, :], AF.Silu), r_=[zk], w_=[gk])
            t, tk = SF()
            op('dve', I('scalar_tensor_tensor', t[0:64, :], OT[0:64, h, :], pc(gname, 0, 0, 64), rs[0:64, :],
                        ALU.mult, ALU.mult), r_=['OT', rk, 'pp'], w_=[tk])
            op('dve', I('tensor_tensor', MX[0:64, h, :], t[0:64, :], gt[0:64, :], ALU.mult), r_=[tk, gk], w_=['MX'])
        if seq:
            for h in range(4):
                on_head(h)
        else:
            two_streams(lambda: [on_head(h) for h in (0, 1)], lambda: [on_head(h) for h in (2, 3)])

    def fox(l):
        NT = S // 128
        KA = kb.sb("KA", [96, 4, S], BF16)
        QA = kb.sb("QA", [96, 4, 512], BF16)
        Vp = kb.sb("Vp", [128, NT, 4, 96], BF16)
        CC = kb.sb("CC", [68, 4, 2], F32)
        wits = W['wits']
        op('pool', I('memset', Vp[:, :, :, 64:96], 0.0), w_=['Vp1'])
        op('pool', I('memset', Vp[:, :, :, 64:65], 1.0), w_=['Vp1'])
        op('pool', I('memset', KA[64:96, :, :], 0.0), w_=['KA0'] + [('KA', h_, b_) for h_ in range(4) for b_ in range(NB)])
        op('pool', I('memset', QA[64:96, :, :], 0.0), w_=['QA0'] + [('QA', h_) for h_ in range(4)])
        kb.dma('sp', wits[:], wb['wit'][l, :, :, 0:256], r_=[('wit', l)], w_=['wits'])
        kb.dma('sp', W['wos'][:], wb['wo'][l, 0], r_=[('wo', l)], w_=['wos'])
        for i in range(NT):
            ps, pk = PS()
            op('pe', *[I('matmul', ps[:, 0:256], hT[:, kc, PAD + i * 128:PAD + (i + 1) * 128], wits[:, kc, 0:256],
                         start=(kc == 0), stop=(kc == 7)) for kc in range(8)], r_=['hT', 'wits'], w_=[pk])
            op('act', I('activation', Vp[:, i, :, 0:64], v3(ps[:, 0:256], 64), AF.Copy), r_=[pk], w_=[('Vp', i)])
        for b in range(NB):
            def fox_proj(h):
                for which in ('k', 'q'):
                    ps, pk = proj_f(l, 'f%s%d' % (which, h), 68, PAD + b * 512, 512)
                    sq, sk = SB()
                    op('act', I('activation', sq[0:64, :], ps[0:64, :], AF.Square), r_=[pk], w_=[sk])
                    p2, pk2 = PS()
                    op('pe', I('matmul', p2[0:64, :], o64[:, :], sq[0:64, :], start=True, stop=True), r_=[sk, 'o64'], w_=[pk2])
                    rs, rk = rstd_from(p2, pk2, 64, 512, epsc[0:64, 0:1],
                                       ebias=(der[0:64, 12:13] if which == 'q' else None))
                    dst = QA[0:64, h, :] if which == 'q' else KA[0:64, h, b * 512:(b + 1) * 512]
                    dk = ('QA', h) if which == 'q' else ('KA', h, b)
                    op('dve', I('scalar_tensor_tensor', dst, ps[0:64, :], pc('fqg' if which == 'q' else 'fkg', 0, 0, 64),
                                rs[0:64, :], ALU.mult, ALU.mult), r_=[pk, rk, 'pp'], w_=[dk])
                    if which == 'k':
                        continue
                    t1, tk1 = SF()
                    op('act', I('activation', t1[64:68, :], ps[64:68, :], AF.Exp, bias=der[64:68, 8 + h:9 + h], scale=-1.0),
                       r_=[pk, 'der'], w_=[tk1])
                    op('act', I('activation', t1[64:68, :], t1[64:68, :], AF.Ln, bias=der[64:68, 13:14]), r_=[tk1, 'der'], w_=[tk1])
                    c, ck = SF()
                    init = 0.0 if b == 0 else CC[64:68, h, 0:1]
                    op('dve', I('tensor_tensor_scan', c[64:68, :], onesf[64:68, :], t1[64:68, :], init, ALU.mult, ALU.subtract),
                       r_=[tk1, 'onesf', ('CC', h)], w_=[ck])
                    op('dve', I('tensor_copy', CC[64:68, h, 0:1], c[64:68, 511:512]), r_=[ck], w_=[('CC', h)])
                    H, hk = SB()
                    M_, mk = SB()
                    r1, rk1 = SF()
                    op('dve', I('tensor_copy', H[64:68, :], c[64:68, :]), r_=[ck], w_=[hk])
                    op('dve', I('tensor_tensor', r1[64:68, :], c[64:68, :], H[64:68, :], ALU.subtract), r_=[ck, hk], w_=[rk1])
                    op('dve', I('tensor_copy', M_[64:68, :], r1[64:68, :]), r_=[rk1], w_=[mk])
                    for (dst2, dk2, a0) in ((QA[64:68, h, :], ('QA', h), 0), (KA[64:68, h, b * 512:(b + 1) * 512], ('KA', h, b), 3)):
                        t2, tk2 = SF()
                        op('dve', I('tensor_scalar', t2[64:68, :], H[64:68, :], aug[64:68, a0:a0 + 1], None, ALU.mult),
                           r_=[hk, 'aug'], w_=[tk2])
                        op('dve', I('scalar_tensor_tensor', t2[64:68, :], M_[64:68, :], aug[64:68, a0 + 1:a0 + 2], t2[64:68, :],
                                    ALU.mult, ALU.add), r_=[mk, tk2, 'aug'], w_=[tk2])
                        op('dve', I('tensor_scalar', dst2, t2[64:68, :], aug[64:68, a0 + 2:a0 + 3], None, ALU.add),
                           r_=[tk2, 'aug'], w_=[dk2])
            two_streams(lambda: [fox_proj(h) for h in (0, 1)], lambda: [fox_proj(h) for h in (2, 3)])
            if os.environ.get('FOXSTOP') == '1':
                continue
            FS = int(os.environ.get('FOXSTOP', '9'))
            def fox_attn(h):
                po, pok = PSA()
                nk = 4 * b + 4
                def ST(i):
                    m = i - 4 * b
                    c0 = 128 * m if m > 0 else 0
                    n = 512 - c0
                    ps, pk = PS()
                    op('pe', I('matmul', ps[:, 0:n], KA[0:96, h, i * 128:(i + 1) * 128], QA[0:96, h, c0:512], start=True, stop=True),
                       r_=[('KA', h, i // 4), ('QA', h), 'KA0', 'QA0'], w_=[pk])
                    return ps, pk, m, c0, n
                cur = ST(0)
                for i in range(nk):
                    nxt = ST(i + 1) if i + 1 < nk else None
                    ps, pk, m, c0, n = cur
                    cur = nxt
                    pt, ptk = SB()
                    if m >= 0:
                        op('dve', I('tensor_scalar', ps[:, 0:128], ps[:, 0:128], 40.0, None, ALU.min), r_=[pk], w_=[pk])
                    op('act', I('activation', pt[:, 0:n], ps[:, 0:n], AF.Exp), r_=[pk], w_=[ptk])
                    if m >= 0 and FS >= 4:
                        op('dve', I('tensor_tensor', pt[:, 0:128], pt[:, 0:128], tri[:, :], ALU.mult), r_=[ptk, 'tri'], w_=[ptk])
                    if FS >= 5:
                        op('pe', I('matmul', po[0:96, c0:512], Vp[:, i, h, 0:96], pt[:, 0:n], start=(i == 0), stop=(i == nk - 1)),
                           r_=[ptk, ('Vp', i), 'Vp1'], w_=[pok])
                if FS < 6:
                    return
                sq, sk = SB()
                of, ok_ = SF()
                op('act', I('activation', sq[0:96, :], po[0:96, :], AF.Square), r_=[pok], w_=[sk])
                op('dve', I('tensor_copy', of[0:64, :], po[0:64, :]), r_=[pok, sk], w_=[ok_])
                if FS < 7:
                    return
                p2, pk2 = PS()
                op('pe', I('matmul', p2[0:64, :], wst[0:96, :], sq[0:96, :], start=True, stop=True), r_=[sk, 'wst'], w_=[pk2])
                rs, rk = rstd_from(p2, pk2, 64, 512, None)
                op('dve', I('scalar_tensor_tensor', MX[0:64, h, :], of[0:64, :], pc('fog', 0, 0, 64), rs[0:64, :],
                            ALU.mult, ALU.mult), r_=[ok_, rk, 'pp'], w_=['MX'])
            two_streams(lambda: [fox_attn(h) for h in (0, 1)], lambda: [fox_attn(h) for h in (2, 3)])
            if os.environ.get('FOXSTOP') != '2':
                wout_apply(b)

    def recurrent(l, kind):
        m = {'gdn': 1, 'hg': 2, 'gla': 3}[kind]
        gdn = kind == 'gdn'
        S32 = kb.sb("S32", [128, 2, 64], F32)
        Sb = kb.sb("Sb", [128, 2, 64], BF16)
        EL = kb.sb("EL", [128, 2, 8, 1], F32)
        OT = kb.sb("OT", [64, 4, 512], BF16)
        qT = [kb.sb("qT%d" % g, [128, 512], BF16) for g in range(2)]
        kd = [kb.sb("kd%d" % g, [128, 512], BF16) for g in range(2)]
        ko = [kb.sb("ko%d" % g, [128, 512], BF16) for g in range(2)]
        if gdn:
            qg = [kb.sb("qg%d" % g, [128, 512], BF16) for g in range(2)]
            kbt = [kb.sb("kbt%d" % g, [128, 512], BF16) for g in range(2)]
            Zl = [kb.sb("Zl%d" % g, [128, 512], F32) for g in range(2)]
            Zr = [kb.sb("Zr%d" % g, [128, 512], F32) for g in range(2)]
            kw = [kb.sb("kw%d" % g, [128, 512], BF16) for g in range(2)]
            vb = [kb.sb("vb%d" % g, [128, 512], BF16) for g in range(2)]
            RAW = [[kb.sb("raw%d%d" % (t, g), [128, 516], BF16) for g in range(2)] for t in range(3)]
        else:
            qg = [kb.sb("qg%d" % g, [128, 512], BF16) for g in range(2)]
            gkT = kb.sb("gkT", [16, 512], BF16)
        wits, wgks = W['wits'], W['wgks']
        AQs2 = [kb.sb("AQs%d" % i, [64, 512], BF16) for i in range(2)]
        KTs2 = [kb.sb("KTs%d" % i, [64, 512], BF16) for i in range(2)]
        VTs2 = [kb.sb("VTs%d" % i, [64, 512], BF16) for i in range(2)] if not gdn else [None, None]
        WTs2 = [kb.sb("WTs%d" % i, [128, 256], BF16) for i in range(2)]
        Us2 = [kb.sb("Us%d" % i, [64, 512], F32) for i in range(2)] if gdn else None
        Sb2 = [Sb, kb.sb("Sb1", [128, 2, 64], BF16)]
        cst = {'n': 0}
        op('pool', I('memset', S32[:], 0.0), w_=['S32'])
        op('pool', I('memset', Sb2[0][:], 0.0), w_=[('Sb', 0)])
        op('pool', I('memset', Sb2[1][:], 0.0), w_=[('Sb', 1)])
        kb.dma('sp', W['wos'][:], wb['wo'][l, m], r_=[('wo', l)], w_=['wos'])
        if not gdn:
            c0w = 256 if kind == 'hg' else 512
            kb.dma('sp', wits[:], wb['wit'][l, :, :, c0w:c0w + 256], r_=[('wit', l)], w_=['wits'])
        if kind == 'gla':
            kb.dma('sp', wgks[:], wb['wgk'][l], r_=[('wgk', l)], w_=['wgks'])

        def decay_tiles(tg, tgk, sc):
            tG, tGk = SF()
            op('dve', I('tensor_tensor_scan', tG[:, :], cm[:, :], tg[:, :], 0.0, ALU.mult, ALU.add), r_=[tgk, 'cm'], w_=[tGk])
            tm, tmk = SF()
            op('dve', I('tensor_tensor', v3(tm[:, :], 64), v3(tG[:, :], 64), bcl(v3(tG[:, :], 64)[:, :, 31:32], 64), ALU.subtract),
               r_=[tGk], w_=[tmk])
            op('act', I('activation', tg[:, :], tG[:, :], AF.Exp, scale=sc), r_=[tGk], w_=[tgk])
            op('act', I('activation', tG[:, :], tm[:, :], AF.Exp, scale=sc), r_=[tmk], w_=[tGk])
            op('act', I('activation', tm[:, :], tm[:, :], AF.Exp, scale=-sc), r_=[tmk], w_=[tmk])
            return (tg, tgk), (tG, tGk), (tm, tmk)

        def pre_la(b, g):
            c0 = PAD + b * 512
            if kind == 'hg':
                ps, pk = proj_f(l, 'hf%d' % g, 128, c0, 512)
                tk_, tkk = SF()
                op('act', I('activation', tk_[:, :], ps[:, :], AF.Exp), r_=[pk], w_=[tkk])
                op('dve', I('tensor_scalar', tk_[:, :], tk_[:, :], 1.0, None, ALU.add), r_=[tkk], w_=[tkk])
                op('dve', I('reciprocal', tk_[:, :], tk_[:, :]), r_=[tkk], w_=[tkk])
                op('dve', I('tensor_scalar', tk_[:, :], tk_[:, :], der[:, 2 + g:3 + g], None, ALU.mult), r_=[tkk, 'der'], w_=[tkk])
                tg, tgk = SF()
                op('act', I('activation', tg[:, :], tk_[:, :], AF.Ln, bias=der[:, 13:14], scale=-1.0), r_=[tkk, 'der'], w_=[tgk])
                sc, qs = 1.0, 0.125
            else:
                ps, pk = PS()
                op('pe', I('matmul', ps[:, :], wgks[0:16, g * 128:(g + 1) * 128], gkT[0:16, :], start=True, stop=True),
                   r_=['wgks', 'gkT'], w_=[pk])
                tg, tgk = SF()
                op('act', I('activation', tg[:, :], ps[:, :], AF.Exp, bias=der[:, 4 + g:5 + g], scale=-1.0), r_=[pk, 'der'], w_=[tgk])
                op('act', I('activation', tg[:, :], tg[:, :], AF.Ln, bias=der[:, 13:14]), r_=[tgk, 'der'], w_=[tgk])
                sc, qs = -1.0 / 16.0, float(32.0 ** -0.5)
            (E1, e1k), (E2, e2k), (E3, e3k) = decay_tiles(tg, tgk, sc)
            op('dve', I('tensor_copy', EL[:, g, :, :], v3(E1[:, :], 64)[:, :, 63:64]), r_=[e1k], w_=['EL'])
            if kind == 'hg':
                ps, pk = proj_f(l, 'hq%d' % g, 128, c0, 512)
                tq, tqk = SF()
                op('act', I('activation', tq[:, :], ps[:, :], AF.Silu), r_=[pk], w_=[tqk])
                qsrc, qk_ = tq, tqk
                op('dve', I('tensor_tensor', kd[g][:, :], tk_[:, :], E3[:, :], ALU.mult), r_=[tkk, e3k], w_=[('kd', g)])
            else:
                ps, pk = proj_f(l, 'lq%d' % g, 128, c0, 512)
                qsrc, qk_ = ps, pk
            op('dve', I('scalar_tensor_tensor', qT[g][:, :], qsrc[:, :], qs, E1[:, :], ALU.mult, ALU.mult), r_=[qk_, e1k], w_=[('qT', g)])
            op('dve', I('scalar_tensor_tensor', qg[g][:, :], qsrc[:, :], qs, E2[:, :], ALU.mult, ALU.mult), r_=[qk_, e2k], w_=[('qg', g)])
            if kind == 'gla':
                ps, pk = proj_f(l, 'lk%d' % g, 128, c0, 512)
                op('dve', I('tensor_tensor', kd[g][:, :], ps[:, :], E3[:, :], ALU.mult), r_=[pk, e3k], w_=[('kd', g)])
            op('dve', I('tensor_tensor', v3(ko[g][:, :], 64), v3(kd[g][:, :], 64), bcl(v3(E2[:, :], 64)[:, :, 63:64], 64), ALU.mult),
               r_=[('kd', g), e2k], w_=[('ko', g)])

        def gk_common(b):
            c0 = PAD + b * 512
            ps, pk = proj_f(l, 'lgk', 16, c0, 512)
            op('act', I('activation', gkT[0:16, :], ps[0:16, :], AF.Copy), r_=[pk], w_=['gkT'])

        def pre_gdn(b, g):
            c0 = PAD + b * 512
            ps, pk = proj_f(l, 'ga%d' % g, 128, c0, 512)
            ta, tak = SF()
            op('act', I('activation', ta[:, :], ps[:, :], AF.Exp, bias=pc('gdt', g)), r_=[pk, 'pp'], w_=[tak])
            op('act', I('activation', ta[:, :], ta[:, :], AF.Ln, bias=der[:, 13:14]), r_=[tak, 'der'], w_=[tak])
            op('dve', I('tensor_scalar', ta[:, :], ta[:, :], der[:, g:g + 1], None, ALU.mult), r_=[tak, 'der'], w_=[tak])
            tG, tGk = SF()
            op('dve', I('tensor_tensor_scan', tG[:, :], cm[:, :], ta[:, :], 0.0, ALU.mult, ALU.add), r_=[tak, 'cm'], w_=[tGk])
            tn, tnk = SF()
            op('dve', I('tensor_scalar', Zl[g][:, :], tG[:, :], zc[:, 0:1], zc[:, 1:2], ALU.mult, ALU.add), r_=[tGk, 'zc'], w_=[('Zl', g)])
            op('dve', I('tensor_scalar', Zr[g][:, :], tG[:, :], zc[:, 2:3], zc[:, 3:4], ALU.mult, ALU.add), r_=[tGk, 'zc'], w_=[('Zr', g)])
            op('act', I('activation', ta[:, :], tG[:, :], AF.Exp), r_=[tGk], w_=[tak])
            op('dve', I('tensor_tensor', v3(tn[:, :], 64), bcl(v3(tG[:, :], 64)[:, :, 63:64], 64), v3(tG[:, :], 64), ALU.subtract),
               r_=[tGk], w_=[tnk])
            op('act', I('activation', tn[:, :], tn[:, :], AF.Exp), r_=[tnk], w_=[tnk])
            op('dve', I('tensor_copy', EL[:, g, :, :], v3(ta[:, :], 64)[:, :, 63:64]), r_=[tak], w_=['EL'])
            ps, pk = proj_f(l, 'gb%d' % g, 128, c0, 512)
            tu, tuk = SF()
            op('act', I('activation', tu[:, :], ps[:, :], AF.Exp, scale=-1.0), r_=[pk], w_=[tuk])
            op('act', I('activation', tu[:, :], tu[:, :], AF.Ln, bias=der[:, 13:14]), r_=[tuk, 'der'], w_=[tuk])
            op('dve', I('tensor_tensor', tG[:, :], tG[:, :], tu[:, :], ALU.subtract), r_=[tGk, tuk], w_=[tGk])
            op('act', I('activation', tG[:, :], tG[:, :], AF.Exp), r_=[tGk], w_=[tGk])
            op('act', I('activation', tu[:, :], tu[:, :], AF.Exp, scale=-1.0), r_=[tuk], w_=[tuk])

            def conv(ti):
                raw = RAW[ti][g]
                rk_ = ('raw', ti, g)
                if b == 0:
                    op('pool', I('memset', raw[:, 0:4], 0.0), w_=[rk_])
                else:
                    op('pool', I('tensor_copy', raw[:, 1:4], raw[:, 513:516]), r_=[rk_], w_=[rk_])
                ps, pk = proj_f(l, ('gq', 'gk', 'gv')[ti] + str(g), 128, c0, 512)
                op('act', I('activation', raw[:, 4:516], ps[:, :], AF.Copy), r_=[pk], w_=[rk_])
                y, yk = SF()
                ci = ti * 2 + g
                op('dve', I('tensor_scalar', y[:, :], raw[:, 4:516], pc('gcw', ci * 4 + 3), None, ALU.mult), r_=[rk_, 'pp'], w_=[yk])
                for j in (2, 1, 0):
                    op('dve', I('scalar_tensor_tensor', y[:, :], raw[:, 1 + j:513 + j], pc('gcw', ci * 4 + j), y[:, :],
                                ALU.mult, ALU.add), r_=[rk_, yk, 'pp'], w_=[yk])
                op('act', I('activation', y[:, :], y[:, :], AF.Silu), r_=[yk], w_=[yk])
                return y, yk

            def l2n(y, yk, ebias):
                sq, sk = SB()
                op('act', I('activation', sq[:, :], y[:, :], AF.Square), r_=[yk], w_=[sk])
                p2, pk2 = PS()
                op('pe', I('matmul', p2[:, :], bd[:, :], sq[:, :], start=True, stop=True), r_=[sk, 'bd'], w_=[pk2])
                rs, rk = SB()
                op('act', I('activation', rs[:, :], p2[:, :], AF.Ln, bias=epsc[:, 0:1]), r_=[pk2, 'epsc'], w_=[rk])
                kw_ = {} if ebias is None else dict(bias=ebias)
                op('act', I('activation', rs[:, :], rs[:, :], AF.Exp, scale=-0.5, **kw_), r_=[rk, 'der'], w_=[rk])
                op('dve', I('tensor_tensor', y[:, :], y[:, :], rs[:, :], ALU.mult), r_=[yk, rk], w_=[yk])

            y, yk = conv(0)
            l2n(y, yk, der[:, 12:13])
            op('dve', I('tensor_tensor', qT[g][:, :], y[:, :], ta[:, :], ALU.mult), r_=[yk, tak], w_=[('qT', g)])
            op('pool', I('tensor_copy', qg[g][:, :], y[:, :]), r_=[yk], w_=[('qg', g)])
            y, yk = conv(1)
            l2n(y, yk, None)
            op('dve', I('tensor_tensor', kw[g][:, :], y[:, :], tG[:, :], ALU.mult), r_=[yk, tGk], w_=[('kw', g)])
            op('pool', I('tensor_copy', kd[g][:, :], y[:, :]), r_=[yk], w_=[('kd', g)])
            op('pool', I('tensor_tensor', kbt[g][:, :], y[:, :], tu[:, :], ALU.mult), r_=[yk, tuk], w_=[('kbt', g)])
            op('pool', I('tensor_tensor', ko[g][:, :], y[:, :], tn[:, :], ALU.mult), r_=[yk, tnk], w_=[('ko', g)])
            y, yk = conv(2)
            op('pool', I('tensor_tensor', vb[g][:, :], y[:, :], tu[:, :], ALU.mult), r_=[yk, tuk], w_=[('vb', g)])

        def hgj(j, h):
            return (j * 4 + h) * 64, h // 2, (h % 2) * 64

        def mm8(ps, pk, lhs, rhs, r_, split=False):
            groups = [(0, 2), (1, 3)] if split else [(0, 1, 2, 3)]
            for hs in groups:
                op('pe', *[I('matmul', ps[0:64, hgj(j, h)[0]:hgj(j, h)[0] + 64], lhs(j, h), rhs(j, h), start=True, stop=True)
                           for j in range(2) for h in hs], r_=r_, w_=[pk])

        def fm(tiles, sc):
            return lambda j, h: tiles[h // 2][(h % 2) * 64:(h % 2) * 64 + 64, (2 * sc + j) * 64:(2 * sc + j + 1) * 64]

        def tm(tile):
            return lambda j, h: tile[0:64, (j * 4 + h) * 64:(j * 4 + h + 1) * 64]

        def transp8(src, sc, r_, dst=None):
            pt, ptk = PT()
            for hs in ((0, 2), (1, 3)):
                op('pe', *[I('transpose', pt[0:64, hgj(j, h)[0]:hgj(j, h)[0] + 64], fm(src, sc)(j, h),
                             ident[hgj(j, h)[2]:hgj(j, h)[2] + 64, hgj(j, h)[2]:hgj(j, h)[2] + 64])
                           for j in range(2) for h in hs], r_=r_ + ['ident'], w_=[ptk])
            t, tk = dst if dst is not None else SB()
            op('act', I('activation', t[0:64, :], pt[0:64, :], AF.Copy), r_=[ptk], w_=[tk])
            return t, tk

        def A_phase(b, sc):
            res = {}
            sl = sc % 2
            AQs, KTs, VTs, WTs = AQs2[sl], KTs2[sl], VTs2[sl], WTs2[sl]
            if gdn:
                zr_ = [('Zl', 0), ('Zl', 1), ('Zr', 0), ('Zr', 1)]
                zf = lambda tiles: (lambda j, h: tiles[h // 2][(h % 2) * 64:(h % 2) * 64 + 2, (2 * sc + j) * 64:(2 * sc + j + 1) * 64])
                psD, pkD = PS()
                mm8(psD, pkD, zf(Zr), zf(Zl), zr_, split=True)
                psDT, pkDT = PS()
                mm8(psDT, pkDT, zf(Zl), zf(Zr), zr_, split=True)
                Dm, dmk = SF()
                DT, dtk = SF()
                DTu, dtuk = SF()
                op('dve', I('tensor_scalar', Dm[0:64, :], psD[0:64, :], 0.0, None, ALU.min), r_=[pkD], w_=[dmk])
                op('act', I('activation', Dm[0:64, :], Dm[0:64, :], AF.Exp), r_=[dmk], w_=[dmk])
                op('pool', I('tensor_tensor', v3(Dm[0:64, :], 64), v3(Dm[0:64, :], 64), bcm(negL[:, :], 8), ALU.mult), r_=[dmk, 'negL'], w_=[dmk])
                op('dve', I('tensor_scalar', DT[0:64, :], psDT[0:64, :], 0.0, None, ALU.min), r_=[pkDT], w_=[dtk])
                op('act', I('activation', DT[0:64, :], DT[0:64, :], AF.Exp), r_=[dtk], w_=[dtk])
                op('pool', I('tensor_tensor', v3(DTu[0:64, :], 64), v3(DT[0:64, :], 64), bcm(negU[:, :], 8), ALU.mult), r_=[dtk, 'negU'], w_=[dtuk])
                op('pool', I('tensor_tensor', v3(DT[0:64, :], 64), v3(DT[0:64, :], 64), bcm(tri[0:64, 0:64], 8), ALU.mult), r_=[dtk, dtuk, 'tri'], w_=[dtk])
            ps, pk = PS()
            mm8(ps, pk, fm(kd, sc), fm(qg, sc), [('kd', 0), ('kd', 1), ('qg', 0), ('qg', 1), ('qT', 0), ('qT', 1)], split=True)
            AQ, aqk = AQs, ('AQs', sl)
            if gdn:
                op('dve', I('tensor_tensor', AQ[0:64, :], ps[0:64, :], DT[0:64, :], ALU.mult), r_=[pk, dtk], w_=[aqk])
            else:
                op('dve', I('tensor_tensor', v3(AQ[0:64, :], 64), v3(ps[0:64, :], 64), bcm(tri[0:64, 0:64], 8), ALU.mult),
                   r_=[pk, 'tri'], w_=[aqk])
            res['AQ'] = (AQ, aqk)
            AS = int(os.environ.get('ASTOP', '9'))
            if AS < 2:
                return res
            res['KT'] = transp8(ko, sc, [('ko', 0), ('ko', 1)], dst=(KTs, ('KTs', sl)))
            if AS < 3:
                return res
            if not gdn:
                VT, vtk = VTs, ('VTs', sl)
                for j in range(2):
                    t0 = PAD + b * 512 + (2 * sc + j) * 64
                    ps, pk = PS()
                    op('pe', *[I('matmul', ps[0:64, 0:256], hT[:, kc, t0:t0 + 64], wits[:, kc, 0:256], start=(kc == 0), stop=(kc == 7))
                               for kc in range(8)], r_=['hT', 'wits'], w_=[pk])
                    op('act', I('activation', VT[0:64, j * 256:(j + 1) * 256], ps[0:64, 0:256], AF.Copy), r_=[pk], w_=[vtk])
                res['VT'] = (VT, vtk)
                return res
            kwr = [('kbt', 0), ('kbt', 1), ('kd', 0), ('kd', 1)]
            psN, pkN = PS()
            mm8(psN, pkN, fm(kbt, sc), fm(kd, sc), kwr, split=True)
            psA, pkA = PS()
            mm8(psA, pkA, fm(kd, sc), fm(kbt, sc), kwr, split=True)
            X, xk = SB()
            A_, ak = SB()
            P_, pk_ = SB()
            op('dve', I('tensor_tensor', X[0:64, :], psN[0:64, :], Dm[0:64, :], ALU.mult), r_=[pkN, dmk], w_=[xk])
            op('dve', I('tensor_tensor', A_[0:64, :], psA[0:64, :], DTu[0:64, :], ALU.mult), r_=[pkA, dtuk], w_=[ak])
            op('pool', I('tensor_tensor', v3(P_[0:64, :], 64), v3(A_[0:64, :], 64), bcm(eye[:, :], 8), ALU.add), r_=[ak, 'eye'], w_=[pk_])
            for lev in range(1, 6):
                psX, pkX = PS()
                mm8(psX, pkX, tm(A_), tm(X), [ak, xk])
                Xn, xnk = SB()
                op('act', I('activation', Xn[0:64, :], psX[0:64, :], AF.Copy), r_=[pkX], w_=[xnk])
                if lev < 5:
                    psA2, pkA2 = PS()
                    mm8(psA2, pkA2, tm(X), tm(A_), [ak, xk])
                    An, ank = SB()
                    op('dve', I('tensor_copy', An[0:64, :], psA2[0:64, :]), r_=[pkA2], w_=[ank])
                psP, pkP = PS()
                mm8(psP, pkP, tm(Xn), tm(P_), [xnk, pk_])
                Pn, pnk = SB()
                op('dve', I('tensor_tensor', Pn[0:64, :], psP[0:64, :], P_[0:64, :], ALU.add), r_=[pkP, pk_], w_=[pnk])
                X, xk = Xn, xnk
                if lev < 5:
                    A_, ak = An, ank
                P_, pk_ = Pn, pnk
            KW, kwk = transp8(kw, sc, [('kw', 0), ('kw', 1)])
            VB, vbk = transp8(vb, sc, [('vb', 0), ('vb', 1)])
            psW, pkW = PS()
            op('pe', *[I('matmul', psW[hgj(j, h)[2]:hgj(j, h)[2] + 64, (j * 2 + h // 2) * 64:(j * 2 + h // 2 + 1) * 64],
                         tm(KW)(j, h), tm(P_)(j, h), start=True, stop=True) for j in range(2) for h in range(4)],
               r_=[kwk, pk_], w_=[pkW])
            WT, wtk = WTs, ('WTs', sl)
            op('act', I('activation', WT[:, 0:256], psW[:, 0:256], AF.Copy), r_=[pkW], w_=[wtk])
            psU, pkU = PS()
            mm8(psU, pkU, tm(P_), tm(VB), [pk_, vbk])
            U, uk = Us2[sl], ('Us', sl)
            op('dve', I('tensor_copy', U[0:64, :], psU[0:64, :]), r_=[pkU], w_=[uk])
            res['WT'] = (WT, wtk)
            res['U'] = (U, uk)
            return res

        def B_phase(b, sc, res):
            AQ, aqk = res['AQ']
            KT, ktk = res['KT']
            for j in range(2):
                n = 2 * sc + j
                cs = n * 64
                cur = cst['n'] % 2
                cst['n'] += 1
                Sc, Sn = Sb2[cur], Sb2[1 - cur]
                sck, snk = ('Sb', cur), ('Sb', 1 - cur)
                if gdn:
                    WT, wtk = res['WT']
                    U, uk = res['U']
                    ps, pk = PS()
                    for hs in ((0, 2), (1, 3)):
                        op('pe', *[I('matmul', ps[0:64, h * 64:(h + 1) * 64],
                                     WT[(h % 2) * 64:(h % 2) * 64 + 64, (j * 2 + h // 2) * 64:(j * 2 + h // 2 + 1) * 64],
                                     Sc[(h % 2) * 64:(h % 2) * 64 + 64, h // 2, :], start=True, stop=True) for h in hs],
                           r_=[wtk, sck], w_=[pk])
                    VT, vtk = SB()
                    op('dve', I('tensor_tensor', VT[0:64, 0:256], U[0:64, j * 256:(j + 1) * 256], ps[0:64, 0:256], ALU.subtract),
                       r_=[uk, pk], w_=[vtk])
                    voff = 0
                else:
                    VT, vtk = res['VT']
                    voff = j * 256
                pu, puk = PS()
                op('pe', *[I('matmul', pu[(h % 2) * 64:(h % 2) * 64 + 64, (h // 2) * 64:(h // 2 + 1) * 64],
                             KT[0:64, (j * 4 + h) * 64:(j * 4 + h + 1) * 64], VT[0:64, voff + h * 64:voff + (h + 1) * 64],
                             start=True, stop=True) for h in range(4)], r_=[ktk, vtk], w_=[puk])
                for g_ in range(2):
                    op('dve', I('scalar_tensor_tensor', Sn[:, g_, :], S32[:, g_, :], EL[:, g_, n, :], pu[:, g_ * 64:(g_ + 1) * 64],
                                ALU.mult, ALU.add), r_=['S32', 'EL', puk], w_=[snk])
                for g_ in range(2):
                    op('dve', I('scalar_tensor_tensor', S32[:, g_, :], S32[:, g_, :], EL[:, g_, n, :], pu[:, g_ * 64:(g_ + 1) * 64],
                                ALU.mult, ALU.add), r_=['S32', 'EL', puk], w_=['S32'])
                po, pok = PS()

                def mS(h):
                    r0 = (h % 2) * 64
                    return I('matmul', po[0:64, h * 64:(h + 1) * 64], Sc[r0:r0 + 64, h // 2, :], qT[h // 2][r0:r0 + 64, cs:cs + 64],
                             start=True, stop=False)

                def mV(h):
                    return I('matmul', po[0:64, h * 64:(h + 1) * 64], VT[0:64, voff + h * 64:voff + (h + 1) * 64],
                             AQ[0:64, (j * 4 + h) * 64:(j * 4 + h + 1) * 64], start=False, stop=True)
                rr = [sck, ('qT', 0), ('qT', 1), vtk, aqk]
                op('pe', mS(0), mV(0), mS(2), mV(2), r_=rr, w_=[pok])
                for h_ in (1, 3):
                    op('pe', mS(h_), r_=rr, w_=[pok])
                    op('pe', mV(h_), r_=rr, w_=[pok])
                op('act', I('activation', OT[0:64, :, cs:cs + 64], v3(po[0:64, 0:256], 64), AF.Copy), r_=[pok], w_=['OT'])

        RS = int(os.environ.get('RSTOP', '9'))
        OVL = not gdn
        gn_, zn_ = {'gdn': 'gog', 'hg': 'hog', 'gla': 'log'}[kind], {'gdn': 'gz', 'hg': 'hg', 'gla': 'lg'}[kind]

        def pre_seq(b):
            kb.label = 'pre'
            if kind == 'gla':
                gk_common(b)
            for g in range(2):
                (pre_gdn if gdn else pre_la)(b, g)

        def pre_par(b):
            kb.label = 'pre'
            if kind == 'gla':
                gk_common(b)
            two_streams(lambda: (pre_gdn if gdn else pre_la)(b, 0), lambda: (pre_gdn if gdn else pre_la)(b, 1))

        def out_seq(b):
            kb.label = 'out'
            outnorm_gate(l, b, gn_, zn_, OT, seq=True)
            wout_apply(b)

        pre_par(0)
        for b in range(NB):
            if RS < 2:
                continue
            for sc0 in (0, 2):
                caps, ress = [], []
                for i_ in range(2):
                    STREAM['s'] = i_
                    kb.label = 'A%d' % (sc0 + i_)
                    caps.append(kb.capture(lambda: ress.append(A_phase(b, sc0 + i_))))
                STREAM['s'] = None
                kb.commit_interleaved(caps)
                if RS >= 3:
                    for i_ in range(2):
                        kb.label = 'B%d' % (sc0 + i_)
                        B_phase(b, sc0 + i_, ress[i_])
            kb.label = 'out'
            if RS < 4:
                continue
            if b + 1 < NB and OVL:
                two_streams(lambda: out_seq(b), lambda: pre_seq(b + 1))
            else:
                outnorm_gate(l, b, gn_, zn_, OT)
                wout_apply(b)
                if b + 1 < NB:
                    pre_par(b + 1)

    def ffn(l):
        GT = kb.sb("GT", [128, 22, 1024], BF16)
        wu = [kb.sb("wu%d" % i, [128, 8, 128], BF16) for i in range(4)]
        wd = [kb.sb("wd%d" % i, [128, 22, 128], BF16) for i in range(2)]
        passes = []
        t = 0
        while t < S:
            e = min(S, t + 1024)
            passes.append((t, e))
            t = e
        wi = 0
        for (p0, p1) in passes:
            blocks = []
            t = p0
            while t < p1:
                n = min(510, p1 - t)
                blocks.append((t, n))
                t += n
            t = p0
            while t < p1:
                n = min(512, p1 - t)
                rmsnorm('n2g', t, n)
                t += n
            for j in range(22):
                wts = []
                for tt in range(2):
                    i = wi % 4
                    wi += 1
                    kb.dma('sp', wu[i][:], wb['wup'][l, 2 * j + tt], r_=[('wup', l)], w_=[('wu', i)])
                    wts.append((wu[i], ('wu', i)))
                for (t0, n) in blocks:
                    ys = []
                    for tt in range(2):
                        c = 2 * j + tt
                        ps, pk = PS()
                        op('pe', *[I('matmul', ps[:, 0:n + 2], wts[tt][0][:, kc, :], hT[:, kc, PAD + t0 - 2:PAD + t0 + n],
                                     start=(kc == 0), stop=(kc == 7)) for kc in range(8)], r_=[wts[tt][1], 'hT'], w_=[pk])
                        y, yk = SF()
                        op('act', I('activation', y[:, 0:n], ps[:, 2:n + 2], AF.Identity, bias=pc('fcb', c), scale=pc('fcw', c * 3 + 2)),
                           r_=[pk, 'pp'], w_=[yk])
                        op('dve', I('scalar_tensor_tensor', y[:, 0:n], ps[:, 1:n + 1], pc('fcw', c * 3 + 1), y[:, 0:n], ALU.mult, ALU.add),
                           r_=[pk, yk, 'pp'], w_=[yk])
                        op('dve', I('scalar_tensor_tensor', y[:, 0:n], ps[:, 0:n], pc('fcw', c * 3), y[:, 0:n], ALU.mult, ALU.add),
                           r_=[pk, yk, 'pp'], w_=[yk])
                        ys.append((y, yk))
                    sg, sgk = SF()
                    op('act', I('activation', sg[:, 0:n], ys[0][0][:, 0:n], AF.Silu), r_=[ys[0][1]], w_=[sgk])
                    op('pool', I('tensor_tensor', GT[:, j, t0 - p0:t0 - p0 + n], sg[:, 0:n], ys[1][0][:, 0:n], ALU.mult),
                       r_=[sgk, ys[1][1]], w_=[('GT', j)])
            for oc in range(8):
                i = oc % 2
                kb.dma('sp', wd[i][:], wb['wdn'][l, oc], r_=[('wdn', l)], w_=[('wd', i)])
                for (t0, n) in blocks:
                    ps, pk = PS()
                    op('pe', *[I('matmul', ps[:, 0:n], wd[i][:, kc, :], GT[:, kc, t0 - p0:t0 - p0 + n], start=(kc == 0), stop=(kc == 21))
                               for kc in range(22)], r_=[('wd', i)] + [('GT', kc) for kc in range(22)], w_=[pk])
                    op('dve', I('tensor_tensor', xT[:, oc, t0:t0 + n], ps[:, 0:n], xT[:, oc, t0:t0 + n], ALU.add),
                       r_=[pk, ('x', oc)], w_=[('x', oc)])

    def layer_setup(l):
        kb.dma('sp', ppt[:], dr['pp'][l], w_=['pp'])
        op('act', I('activation', der[:, 0:2], ppt[:, PPC['galog']:PPC['galog'] + 2], AF.Exp), r_=['pp'], w_=['der'])
        op('dve', I('tensor_scalar', der[:, 0:2], der[:, 0:2], -1.0, None, ALU.mult), r_=['der'], w_=['der'])
        if l == 0:
            op('pool', I('memset', der[:, 2:4], 1.0), w_=['der'])
        else:
            op('dve', I('tensor_tensor', der[:, 2:4], ppt[:, PPC['hlb1']:PPC['hlb1'] + 2], ppt[:, PPC['hlb0']:PPC['hlb0'] + 2],
                        ALU.subtract), r_=['pp'], w_=['der'])
            op('act', I('activation', der[:, 2:4], der[:, 2:4], AF.Exp), r_=['der'], w_=['der'])
            op('dve', I('tensor_scalar', der[:, 2:4], der[:, 2:4], 1.0, None, ALU.add), r_=['der'], w_=['der'])
            op('dve', I('reciprocal', der[:, 2:4], der[:, 2:4]), r_=['der'], w_=['der'])
        op('dve', I('tensor_scalar', der[:, 4:6], ppt[:, PPC['lbgk']:PPC['lbgk'] + 2], -1.0, None, ALU.mult), r_=['pp'], w_=['der'])
        op('dve', I('tensor_scalar', der[:, 8:12], ppt[:, PPC['fbf']:PPC['fbf'] + 4], -1.0, None, ALU.mult), r_=['pp'], w_=['der'])
        op('pool', I('memset', der[:, 12:13], -float(np.log(8.0))), w_=['der'])
        op('pool', I('memset', der[:, 13:14], 1.0), w_=['der'])

    for n in range(NSEQ):
        for c in range(8):
            kb.dma('sp', xT[:, c, :], dr['xT'][n, :, c, :], w_=[('x', c)])
        for l in range(L):
            layer_setup(l)
            with Phase():
                alloc_mixer_weights()
                for b in range(NB):
                    rmsnorm('n1g', b * 512, 512)
                for mi, fn in enumerate((lambda: fox(l), lambda: recurrent(l, 'gdn'), lambda: recurrent(l, 'hg'),
                                         lambda: recurrent(l, 'gla'))):
                    if mi in MIXERS:
                        with Phase():
                            fn()
            if FFN:
                with Phase():
                    ffn(l)
        for c in range(8):
            kb.dma('sp', yT[n, :, c, :], xT[:, c, :], r_=[('x', c)], out_final=True)
    kb.emit()
    nc._kb_labels = kb.labels
    return nc


_CACHE = {}


def kernel(**inputs):
    x = np.asarray(inputs['x'], np.float32)
    B, S, _ = x.shape
    L = np.asarray(inputs['w_in']).shape[0]
    nseq = B // NCORES
    packed = pack_weights(inputs)
    key = (S, nseq, L)
    if key not in _CACHE:
        _CACHE[key] = build_program(S, nseq, L)
    nc = _CACHE[key]
    in_maps = []
    for c in range(NCORES):
        xs = x[c * nseq:(c + 1) * nseq]
        xt = np.ascontiguousarray(xs.reshape(nseq, S, 8, 128).transpose(0, 3, 2, 1))
        m = {"xT": xt}
        m.update(packed)
        in_maps.append(m)
    res = run_bass_kernel_spmd(nc, in_maps, core_ids=list(range(NCORES)))
    out = np.empty((B, S, D), np.float32)
    for c in range(NCORES):
        yt = np.asarray(res.results[c]["yT"])
        out[c * nseq:(c + 1) * nseq] = yt.transpose(0, 3, 2, 1).reshape(nseq, S, D)
    return out
```
